# Optimizing a Trainium2 kernel written in Bass

```python
import jax, jax.numpy as jnp
from jax import lax
import numpy as np

D_MODEL = 1024
BATCH = 2
SEQ = 16384
DEPTH = 2

GRID_W = 64
CTX_LEN = 256
HEAD_DIM = 64
ATTN_WIDTH = D_MODEL // 2
RET_WIDTH = D_MODEL // 4
POOL_WIDTH = D_MODEL // 4
ATTN_Q_HEADS = ATTN_WIDTH // HEAD_DIM
ATTN_KV_HEADS = 2
ATTN_GROUP = ATTN_Q_HEADS // ATTN_KV_HEADS
KV_WIDTH = ATTN_KV_HEADS * HEAD_DIM
RET_HEADS = RET_WIDTH // HEAD_DIM
POOL_WINDOWS = (2, 4, 8, 16)
POOL_GROUP = POOL_WIDTH // len(POOL_WINDOWS)
MIX_WIDTH = ATTN_WIDTH + RET_WIDTH + POOL_WIDTH
IN_WIDTH = ATTN_WIDTH + 2 * KV_WIDTH + 4 * RET_WIDTH + POOL_WIDTH
Q_BLOCK = 128
RET_CHUNK = 128
ROPE_THETA = 10000.0
D_FF = ((8 * D_MODEL // 3 + 127) // 128) * 128
N_EXPERTS = 8
TOP_K = 2
N_DENSE = (DEPTH + 1) // 2
N_MOE = DEPTH // 2
NORM_EPS = 1e-6

kernel_name = 'hybrid_attn_retention_pool_moe_dit'


def rms_norm(x, g):
    xf = x.astype(jnp.float32)
    y = xf * lax.rsqrt(jnp.mean(xf * xf, axis=-1, keepdims=True) + NORM_EPS)
    return (y * g.astype(jnp.float32)).astype(x.dtype)


def axial_rope_tables(n_tokens):
    rows = n_tokens // GRID_W
    row_pos = jnp.repeat(jnp.arange(rows, dtype=jnp.float32), GRID_W)
    col_pos = jnp.tile(jnp.arange(GRID_W, dtype=jnp.float32), rows)
    n_freq = HEAD_DIM // 4
    inv = ROPE_THETA ** (-jnp.arange(n_freq, dtype=jnp.float32) / n_freq)
    ang = jnp.stack([row_pos[:, None] * inv, col_pos[:, None] * inv], axis=1)
    return jnp.cos(ang), jnp.sin(ang)


def apply_axial_rope(x, cos, sin):
    xs = x.astype(jnp.float32).reshape(x.shape[:-1] + (2, 2, HEAD_DIM // 4))
    x1 = xs[..., :, 0, :]
    x2 = xs[..., :, 1, :]
    out = jnp.stack([x1 * cos - x2 * sin, x2 * cos + x1 * sin], axis=-2)
    return out.reshape(x.shape).astype(x.dtype)


def _split_projection(p):
    sizes = (ATTN_WIDTH, KV_WIDTH, KV_WIDTH, RET_WIDTH, RET_WIDTH, RET_WIDTH, RET_WIDTH, POOL_WIDTH)
    out, off = [], 0
    for s in sizes:
        out.append(p[..., off:off + s])
        off += s
    return out


def _attn_heads(q, k, v, q_gain, k_gain):
    B, T, _ = q.shape
    q = q.reshape(B, T, ATTN_KV_HEADS, ATTN_GROUP, HEAD_DIM).transpose(0, 2, 3, 1, 4)
    k = k.reshape(B, T, ATTN_KV_HEADS, HEAD_DIM).transpose(0, 2, 1, 3)
    v = v.reshape(B, T, ATTN_KV_HEADS, HEAD_DIM).transpose(0, 2, 1, 3)
    return rms_norm(q, q_gain), rms_norm(k, k_gain), v


def _sdpa(q, k, v):
    s = jnp.einsum('bkgqd,bknd->bkgqn', q, k).astype(jnp.float32) * (HEAD_DIM ** -0.5)
    p = jax.nn.softmax(s, axis=-1).astype(v.dtype)
    return jnp.einsum('bkgqn,bknd->bkgqd', p, v)


def _blocked_attention(q, k, v):
    B, KVH, G, T, d = q.shape
    nb = T // Q_BLOCK
    qb = q.reshape(B, KVH, G, nb, Q_BLOCK, d).transpose(3, 0, 1, 2, 4, 5)
    ob = lax.map(lambda qi: _sdpa(qi, k, v), qb)
    return ob.transpose(1, 2, 3, 0, 4, 5).reshape(B, KVH, G, T, d)


def _merge_attn(o):
    B, KVH, G, T, d = o.shape
    return o.transpose(0, 3, 1, 2, 4).reshape(B, T, ATTN_WIDTH)


def _ret_heads(t):
    B, T, _ = t.shape
    return t.reshape(B, T, RET_HEADS, HEAD_DIM).transpose(0, 2, 1, 3)


def retention_scan(q, k, v, log_gamma, state0):
    B, H, T, d = q.shape
    C = RET_CHUNK
    n = T // C
    qc = q.astype(jnp.float32).reshape(B, H, n, C, d)
    kc = k.astype(jnp.float32).reshape(B, H, n, C, d)
    vc = v.astype(jnp.float32).reshape(B, H, n, C, d)
    pos = jnp.arange(C, dtype=jnp.float32)
    lg = log_gamma.astype(jnp.float32)[:, None]
    diff = pos[:, None] - pos[None, :]
    dmat = jnp.where(diff >= 0, jnp.exp(lg[:, :, None] * jnp.maximum(diff, 0.0)), 0.0)
    scores = jnp.einsum('bhncd,bhnmd->bhncm', qc, kc) * dmat[:, None]
    intra = jnp.einsum('bhncm,bhnme->bhnce', scores, vc)
    zeta = jnp.exp(lg * (C - 1 - pos))
    upd = jnp.einsum('bhncd,hc,bhnce->nbhde', kc, zeta, vc)
    decay_chunk = jnp.exp(lg[:, 0] * C)[None, :, None, None]

    def step(state, u):
        return decay_chunk * state + u, state

    final, prev = lax.scan(step, state0, upd)
    xi = jnp.exp(lg * (pos + 1))
    cross = jnp.einsum('bhncd,hc,nbhde->bhnce', qc, xi, prev)
    return (intra + cross).reshape(B, H, T, d), final


def _bi_retention(q, k, v, lg_fwd, lg_bwd, init_fwd, init_bwd):
    of, sf = retention_scan(q, k, v, lg_fwd, init_fwd)
    fl = lambda t: jnp.flip(t, axis=2)
    ob, sb = retention_scan(fl(q), fl(k), fl(v), lg_bwd, init_bwd)
    return of + fl(ob), sf, sb


def _ret_output(o, g):
    mu = jnp.mean(o, axis=-1, keepdims=True)
    var = jnp.mean(jnp.square(o - mu), axis=-1, keepdims=True)
    y = (o - mu) * lax.rsqrt(var + NORM_EPS)
    B, H, T, d = o.shape
    y = y.transpose(0, 2, 1, 3).reshape(B, T, RET_WIDTH).astype(g.dtype)
    return jax.nn.silu(g) * y


def multiscale_pool(p, w_pool, pool_scale):
    B, T, _ = p.shape
    ng = len(POOL_WINDOWS)
    pg = p.astype(jnp.float32).reshape(B, T, ng, POOL_GROUP)
    cs = jnp.concatenate([jnp.zeros((B, 1, ng, POOL_GROUP), jnp.float32), jnp.cumsum(pg, axis=1)], axis=1)
    t = jnp.arange(T)
    outs = []
    for gi, w in enumerate(POOL_WINDOWS):
        lo = jnp.clip(t - w // 2, 0, T)
        hi = jnp.clip(t + w // 2, 0, T)
        csg = cs[:, :, gi]
        mean = (csg[:, hi] - csg[:, lo]) / (hi - lo).astype(jnp.float32)[None, :, None]
        outs.append(mean - pg[:, :, gi])
    mixed = jnp.stack(outs, axis=2)
    y = jnp.einsum('btgc,gcd->btgd', mixed, w_pool.astype(jnp.float32)).reshape(B, T, POOL_WIDTH)
    return (y * pool_scale.astype(jnp.float32)).astype(p.dtype)


def token_mixers(hx, hc, w_in, w_out, q_gain, k_gain, decay_logit, pool_w, pool_scale, cos, sin, need_ctx):
    B = hx.shape[0]
    aqx, akx, avx, rqx, rkx, rvx, rgx, ppx = _split_projection(hx @ w_in)
    aqc, akc, avc, rqc, rkc, rvc, rgc, ppc = _split_projection(hc @ w_in)

    qx, kx, vx = _attn_heads(aqx, akx, avx, q_gain, k_gain)
    qc, kc, vc = _attn_heads(aqc, akc, avc, q_gain, k_gain)
    qx = apply_axial_rope(qx, cos, sin)
    kx = apply_axial_rope(kx, cos, sin)
    k_all = jnp.concatenate([kc, kx], axis=2)
    v_all = jnp.concatenate([vc, vx], axis=2)
    attn_x = _merge_attn(_blocked_attention(qx, k_all, v_all))

    log_gamma = jax.nn.log_sigmoid(decay_logit.astype(jnp.float32))
    k_scale = HEAD_DIM ** -0.5
    zero = jnp.zeros((B, RET_HEADS, HEAD_DIM, HEAD_DIM), jnp.float32)
    ret_c, s_fwd, s_bwd = _bi_retention(_ret_heads(rqc), _ret_heads(rkc) * k_scale, _ret_heads(rvc),
                                        log_gamma[0], log_gamma[1], zero, zero)
    ret_x, _, _ = _bi_retention(_ret_heads(rqx), _ret_heads(rkx) * k_scale, _ret_heads(rvx),
                                log_gamma[0], log_gamma[1], s_fwd, s_bwd)
    ret_x = _ret_output(ret_x, rgx)

    pool_x = multiscale_pool(ppx, pool_w, pool_scale)

    mx = jnp.concatenate([attn_x.astype(hx.dtype), ret_x.astype(hx.dtype), pool_x], axis=-1) @ w_out
    if not need_ctx:
        return mx, None
    attn_c = _merge_attn(_sdpa(qc, kc, vc))
    ret_c = _ret_output(ret_c, rgc)
    pool_c = multiscale_pool(ppc, pool_w, pool_scale)
    mc = jnp.concatenate([attn_c.astype(hc.dtype), ret_c.astype(hc.dtype), pool_c], axis=-1) @ w_out
    return mx, mc


def swiglu(h, w1, w3, w2):
    return (jax.nn.silu(h @ w1) * (h @ w3)) @ w2


def moe_swiglu(h, router, w1, w3, w2):
    logits = (h @ router).astype(jnp.float32)
    top_vals, top_idx = lax.top_k(logits, TOP_K)
    top_w = jax.nn.softmax(top_vals, axis=-1)
    gates = jnp.sum(jax.nn.one_hot(top_idx, N_EXPERTS, dtype=jnp.float32) * top_w[..., None], axis=-2)
    out = jnp.zeros_like(h)
    for e in range(N_EXPERTS):
        out = out + gates[..., e:e + 1].astype(h.dtype) * swiglu(h, w1[e], w3[e], w2[e])
    return out


def setup_inputs(seed: int = 0) -> dict:
    key = jax.random.key(seed)
    ks = jax.random.split(key, 26)
    nrm = lambda k, shape, s: jax.random.normal(k, shape, jnp.float32) * s
    base_gamma = 1.0 - 2.0 ** (-5.0 - jnp.arange(RET_HEADS, dtype=jnp.float32))
    base_logit = jnp.log(base_gamma) - jnp.log1p(-base_gamma)
    return {
        'x': nrm(ks[0], (BATCH, SEQ, D_MODEL), 1.0),
        'c': nrm(ks[1], (BATCH, D_MODEL), 1.0),
        'ctx': nrm(ks[2], (BATCH, CTX_LEN, D_MODEL), 1.0),
        'c_ctx': nrm(ks[3], (D_MODEL,), 1.0),
        'w_mod': nrm(ks[4], (DEPTH, D_MODEL, 6 * D_MODEL), 0.5 * D_MODEL ** -0.5),
        'b_mod': nrm(ks[5], (DEPTH, 6 * D_MODEL), 0.02),
        'norm_pre_mix': 1.0 + nrm(ks[6], (DEPTH, D_MODEL), 0.05),
        'norm_post_mix': 1.0 + nrm(ks[7], (DEPTH, D_MODEL), 0.05),
        'norm_pre_ffn': 1.0 + nrm(ks[8], (DEPTH, D_MODEL), 0.05),
        'norm_post_ffn': 1.0 + nrm(ks[9], (DEPTH, D_MODEL), 0.05),
        'w_in': nrm(ks[10], (DEPTH, D_MODEL, IN_WIDTH), D_MODEL ** -0.5),
        'w_out': nrm(ks[11], (DEPTH, MIX_WIDTH, D_MODEL), MIX_WIDTH ** -0.5),
        'q_norm': 1.0 + nrm(ks[12], (DEPTH, HEAD_DIM), 0.05),
        'k_norm': 1.0 + nrm(ks[13], (DEPTH, HEAD_DIM), 0.05),
        'ret_decay_logit': jnp.broadcast_to(base_logit, (DEPTH, 2, RET_HEADS)) + nrm(ks[14], (DEPTH, 2, RET_HEADS), 0.1),
        'pool_w': nrm(ks[15], (DEPTH, len(POOL_WINDOWS), POOL_GROUP, POOL_GROUP), POOL_GROUP ** -0.5),
        'pool_scale': 1.0 + nrm(ks[16], (DEPTH, POOL_WIDTH), 0.05),
        'ffn_w1': nrm(ks[17], (N_DENSE, D_MODEL, D_FF), D_MODEL ** -0.5),
        'ffn_w3': nrm(ks[18], (N_DENSE, D_MODEL, D_FF), D_MODEL ** -0.5),
        'ffn_w2': nrm(ks[19], (N_DENSE, D_FF, D_MODEL), D_FF ** -0.5),
        'moe_router': nrm(ks[20], (N_MOE, D_MODEL, N_EXPERTS), D_MODEL ** -0.5),
        'moe_w1': nrm(ks[21], (N_MOE, N_EXPERTS, D_MODEL, D_FF), D_MODEL ** -0.5),
        'moe_w3': nrm(ks[22], (N_MOE, N_EXPERTS, D_MODEL, D_FF), D_MODEL ** -0.5),
        'moe_w2': nrm(ks[23], (N_MOE, N_EXPERTS, D_FF, D_MODEL), D_FF ** -0.5),
    }


def reference(x, c, ctx, c_ctx, w_mod, b_mod, norm_pre_mix, norm_post_mix, norm_pre_ffn, norm_post_ffn,
              w_in, w_out, q_norm, k_norm, ret_decay_logit, pool_w, pool_scale,
              ffn_w1, ffn_w3, ffn_w2, moe_router, moe_w1, moe_w3, moe_w2):
    n_tokens = x.shape[1]
    cos, sin = axial_rope_tables(n_tokens)
    xc = ctx
    silu_c = jax.nn.silu(c)
    silu_cc = jax.nn.silu(c_ctx)
    for i in range(DEPTH):
        need_ctx = i < DEPTH - 1
        mod_x = (silu_c @ w_mod[i] + b_mod[i])[:, None, :]
        mod_c = (silu_cc @ w_mod[i] + b_mod[i])[None, None, :]
        sh1, sc1, g1, sh2, sc2, g2 = jnp.split(mod_x, 6, axis=-1)
        csh1, csc1, cg1, csh2, csc2, cg2 = jnp.split(mod_c, 6, axis=-1)

        hx = rms_norm(x, norm_pre_mix[i]) * (1.0 + sc1) + sh1
        hc = rms_norm(xc, norm_pre_mix[i]) * (1.0 + csc1) + csh1
        mx, mc = token_mixers(hx, hc, w_in[i], w_out[i], q_norm[i], k_norm[i], ret_decay_logit[i],
                              pool_w[i], pool_scale[i], cos, sin, need_ctx)
        x = x + g1 * rms_norm(mx, norm_post_mix[i])
        if need_ctx:
            xc = xc + cg1 * rms_norm(mc, norm_post_mix[i])

        j = i // 2
        if i % 2 == 0:
            ffn = lambda h: swiglu(h, ffn_w1[j], ffn_w3[j], ffn_w2[j])
        else:
            ffn = lambda h: moe_swiglu(h, moe_router[j], moe_w1[j], moe_w3[j], moe_w2[j])
        hx = rms_norm(x, norm_pre_ffn[i]) * (1.0 + sc2) + sh2
        x = x + g2 * rms_norm(ffn(hx), norm_post_ffn[i])
        if need_ctx:
            hc = rms_norm(xc, norm_pre_ffn[i]) * (1.0 + csc2) + csh2
            xc = xc + cg2 * rms_norm(ffn(hc), norm_post_ffn[i])
    return x
```

```python
import contextlib
import numpy as np
import ml_dtypes
import concourse.bass as bass
import concourse.mybir as mybir
from concourse.bass_utils import run_bass_kernel_spmd

F32 = mybir.dt.float32
BF16 = mybir.dt.bfloat16
AF = mybir.ActivationFunctionType
ALU = mybir.AluOpType
AX = mybir.AxisListType
NPBF = ml_dtypes.bfloat16

D = 1024
CT = 256
DFF = 2816
NF = DFF // 128
NE = 8
EPS = 1e-6
C = 128

ENGS = ["pe", "act", "dve", "pool", "sp"]
NDMA = 8
SAME_ENG_SYNC = {"pe": False, "act": True, "dve": True, "pool": True, "sp": False}


class Prog:
    def __init__(self, nc, stack):
        self.nc = nc
        self.ops = {e: [] for e in ENGS}
        self.count = {e: 0 for e in ENGS}
        self.waited = {e: {} for e in ENGS}
        self.last_w = {}
        self.readers = {}
        self.dma_n = {e: 0 for e in ENGS}
        self.sems = {}
        for e in ENGS:
            self.sems[("c", e)] = stack.enter_context(nc.semaphore("c_" + e))
        for e in ("sp", "pool", "act"):
            for i in range(NDMA):
                self.sems[("d", e, i)] = stack.enter_context(nc.semaphore("d_%s%d" % (e, i)))
        self.semval = {k: 0 for k in self.sems}
        self.nops = 0

    def op(self, eng, fn, reads=(), writes=(), dma=False):
        deps = {}

        def add(d):
            if d is None:
                return
            sk, v = d
            if deps.get(sk, 0) < v:
                deps[sk] = v

        for k in reads:
            add(self.last_w.get(k))
        for k in writes:
            add(self.last_w.get(k))
            for sk, v in self.readers.get(k, {}).items():
                add((sk, v))
        if dma:
            n = self.dma_n[eng]
            idx, use = n % NDMA, n // NDMA
            semkey = ("d", eng, idx)
            val = 16 * (use + 1)
            if use > 0:
                add((semkey, 16 * use))
            self.dma_n[eng] += 1
            inc = 16
        else:
            self.count[eng] += 1
            semkey = ("c", eng)
            val = self.count[eng]
            inc = 1
        self.semval[semkey] = val
        for sk, v in deps.items():
            if sk == ("c", eng) and not SAME_ENG_SYNC[eng]:
                continue
            if self.waited[eng].get(sk, 0) >= v:
                continue
            self.waited[eng][sk] = v
            s = self.sems[sk]
            self.ops[eng].append(lambda e, s=s, v=v: e.wait_ge(s, v))
        s = self.sems[semkey]
        self.ops[eng].append(lambda e, s=s, inc=inc: fn(e).then_inc(s, inc))
        me = (semkey, val)
        for k in reads:
            r = self.readers.setdefault(k, {})
            if r.get(semkey, 0) < val:
                r[semkey] = val
        for k in writes:
            self.last_w[k] = me
            self.readers[k] = {}
        self.nops += 1

    def new_phase(self, stack):
        self.phase_id = getattr(self, "phase_id", 0) + 1
        for e in ENGS:
            k = ("c", e)
            self.sems[k] = stack.enter_context(self.nc.semaphore("c%d_%s" % (self.phase_id, e)))
            self.semval[k] = 0
            self.count[e] = 0
            for w in self.waited.values():
                w.pop(k, None)

    def barrier(self):
        for e in ENGS:
            for sk, v in self.semval.items():
                if v == 0:
                    continue
                if sk == ("c", e) and e in ("pe", "sp"):
                    continue
                if self.waited[e].get(sk, 0) >= v:
                    continue
                self.waited[e][sk] = v
                s = self.sems[sk]
                self.ops[e].append(lambda en, s=s, v=v: en.wait_ge(s, v))
        self.last_w = {}
        self.readers = {}

    def emit(self):
        nc = self.nc
        ops = self.ops
        with nc.Block() as block:
            @block.tensor
            def _(e):
                for f in ops["pe"]:
                    f(e)

            @block.scalar
            def _(e):
                for f in ops["act"]:
                    f(e)

            @block.vector
            def _(e):
                for f in ops["dve"]:
                    f(e)

            @block.gpsimd
            def _(e):
                for f in ops["pool"]:
                    f(e)

            @block.sync
            def _(e):
                for f in ops["sp"]:
                    f(e)
        self.ops = {e: [] for e in ENGS}

    def dma(self, out, in_, reads=(), writes=(), eng="sp", **kw):
        self.op(eng, lambda e: e.dma_start(out=out, in_=in_, **kw), reads, writes, dma=True)

    def mm(self, out, lhsT, rhs, start, stop, reads=(), writes=(), **kw):
        self.op("pe", lambda e: e.matmul(out, lhsT, rhs, start=start, stop=stop, **kw), reads, writes)

    def tr(self, out, in_, ident, reads=(), writes=()):
        self.op("pe", lambda e: e.transpose(out, in_, ident), reads, writes)

    def act(self, out, in_, func, reads=(), writes=(), **kw):
        self.op("act", lambda e: e.activation(out, in_, func, **kw), reads, writes)

    def ts(self, eng, out, in0, s1, s2, op0, op1=None, reads=(), writes=()):
        if op1 is None:
            self.op(eng, lambda e: e.tensor_scalar(out, in0, s1, None, op0), reads, writes)
        else:
            self.op(eng, lambda e: e.tensor_scalar(out, in0, s1, s2, op0, op1), reads, writes)

    def tt(self, eng, out, in0, in1, op, reads=(), writes=()):
        self.op(eng, lambda e: e.tensor_tensor(out, in0, in1, op), reads, writes)

    def stt(self, eng, out, in0, scalar, in1, op0, op1, reads=(), writes=()):
        self.op(eng, lambda e: e.scalar_tensor_tensor(out, in0, scalar, in1, op0, op1), reads, writes)

    def cp(self, eng, out, in_, reads=(), writes=()):
        if eng == "act":
            self.op(eng, lambda e: e.copy(out, in_), reads, writes)
        else:
            self.op(eng, lambda e: e.tensor_copy(out, in_), reads, writes)


class Builder:
    def __init__(self, T, layer, phases, n_exp):
        self.T = T
        self.Tl = T // 4
        self.Tc = CT + self.Tl
        self.NCL = self.Tl // 128
        self.NKT = (CT + T) // 128
        self.layer = layer
        self.phases = phases
        self.n_exp = n_exp
        self.need_ctx = layer == 0
        self.nc = bass.Bass("TRN2", target_bir_lowering=False)
        self.dr = {}
        self.blocks = [(0, CT)] + [(CT + 512 * i, 512) for i in range(self.Tl // 512)]

    def din(self, name, shape, dt):
        self.dr[name] = self.nc.dram_tensor(name, list(shape), dt, kind="ExternalInput").ap()
        return self.dr[name]

    def dout(self, name, shape, dt):
        self.dr[name] = self.nc.dram_tensor(name, list(shape), dt, kind="ExternalOutput").ap()
        return self.dr[name]

    def build(self):
        nc = self.nc
        with contextlib.ExitStack() as gst:
            self.P = Prog(nc, gst)
            if "A" in self.phases:
                self.declare_A()
            if "B" in self.phases:
                self.declare_B()
            if "A" in self.phases:
                with contextlib.ExitStack() as st:
                    self.st = st
                    self.common_consts()
                    self.emit_mod()
                    import os as _os
                    self.kstop = int(_os.environ.get("KSTOP", "0"))
                    if self.kstop != 1:
                        self.phase_A()
                    self.P.barrier()
                    self.P.emit()
            if "B" in self.phases:
                with contextlib.ExitStack() as st:
                    self.st = st
                    self.common_consts()
                    self.emit_mod()
                    import os as _os
                    self.kb = int(_os.environ.get("KSTOPB", "0"))
                    self.phase_B1()
                    self.P.barrier()
                    self.P.emit()
                    self.P.new_phase(gst)
                with contextlib.ExitStack() as st:
                    self.st = st
                    self.common_consts()
                    self.emit_mod(only_ffn=True)
                    if self.kb in (0, 6, 7):
                        self.phase_B2()
                    self.P.barrier()
                    self.P.emit()
        return nc

    def sb(self, name, shape, dt):
        self.uid = getattr(self, "uid", 0) + 1
        return self.st.enter_context(self.nc.sbuf_tensor("s%d_%s" % (self.uid, name), list(shape), dt))

    def ps(self, name, shape, dt):
        self.uid = getattr(self, "uid", 0) + 1
        return self.st.enter_context(self.nc.psum_tensor("p%d_%s" % (self.uid, name), list(shape), dt))

    def declare_common(self):
        if "xin" in self.dr:
            return
        Tl, Tc = self.Tl, self.Tc
        self.din("xin", [Tl, D], F32)
        self.din("cin", [CT, D], F32)
        self.din("cvec", [2, D], F32)
        self.din("w_mod", [D, 6 * D], F32)
        self.din("b_mod", [6 * D], F32)
        self.din("norms", [4, D], F32)
        self.din("decay", [8], F32)
        self.din("ident", [128, 128], F32)
        self.din("zexp", [128, 2], F32)
        self.din("nC", [128, self.NCL + 2], F32)

    def declare_A(self):
        self.declare_common()
        Tl, Tc = self.Tl, self.Tc
        self.din("w_fm", [D, 1792], F32)
        self.din("w_tm", [D, 1152], F32)
        self.din("gains", [128, 4], F32)
        self.din("bones", [128, 128], F32)
        self.din("cosT", [128, Tc], F32)
        self.din("sinT", [128, Tc], F32)
        o = self.dout if "B" not in self.phases else self.dint
        o("QT", [128, 4, Tc], BF16)
        o("KT", [128, Tc], BF16)
        o("VA", [Tc, 132], BF16)
        o("rqT", [128, 2, Tc], BF16)
        o("rkT", [128, 2, Tc], BF16)
        o("rk", [Tc, 256], BF16)
        o("rv", [Tc, 256], BF16)
        o("rg", [Tc, 256], BF16)
        o("pp", [Tc, 256], BF16)
        o("Lout", [256, 128], F32)

    def declare_B(self):
        self.declare_common()
        Tl, Tc, T = self.Tl, self.Tc, self.T
        if "A" not in self.phases:
            i = self.din
            i("QT", [128, 4, Tc], BF16)
            i("KT", [128, Tc], BF16)
            i("VA", [Tc, 132], BF16)
            i("rqT", [128, 2, Tc], BF16)
            i("rkT", [128, 2, Tc], BF16)
            i("rk", [Tc, 256], BF16)
            i("rv", [Tc, 256], BF16)
            i("rg", [Tc, 256], BF16)
            i("pp", [Tc, 256], BF16)
            i("KTg", [4 * 128, Tl], BF16)
            i("VAg", [4 * Tl, 132], BF16)
            i("Lg", [4 * 256, 128], F32)
            i("Hg", [64, 256], BF16)
        self.din("w_out", [D, D], F32)
        self.din("dmask", [128, 4, 128], F32)
        self.din("xiexp", [128, 2, 128], F32)
        self.din("sel", [128, 4], F32)
        self.din("Amain", [128, 12, 128], BF16)
        self.din("Anb", [128, 8, 128], BF16)
        self.din("Actx", [128, 8, 128], BF16)
        self.din("Ahalo", [64, 8, 128], BF16)
        self.din("pool_w", [64, 4, 64], F32)
        self.din("pool_s", [128, 2], F32)
        E = self.n_exp
        self.din("w1", [E, D, DFF], F32)
        self.din("w3", [E, D, DFF], F32)
        self.din("w2", [E, DFF, D], F32)
        if E > 1:
            self.din("router", [D, NE], F32)
        self.dout("xout", [Tl, D], F32)
        if self.need_ctx:
            self.dout("cout", [CT, D], F32)
        self.dr["hTd"] = self.nc.dram_tensor("hTd", [len(self.blocks), 128, 8, 512], BF16, kind="Internal").ap()
        self.dr["accd"] = self.nc.dram_tensor("accd", [self.Tc, D], F32, kind="Internal").ap()

    def dint(self, name, shape, dt):
        self.dr[name] = self.nc.dram_tensor(name, list(shape), dt, kind="Internal").ap()
        return self.dr[name]

    def common_consts(self):
        P, dr = self.P, self.dr
        self.identf = self.sb("identf", [128, 128], F32)
        self.identb = self.sb("identb", [128, 128], BF16)
        P.dma(self.identf[:], dr["ident"], writes=["identf"])
        P.cp("dve", self.identb[:], self.identf[:], ["identf"], ["identb"])

    def emit_mod(self, only_ffn=False):
        P, dr = self.P, self.dr
        self.modT = self.sb("modT", [128, 2, 4, 8], F32)
        self.gv = self.sb("gv", [128, 2, D], F32)
        if "modd" not in dr:
            dr["modd"] = self.nc.dram_tensor("modd", [2, 6 * D], F32, kind="Internal").ap()
        outer = self.st
        with contextlib.ExitStack() as tmp:
            self.st = tmp
            cv = self.sb("cv", [128, 2, 8], F32)
            scv = self.sb("scv", [128, 2, 8], F32)
            for s_ in range(2):
                P.dma(cv[:, s_, :], dr["cvec"][s_].rearrange("(c p) -> p c", p=128), reads=["cv"], writes=["cv"],
                      allow_slow_non_contiguous=True)
            P.act(scv[:], cv[:], AF.Silu, ["cv"], ["scv"])
            modrow = self.sb("modrow", [2, 6 * D], F32)
            brow = self.sb("brow", [2, 6 * D], F32)
            P.dma(brow[:], dr["b_mod"].partition_broadcast(2), writes=["brow"])
            wm = [self.sb("wm%d" % i, [128, 8, 512], F32) for i in range(2)]
            pm = self.ps("pm", [128, 512], F32)
            for j in range(12):
                w = wm[j % 2]
                P.dma(w[:], dr["w_mod"][:, j * 512:(j + 1) * 512].rearrange("(c p) n -> p c n", p=128),
                      writes=["wm%d" % (j % 2)])
                for c in range(8):
                    P.mm(pm[0:2, :], scv[:, :, c], w[:, c, :], c == 0, c == 7, ["scv", "wm%d" % (j % 2)], ["pm"])
                P.tt("dve", modrow[:, j * 512:(j + 1) * 512], pm[0:2, :], brow[:, j * 512:(j + 1) * 512], ALU.add,
                     ["pm", "brow"], ["modrow"])
            P.dma(dr["modd"], modrow[:], reads=["modrow"], writes=["modd"])
            nT = self.sb("nT", [128, 4, 8], F32)
            for k_ in range(4):
                P.dma(nT[:, k_, :], dr["norms"][k_].rearrange("(c p) -> p c", p=128), reads=["nT"], writes=["nT"],
                      allow_slow_non_contiguous=True)
            mT = self.sb("mT", [128, 6, 8, 2], F32)
            pmt = self.ps("pmt", [128, 48, 2], F32)
            for v in range(6):
                for c in range(8):
                    P.tr(pmt[:, v * 8 + c, :], modrow[0:2, v * D + c * 128: v * D + (c + 1) * 128], self.identf[0:2, 0:2],
                         ["modrow", "identf"], ["pmt"])
            P.cp("dve", mT[:].rearrange("p v c s -> p (v c s)"), pmt[:].rearrange("p a s -> p (a s)"), ["pmt"], ["mT"])
            for s_ in range(2):
                P.stt("dve", self.modT[:, s_, 0, :], mT[:, 1, :, s_], 1.0, nT[:, 0, :], ALU.add, ALU.mult, ["mT", "nT"], ["modT"])
                P.cp("dve", self.modT[:, s_, 1, :], mT[:, 0, :, s_], ["mT"], ["modT"])
                P.stt("dve", self.modT[:, s_, 2, :], mT[:, 4, :, s_], 1.0, nT[:, 2, :], ALU.add, ALU.mult, ["mT", "nT"], ["modT"])
                P.cp("dve", self.modT[:, s_, 3, :], mT[:, 3, :, s_], ["mT"], ["modT"])
            nb = self.sb("nb", [128, D], F32)
            P.dma(nb[:], dr["norms"][3 if only_ffn else 1].partition_broadcast(128), writes=["nb"])
            v = 5 if only_ffn else 2
            for s_ in range(2):
                P.dma(self.gv[:, s_, :], dr["modd"][s_, v * D:(v + 1) * D].partition_broadcast(128), reads=["modd", "gv"], writes=["gv"])
            for s_ in range(2):
                P.tt("dve", self.gv[:, s_, :], self.gv[:, s_, :], nb[:], ALU.mult, ["gv", "nb"], ["gv"])
            P.barrier()
        self.st = outer

    def norm_tile_to_hT(self, xt, xkey, hT, hkey, col, sset, v, tagi):
        P = self.P
        junk, ss, xs, pT = self.n_junk, self.n_ss[tagi % 2], self.n_xs[tagi % 2], self.n_pT
        ssk, xsk = "n_ss%d" % (tagi % 2), "n_xs%d" % (tagi % 2)
        P.act(junk[:], xt, AF.Square, [xkey], ["n_junk", ssk], accum_out=ss[:])
        P.ts("dve", ss[:], ss[:], 1.0 / D, EPS, ALU.mult, ALU.add, [ssk], [ssk])
        P.act(ss[:], ss[:], AF.Sqrt, [ssk], [ssk])
        P.op("dve", lambda e: e.reciprocal(ss[:], ss[:]), [ssk], [ssk])
        P.ts("dve", xs[:], xt, ss[:, 0:1], None, ALU.mult, None, [xkey, ssk], [xsk])
        for c in range(8):
            P.tr(pT[:, c, :], xs[:, c * 128:(c + 1) * 128], self.identb[:], [xsk, "identb"], ["n_pT"])
        for c in range(8):
            eng = "dve" if c % 2 == 0 else "pool"
            if eng == "pool":
                eng = "dve"
            P.ts(eng, hT[:, c, col:col + 128], pT[:, c, :], self.modT[:, sset, v, c:c + 1], self.modT[:, sset, v + 1, c:c + 1],
                 ALU.mult, ALU.add, ["n_pT", "modT"], [hkey])

    def alloc_norm(self):
        self.n_junk = self.sb("n_junk", [128, D], BF16)
        self.n_ss = [self.sb("n_ss%d" % i, [128, 1], F32) for i in range(2)]
        self.n_xs = [self.sb("n_xs%d" % i, [128, D], BF16) for i in range(2)]
        self.n_pT = self.ps("n_pT", [128, 8, 128], BF16)

    def src_x(self, col, n):
        if col < CT:
            return self.dr["cin"][col:col + n, :]
        return self.dr["xin"][col - CT:col - CT + n, :]

    def ret_consts(self):
        P, dr = self.P, self.dr
        dl = self.sb("dl", [128, 8], F32)
        P.dma(dl[:], dr["decay"].partition_broadcast(128), writes=["dl"])
        lg = self.sb("lg", [128, 8], F32)
        P.act(lg[:], dl[:], AF.Exp, ["dl"], ["lg"], scale=-1.0)
        P.act(lg[:], lg[:], AF.Ln, ["lg"], ["lg"], bias=1.0)
        P.ts("dve", lg[:], lg[:], -1.0, None, ALU.mult, None, ["lg"], ["lg"])
        self.lg = lg
        lgs = self.sb("lgs", [128, 2, 2], F32)
        for d_ in range(2):
            for p in range(2):
                for hl in range(2):
                    P.cp("dve", lgs[hl * 64:(hl + 1) * 64, d_, p:p + 1], lg[hl * 64:(hl + 1) * 64, d_ * 4 + 2 * p + hl: d_ * 4 + 2 * p + hl + 1],
                         ["lg"], ["lgs"])
        self.lgs = lgs
        zx = self.sb("zx", [128, 2], F32)
        P.dma(zx[:], dr["zexp"], writes=["zx"])
        nCt = self.sb("nCt", [128, self.NCL + 2], F32)
        P.dma(nCt[:], dr["nC"], writes=["nCt"])
        zt = self.sb("zt", [128, 8], F32)
        for d_ in range(2):
            for h in range(4):
                P.act(zt[:, d_ * 4 + h: d_ * 4 + h + 1], zx[:, d_:d_ + 1], AF.Exp, ["zx", "lg"], ["zt"],
                      scale=lg[:, d_ * 4 + h: d_ * 4 + h + 1])
        ones = self.sb("ones64", [128, 64], F32)
        P.op("pool", lambda e: e.memset(ones[:], 1.0), (), ["ones64"])
        self.Z = self.sb("Z", [128, 2, 256], F32)
        for d_ in range(2):
            for h in range(4):
                P.ts("dve", self.Z[:, d_, h * 64:(h + 1) * 64], ones[:], zt[:, d_ * 4 + h: d_ * 4 + h + 1], None, ALU.mult, None,
                     ["ones64", "zt"], ["Z"])
        self.pw = self.sb("pw", [128, 2, 2, self.NCL + 2], F32)
        for d_ in range(2):
            for p in range(2):
                P.act(self.pw[:, d_, p, :], nCt[:], AF.Exp, ["nCt", "lgs"], ["pw"], scale=lgs[:, d_, p:p + 1])

    def emit_U(self, rk_t, rv_t, keys, pU, tag):
        P = self.P
        kz = self.u_kz
        for d_ in range(2):
            P.tt("dve", kz[:, d_, :], rk_t, self.Z[:, d_, :], ALU.mult, keys + ["Z"], ["u_kz"])
        for d_ in range(2):
            for p in range(2):
                P.mm(pU[:, d_ * 2 + p, :], kz[:, d_, p * 128:(p + 1) * 128], rv_t[:, p * 128:(p + 1) * 128], True, True,
                     ["u_kz"] + keys, [tag])

    def phase_A(self):
        P, dr = self.P, self.dr
        Tc = self.Tc
        self.alloc_norm()
        self.ret_consts()
        wfm = self.sb("wfm", [128, 8, 1792], BF16)
        wtm = self.sb("wtm", [128, 8, 1152], BF16)
        for c in range(8):
            P.dma(wfm[:, c, :], dr["w_fm"][c * 128:(c + 1) * 128, :], writes=["wfm"], reads=["wfm"], eng="pool")
            P.dma(wtm[:, c, :], dr["w_tm"][c * 128:(c + 1) * 128, :], writes=["wtm"], reads=["wtm"], eng="pool")
        gains = self.sb("gains", [128, 4], F32)
        P.dma(gains[:], dr["gains"], writes=["gains"])
        bonesf = self.sb("bonesf", [128, 128], F32)
        bones = self.sb("bones", [128, 128], BF16)
        P.dma(bonesf[:], dr["bones"], writes=["bonesf"])
        P.cp("dve", bones[:], bonesf[:], ["bonesf"], ["bones"])
        cosT = self.sb("cosT", [128, Tc], F32)
        sinT = self.sb("sinT", [128, Tc], F32)
        P.dma(cosT[:], dr["cosT"], writes=["cosT"])
        P.dma(sinT[:], dr["sinT"], writes=["sinT"])
        self.u_kz = self.sb("u_kz", [128, 2, 256], BF16)
        Lacc = self.sb("Lacc", [128, 2, 2, 64], F32)
        P.op("pool", lambda e: e.memset(Lacc[:], 0.0), (), ["Lacc"])

        xt = [self.sb("xt%d" % i, [128, D], F32) for i in range(2)]
        hT = [self.sb("hT%d" % i, [128, 8, 512], BF16) for i in range(2)]
        pa = self.ps("pa", [128, 512], F32)
        pb = self.ps("pb", [128, 512], F32)
        pss = self.ps("pss", [128, 512], F32)
        ptm = [self.ps("ptm%d" % i, [128, 512], F32) for i in range(2)]
        pU = self.ps("pU", [128, 4, 128], F32)
        sq = self.sb("sq", [128, 512], BF16)
        rr = self.sb("rr", [128, 512], F32)
        t1 = self.sb("t1", [128, 512], F32)
        t2 = self.sb("t2", [128, 512], F32)
        qo = [self.sb("qo%d" % i, [128, 4, 512], BF16) for i in range(2)]
        ko = [self.sb("ko%d" % i, [128, 512], BF16) for i in range(2)]
        rqo = [self.sb("rqo%d" % i, [128, 2, 512], BF16) for i in range(2)]
        rko = [self.sb("rko%d" % i, [128, 2, 512], BF16) for i in range(2)]
        va = [self.sb("va%d" % i, [128, 132], BF16) for i in range(2)]
        tko = [self.sb("tko%d" % i, [128, 256], BF16) for i in range(2)]
        tvo = [self.sb("tvo%d" % i, [128, 256], BF16) for i in range(2)]
        tgo = [self.sb("tgo%d" % i, [128, 256], BF16) for i in range(2)]
        tpo = [self.sb("tpo%d" % i, [128, 256], BF16) for i in range(2)]
        for i in range(2):
            P.op("pool", lambda e, i=i: e.memset(va[i][:], 1.0), (), ["va%d" % i])

        ti = 0
        if self.kstop == 2:
            return
        for bi, (col0, ntok) in enumerate(self.blocks):
            nt = ntok // 128
            hb = hT[bi % 2]
            hk = "hT%d" % (bi % 2)
            sset = 1 if col0 < CT else 0
            if self.kstop == 3 and bi > 0:
                return
            for t in range(nt):
                x_ = xt[ti % 2]
                xk = "xt%d" % (ti % 2)
                P.dma(x_[:], self.src_x(col0 + t * 128, 128), writes=[xk])
                self.norm_tile_to_hT(x_[:], xk, hb, hk, t * 128, sset, 0, ti)
                ti += 1
            if self.kstop == 4:
                return
            qb_, qk_ = qo[bi % 2], "qo%d" % (bi % 2)
            kb_, kk_ = ko[bi % 2], "ko%d" % (bi % 2)

            def fm(ps_, m):
                for c in range(8):
                    P.mm(ps_[:, 0:ntok], wfm[:, c, m * 128:(m + 1) * 128], hb[:, c, 0:ntok], c == 0, c == 7,
                         ["wfm", hk], [ps_.name if hasattr(ps_, "name") else "px"])

            for g in range(5):
                ma, mb = (g, g + 4) if g < 4 else (8, 9)
                gcol = 0 if g < 4 else 2
                for c in range(8):
                    P.mm(pa[:, 0:ntok], wfm[:, c, ma * 128:(ma + 1) * 128], hb[:, c, 0:ntok], c == 0, c == 7, ["wfm", hk], ["pa"])
                for c in range(8):
                    P.mm(pb[:, 0:ntok], wfm[:, c, mb * 128:(mb + 1) * 128], hb[:, c, 0:ntok], c == 0, c == 7, ["wfm", hk], ["pb"])
                P.act(sq[:, 0:ntok], pa[:, 0:ntok], AF.Square, ["pa"], ["sq"])
                P.mm(pss[:, 0:ntok], bones[:], sq[:, 0:ntok], True, True, ["bones", "sq"], ["pss"])
                P.act(rr[:, 0:ntok], pss[:, 0:ntok], AF.Sqrt, ["pss"], ["rr"], scale=1.0 / 64, bias=EPS)
                P.op("dve", lambda e, n=ntok: e.reciprocal(rr[:, 0:n], rr[:, 0:n]), ["rr"], ["rr"])
                P.stt("dve", t1[:, 0:ntok], pa[:, 0:ntok], gains[:, gcol:gcol + 1], cosT[:, col0:col0 + ntok], ALU.mult, ALU.mult,
                      ["pa", "gains", "cosT"], ["t1"])
                P.stt("dve", t2[:, 0:ntok], pb[:, 0:ntok], gains[:, gcol + 1:gcol + 2], sinT[:, col0:col0 + ntok], ALU.mult, ALU.mult,
                      ["pb", "gains", "sinT"], ["t2"])
                P.tt("pool", t1[:, 0:ntok], t1[:, 0:ntok], t2[:, 0:ntok], ALU.add, ["t1", "t2"], ["t1"])
                if g < 4:
                    P.tt("pool", qb_[:, g, 0:ntok], t1[:, 0:ntok], rr[:, 0:ntok], ALU.mult, ["t1", "rr"], [qk_])
                else:
                    P.tt("pool", kb_[:, 0:ntok], t1[:, 0:ntok], rr[:, 0:ntok], ALU.mult, ["t1", "rr"], [kk_])
            if self.kstop == 5:
                return
            P.dma(dr["QT"][:, :, col0:col0 + ntok], qb_[:, :, 0:ntok], reads=[qk_])
            P.dma(dr["KT"][:, col0:col0 + ntok], kb_[:, 0:ntok], reads=[kk_])
            rq_, rqk = rqo[bi % 2], "rqo%d" % (bi % 2)
            rk_, rkk = rko[bi % 2], "rko%d" % (bi % 2)
            for p in range(2):
                for c in range(8):
                    P.mm(pa[:, 0:ntok], wfm[:, c, (10 + p) * 128:(11 + p) * 128], hb[:, c, 0:ntok], c == 0, c == 7, ["wfm", hk], ["pa"])
                P.cp("act", rq_[:, p, 0:ntok], pa[:, 0:ntok], ["pa"], [rqk])
                for c in range(8):
                    P.mm(pb[:, 0:ntok], wfm[:, c, (12 + p) * 128:(13 + p) * 128], hb[:, c, 0:ntok], c == 0, c == 7, ["wfm", hk], ["pb"])
                P.ts("dve", rk_[:, p, 0:ntok], pb[:, 0:ntok], 0.125, None, ALU.mult, None, ["pb"], [rkk])
            P.dma(dr["rqT"][:, :, col0:col0 + ntok], rq_[:, :, 0:ntok], reads=[rqk])
            P.dma(dr["rkT"][:, :, col0:col0 + ntok], rk_[:, :, 0:ntok], reads=[rkk])
            if self.kstop == 6:
                return
            for t in range(nt):
                tcol = col0 + t * 128
                s = (bi * 4 + t) % 2
                p0 = ptm[0]
                for c in range(8):
                    P.mm(p0[:, 0:384], hb[:, c, t * 128:(t + 1) * 128], wtm[:, c, 0:384], c == 0, c == 7, [hk, "wtm"], ["ptm0"])
                if self.kstop == 9:
                    continue
                P.cp("dve", va[s][:, 0:64], p0[:, 0:64], ["ptm0"], ["va%d" % s])
                P.cp("dve", va[s][:, 66:130], p0[:, 64:128], ["ptm0"], ["va%d" % s])
                P.ts("dve", tko[s][:], p0[:, 128:384], 0.125, None, ALU.mult, None, ["ptm0"], ["tko%d" % s])
                if self.kstop == 8:
                    continue
                p1 = ptm[1]
                for c in range(8):
                    P.mm(p1[:, 0:512], hb[:, c, t * 128:(t + 1) * 128], wtm[:, c, 384:896], c == 0, c == 7, [hk, "wtm"], ["ptm1"])
                P.cp("dve", tvo[s][:], p1[:, 0:256], ["ptm1"], ["tvo%d" % s])
                P.cp("dve", tgo[s][:], p1[:, 256:512], ["ptm1"], ["tgo%d" % s])
                for c in range(8):
                    P.mm(p0[:, 0:256], hb[:, c, t * 128:(t + 1) * 128], wtm[:, c, 896:1152], c == 0, c == 7, [hk, "wtm"], ["ptm0"])
                P.cp("dve", tpo[s][:], p0[:, 0:256], ["ptm0"], ["tpo%d" % s])
                if self.kstop == 7:
                    continue
                P.dma(dr["VA"][tcol:tcol + 128, :], va[s][:], reads=["va%d" % s], eng="sp")
                P.dma(dr["rk"][tcol:tcol + 128, :], tko[s][:], reads=["tko%d" % s], eng="sp")
                P.dma(dr["rv"][tcol:tcol + 128, :], tvo[s][:], reads=["tvo%d" % s], eng="sp")
                P.dma(dr["rg"][tcol:tcol + 128, :], tgo[s][:], reads=["tgo%d" % s], eng="sp")
                P.dma(dr["pp"][tcol:tcol + 128, :], tpo[s][:], reads=["tpo%d" % s], eng="sp")
                if col0 >= CT and self.kstop != 10:
                    n = (tcol - CT) // 128
                    if self.kstop == 12:
                        continue
                    self.emit_U(tko[s][:], tvo[s][:], ["tko%d" % s, "tvo%d" % s], pU, "pU")
                    for d_ in range(2):
                        pwi = (self.NCL - 1 - n) if d_ == 0 else n
                        for p in range(2):
                            for hl in range(2):
                                r = slice(hl * 64, (hl + 1) * 64)
                                P.stt("dve", Lacc[r, d_, p, :], pU[r, d_ * 2 + p, hl * 64:(hl + 1) * 64],
                                      self.pw[r, d_, p, pwi:pwi + 1], Lacc[r, d_, p, :], ALU.mult, ALU.add,
                                      ["pU", "pw", "Lacc"], ["Lacc"])
        for d_ in range(2):
            P.dma(dr["Lout"][d_ * 128:(d_ + 1) * 128, :], Lacc[:, d_, :, :].rearrange("p a e -> p (a e)"), reads=["Lacc"])

    def phase_B1(self):
        P, dr = self.P, self.dr
        T, Tl, Tc, NCL, NKT = self.T, self.Tl, self.Tc, self.NCL, self.NKT
        self.ret_consts()
        lg, lgs = self.lg, self.lgs
        wo = self.sb("wo", [128, 8, D], BF16)
        for c in range(8):
            P.dma(wo[:, c, :], dr["w_out"][c * 128:(c + 1) * 128, :], writes=["wo"], reads=["wo"], eng="pool")
        KTa = self.sb("KTa", [128, CT + T], BF16)
        VAa = self.sb("VAa", [128, NKT, 132], BF16)
        P.dma(KTa[:, 0:CT], dr["KT"][:, 0:CT], writes=["KTa"])
        for r in range(4):
            P.dma(KTa[:, CT + r * Tl: CT + (r + 1) * Tl], dr["KTg"][r * 128:(r + 1) * 128, :], reads=["KTa"], writes=["KTa"])
        P.dma(VAa[:, 0:2, :], dr["VA"][0:CT, :].rearrange("(k p) c -> p k c", p=128), writes=["VAa"])
        for k0 in range(0, 4 * NCL, 8):
            k1 = min(k0 + 8, 4 * NCL)
            P.dma(VAa[:, 2 + k0: 2 + k1, :], dr["VAg"][k0 * 128:k1 * 128, :].rearrange("(k p) c -> p k c", p=128),
                  reads=["VAa"], writes=["VAa"])
        dm = self.sb("dm", [128, 4, 128], F32)
        P.dma(dm[:], dr["dmask"], writes=["dm"])
        DT = self.sb("DT", [128, 4, 128], F32)
        dtmp = self.sb("dtmp", [128, 128], F32)
        for h in range(4):
            P.act(DT[:, h, :], dm[:, 0, :], AF.Exp, ["dm", "lg"], ["DT"], scale=lg[:, h:h + 1])
            P.tt("dve", DT[:, h, :], DT[:, h, :], dm[:, 1, :], ALU.mult, ["DT", "dm"], ["DT"])
            P.act(dtmp[:], dm[:, 2, :], AF.Exp, ["dm", "lg"], ["dtmp"], scale=lg[:, 4 + h:5 + h])
            P.tt("dve", dtmp[:], dtmp[:], dm[:, 3, :], ALU.mult, ["dtmp", "dm"], ["dtmp"])
            P.tt("dve", DT[:, h, :], DT[:, h, :], dtmp[:], ALU.add, ["DT", "dtmp"], ["DT"])
        xe = self.sb("xe", [128, 2, 128], F32)
        P.dma(xe[:], dr["xiexp"], writes=["xe"])
        XiT = self.sb("XiT", [128, 2, 2, 128], BF16)
        for d_ in range(2):
            for p in range(2):
                P.act(XiT[:, d_, p, :], xe[:, d_, :], AF.Exp, ["xe", "lgs"], ["XiT"], scale=lgs[:, d_, p:p + 1])
        sel = self.sb("sel", [128, 4], F32)
        P.dma(sel[:], dr["sel"], writes=["sel"])
        def ldc(name, src, shape, eng="sp"):
            b = self.sb(name, shape, BF16)
            P.dma(b[:], src, writes=[name], eng=eng)
            return b
        Amain = ldc("Amain", dr["Amain"], [128, 12, 128])
        Anb = ldc("Anb", dr["Anb"], [128, 8, 128])
        Actx = ldc("Actx", dr["Actx"], [128, 8, 128])
        Ahalo = ldc("Ahalo", dr["Ahalo"], [64, 8, 128])
        pw_ = ldc("poolw", dr["pool_w"], [64, 4, 64], eng="pool")
        pscale = self.sb("pscale", [128, 2], F32)
        P.dma(pscale[:], dr["pool_s"], writes=["pscale"])
        Hg = self.sb("Hg", [64, 256], BF16)
        P.dma(Hg[:], dr["Hg"], writes=["Hg"])

        bS = [self.ps("bS%d" % i, [128, 512], F32) for i in range(2)]
        bO = self.ps("bO", [128, 512], F32)
        bR = self.ps("bR", [128, 512], F32)
        bY = self.ps("bY", [128, 512], F32)
        bU = self.ps("bU", [128, 4, 128], F32)
        bM = self.ps("bM", [128, 512], F32)
        self.n_pT = self.ps("n_pT", [128, 8, 128], BF16)
        pT = self.n_pT

        NCc = NCL + 2
        Sb = self.sb("Sb", [128, NCc, 2, 2, 64], BF16)
        outer = self.st
        tmpstack = contextlib.ExitStack()
        self.st = tmpstack
        self.u_kz = self.sb("u_kz", [128, 2, 256], BF16)
        rk_all = self.sb("rk_all", [128, NCc, 256], BF16)
        rv_all = self.sb("rv_all", [128, NCc, 256], BF16)
        for k0 in range(0, NCc, 8):
            k1 = min(k0 + 8, NCc)
            P.dma(rk_all[:, k0:k1, :], dr["rk"][k0 * 128:k1 * 128, :].rearrange("(k p) c -> p k c", p=128), reads=["rk_all"], writes=["rk_all"])
            P.dma(rv_all[:, k0:k1, :], dr["rv"][k0 * 128:k1 * 128, :].rearrange("(k p) c -> p k c", p=128), reads=["rv_all"], writes=["rv_all"])
        Uall = self.sb("Uall", [128, NCc, 2, 2, 64], F32)
        for n in range(NCc):
            self.emit_U(rk_all[:, n, :], rv_all[:, n, :], ["rk_all", "rv_all"], bU, "bU")
            for d_ in range(2):
                for p in range(2):
                    for hl in range(2):
                        r = slice(hl * 64, (hl + 1) * 64)
                        eng = "dve"
                        P.cp(eng, Uall[r, n, d_, p, :], bU[r, d_ * 2 + p, hl * 64:(hl + 1) * 64], ["bU"], [("Uall", n)])
        gC = self.pw[:, :, :, 1]
        sctx = self.sb("sctx", [128, 2, 2, 64], F32)
        for p in range(2):
            P.stt("dve", sctx[:, 0, p, :], Uall[:, 0, 0, p, :], self.pw[:, 0, p, 1:2], Uall[:, 1, 0, p, :], ALU.mult, ALU.add,
                  [("Uall", 0), ("Uall", 1), "pw"], ["sctx"])
            P.stt("dve", sctx[:, 1, p, :], Uall[:, 1, 1, p, :], self.pw[:, 1, p, 1:2], Uall[:, 0, 1, p, :], ALU.mult, ALU.add,
                  [("Uall", 0), ("Uall", 1), "pw"], ["sctx"])
        Lg = self.sb("Lg", [128, 4, 2, 128], F32)
        P.dma(Lg[:], dr["Lg"].rearrange("(r d p) c -> p r d c", r=4, d=2, p=128), writes=["Lg"])
        G = self.sb("G", [128, 2, 4, 2, 64], F32)
        for p in range(2):
            P.cp("dve", G[:, 0, 0, p, :], sctx[:, 0, p, :], ["sctx"], ["G"])
            for i in range(1, 4):
                P.stt("dve", G[:, 0, i, p, :], G[:, 0, i - 1, p, :], self.pw[:, 0, p, NCL:NCL + 1], Lg[:, i - 1, 0, p * 64:(p + 1) * 64],
                      ALU.mult, ALU.add, ["G", "pw", "Lg"], ["G"])
            P.cp("dve", G[:, 1, 3, p, :], sctx[:, 1, p, :], ["sctx"], ["G"])
            for i in (2, 1, 0):
                P.stt("dve", G[:, 1, i, p, :], G[:, 1, i + 1, p, :], self.pw[:, 1, p, NCL:NCL + 1], Lg[:, i + 1, 1, p * 64:(p + 1) * 64],
                      ALU.mult, ALU.add, ["G", "pw", "Lg"], ["G"])
        Gown = self.sb("Gown", [128, 2, 2, 64], F32)
        for d_ in range(2):
            P.ts("dve", Gown[:, d_, :, :], G[:, d_, 0, :, :], sel[:, 0:1], None, ALU.mult, None, ["G", "sel"], ["Gown"])
            for i in range(1, 4):
                P.stt("dve", Gown[:, d_, :, :], G[:, d_, i, :, :], sel[:, i:i + 1], Gown[:, d_, :, :], ALU.mult, ALU.add,
                      ["G", "sel", "Gown"], ["Gown"])
        run = self.sb("run", [128, 2, 2, 64], F32)
        P.op("pool", lambda e: e.memset(Sb[:, 0:2, :, :, :], 0.0), (), [("Sb", 0), ("Sb", 1)])
        P.cp("dve", Sb[:, 1, 0, :, :], Uall[:, 0, 0, :, :], [("Uall", 0), ("Sb", 1)], [("Sb", 1)])
        P.cp("dve", Sb[:, 0, 1, :, :], Uall[:, 1, 1, :, :], [("Uall", 1), ("Sb", 0)], [("Sb", 0)])
        P.cp("dve", run[:, 0, :, :], Gown[:, 0, :, :], ["Gown"], ["run"])
        for n in range(NCL):
            P.cp("pool", Sb[:, 2 + n, 0, :, :], run[:, 0, :, :], ["run"], [("Sb", 2 + n)])
            if n < NCL - 1:
                for p in range(2):
                    P.stt("dve", run[:, 0, p, :], run[:, 0, p, :], self.pw[:, 0, p, 1:2], Uall[:, 2 + n, 0, p, :], ALU.mult, ALU.add,
                          ["run", "pw", ("Uall", 2 + n)], ["run"])
        P.cp("dve", run[:, 1, :, :], Gown[:, 1, :, :], ["Gown"], ["run"])
        for n in range(NCL - 1, -1, -1):
            P.cp("pool", Sb[:, 2 + n, 1, :, :], run[:, 1, :, :], ["run", ("Sb", 2 + n)], [("Sb", 2 + n)])
            if n > 0:
                for p in range(2):
                    P.stt("dve", run[:, 1, p, :], run[:, 1, p, :], self.pw[:, 1, p, 1:2], Uall[:, 2 + n, 1, p, :], ALU.mult, ALU.add,
                          ["run", "pw", ("Uall", 2 + n)], ["run"])

        P.barrier()
        tmpstack.close()
        self.st = outer

        QTb = [self.sb("QTb%d" % i, [128, 4, 512], BF16) for i in range(2)]
        rqb = [self.sb("rqb%d" % i, [128, 2, 512], BF16) for i in range(2)]
        rkb = [self.sb("rkb%d" % i, [128, 2, 512], BF16) for i in range(2)]
        rgb = [self.sb("rgb%d" % i, [128, 4, 256], BF16) for i in range(2)]
        rvb = [self.sb("rvb%d" % i, [128, 4, 256], BF16) for i in range(2)]
        ppb = [self.sb("ppb%d" % i, [128, 6, 256], BF16) for i in range(2)]
        PT = [self.sb("PT%d" % i, [128, 512], BF16) for i in range(3)]
        zeros = self.sb("zeros", [128, 260], BF16)
        P.op("pool", lambda e: e.memset(zeros[:], 0.0), (), ["zeros"])
        mix = [self.sb("mix%d" % i, [128, D], BF16) for i in range(2)]
        mixT = [self.sb("mixT%d" % i, [128, 8, 128], BF16) for i in range(2)]
        rden = self.sb("rden", [128, 4], F32)
        oT = self.sb("oT", [128, 512], F32)
        sT = self.sb("sT", [128, 4, 128], BF16)
        qx = self.sb("qx", [128, 2, 2, 128], BF16)
        osq = self.sb("osq", [128, 256], F32)
        ob = self.sb("ob", [128, 256], F32)
        st1 = self.sb("st1", [128, 4], F32)
        st2 = self.sb("st2", [128, 4], F32)
        mean = self.sb("mean", [128, 4], F32)
        yn = self.sb("yn", [128, 256], F32)
        sg = self.sb("sg", [128, 256], F32)
        mxd = self.sb("mxd", [64, 4, 128], BF16)
        xt = [self.sb("xt%d" % i, [128, D], F32) for i in range(2)]
        mjunk = self.sb("mjunk", [128, D], F32)
        mss = self.sb("mss", [128, 1], F32)
        mtmp = self.sb("mtmp", [128, D], F32)
        xo = [self.sb("xo%d" % i, [128, D], F32) for i in range(2)]

        ptc = [0]
        gti = 0
        if self.kb == 1:
            return
        for bi, (col0, ntok) in enumerate(self.blocks):
            isctx = col0 < CT
            if isctx and not self.need_ctx:
                continue
            nt = ntok // 128
            sset = 1 if isctx else 0
            s2 = bi % 2
            Qb, Qk = QTb[s2], "QTb%d" % s2
            P.dma(Qb[:, :, 0:ntok], dr["QT"][:, :, col0:col0 + ntok], writes=[Qk])
            P.dma(rqb[s2][:, :, 0:ntok], dr["rqT"][:, :, col0:col0 + ntok], writes=["rqb%d" % s2])
            P.dma(rkb[s2][:, :, 0:ntok], dr["rkT"][:, :, col0:col0 + ntok], writes=["rkb%d" % s2])
            P.dma(rgb[s2][:, 0:nt, :], dr["rg"][col0:col0 + ntok, :].rearrange("(k p) c -> p k c", p=128), writes=["rgb%d" % s2])
            P.dma(rvb[s2][:, 0:nt, :], dr["rv"][col0:col0 + ntok, :].rearrange("(k p) c -> p k c", p=128), writes=["rvb%d" % s2])
            lo_t = col0 - 128 if (col0 > CT or (isctx and col0 > 0)) else col0
            hi_t = col0 + ntok + 128
            if isctx:
                hi_t = min(hi_t, CT)
            else:
                hi_t = min(hi_t, Tc)
            k0 = (lo_t - (col0 - 128)) // 128
            nk = (hi_t - lo_t) // 128
            P.dma(ppb[s2][:, k0:k0 + nk, :], dr["pp"][lo_t:hi_t, :].rearrange("(k p) c -> p k c", p=128), writes=["ppb%d" % s2])
            kts = list(range(2)) if isctx else list(range(NKT))
            for t in range(nt):
                tcol = col0 + t * 128
                mx_, mxk = mix[gti % 2], "mix%d" % (gti % 2)
                for kvh in range(2):
                    r = slice(kvh * 64, (kvh + 1) * 64)
                    def issue_S(ki_):
                        kt_ = kts[ki_]
                        S_ = bS[ptc[0] % 2]
                        Sk = "bS%d" % (ptc[0] % 2)
                        Pt_, Pk_ = PT[ptc[0] % 3], "PT%d" % (ptc[0] % 3)
                        ptc[0] += 1
                        P.mm(S_[:, :].rearrange("p (g q) -> p g q", g=4), KTa[r, kt_ * 128:(kt_ + 1) * 128],
                             Qb[r, :, t * 128:(t + 1) * 128], True, True, ["KTa", Qk], [Sk])
                        P.act(Pt_[:], S_[:], AF.Exp, [Sk], [Pk_], scale=0.125)
                        return Pt_, Pk_
                    cur = issue_S(0)
                    for ki, kt in enumerate(kts):
                        nxt = issue_S(ki + 1) if ki + 1 < len(kts) else None
                        Pt, Pk = cur
                        P.mm(bO[0:65, :], VAa[:, kt, kvh * 66:kvh * 66 + 65], Pt[:, :], ki == 0, ki == len(kts) - 1,
                             [Pk, "VAa"], ["bO"])
                        cur = nxt
                    P.cp("dve", oT[0:65, :], bO[0:65, :], ["bO"], ["oT"])
                    for g in range(4):
                        P.tr(bO[:, g * 65:(g + 1) * 65], oT[0:65, g * 128:(g + 1) * 128], self.identf[0:65, 0:65],
                             ["oT", "identf"], ["bO"])
                    for g in range(4):
                        P.op("dve", lambda e, g=g: e.reciprocal(rden[:, g:g + 1], bO[:, g * 65 + 64: g * 65 + 65]), ["bO"], ["rden"])
                    for g in range(4):
                        h = kvh * 4 + g
                        P.ts("dve", mx_[:, h * 64:(h + 1) * 64], bO[:, g * 65: g * 65 + 64], rden[:, g:g + 1], None, ALU.mult, None,
                             ["bO", "rden"], [mxk])
                if self.kb == 2:
                    continue
                n = tcol // 128
                for d_ in range(2):
                    for p in range(2):
                        P.tt("pool", qx[:, d_, p, :], rqb[s2][:, p, t * 128:(t + 1) * 128], XiT[:, d_, p, :], ALU.mult,
                             ["rqb%d" % s2, "XiT"], ["qx"])
                bUf = bU[:, :, :].rearrange("p a b -> p (a b)")
                for h in range(4):
                    p, hl = h // 2, h % 2
                    r = slice(hl * 64, (hl + 1) * 64)
                    bank, bkey = (bR, "bR") if hl == 0 else (bUf, "bU")
                    P.mm(bank[:, p * 128:(p + 1) * 128], rkb[s2][r, p, t * 128:(t + 1) * 128], rqb[s2][r, p, t * 128:(t + 1) * 128],
                         True, True, ["rkb%d" % s2, "rqb%d" % s2], [bkey])
                for h in range(4):
                    p, hl = h // 2, h % 2
                    bank, bkey = (bR, "bR") if hl == 0 else (bUf, "bU")
                    P.tt("dve", sT[:, h, :], bank[:, p * 128:(p + 1) * 128], DT[:, h, :], ALU.mult, [bkey, "DT"], ["sT"])
                for h in range(4):
                    p, hl = h // 2, h % 2
                    r = slice(hl * 64, (hl + 1) * 64)
                    if hl == 0:
                        oreg, okey = bY[:, p * 64:(p + 1) * 64], "bY"
                    else:
                        oreg, okey = bO[:, 320 + p * 64: 320 + (p + 1) * 64], "bO"
                    P.mm(oreg, sT[:, h, :], rvb[s2][:, t, h * 64:(h + 1) * 64], True, False,
                         ["sT", "rvb%d" % s2], [okey], skip_group_check=True)
                    P.mm(oreg, qx[r, 0, p, :], Sb[r, n, 0, p, :], False, False,
                         ["qx", ("Sb", n)], [okey], skip_group_check=True)
                    P.mm(oreg, qx[r, 1, p, :], Sb[r, n, 1, p, :], False, True,
                         ["qx", ("Sb", n)], [okey], skip_group_check=True)
                ob4 = ob[:].rearrange("p (a b e) -> p a b e", a=2, b=2)
                P.cp("dve", ob4[:, :, 0, :], bY[:, 0:128].rearrange("p (a e) -> p a e", a=2), ["bY"], ["ob"])
                P.cp("dve", ob4[:, :, 1, :], bO[:, 320:448].rearrange("p (a e) -> p a e", a=2), ["bO"], ["ob"])
                P.op("dve", lambda e: e.reduce_sum(st1[:], ob[:].rearrange("p (h e) -> p h e", h=4), AX.X), ["ob"], ["st1"])
                P.act(osq[:], ob[:], AF.Square, ["ob"], ["osq"])
                P.op("dve", lambda e: e.reduce_sum(st2[:], osq[:].rearrange("p (h e) -> p h e", h=4), AX.X), ["osq"], ["st2"])
                P.ts("dve", mean[:], st1[:], 1.0 / 64, None, ALU.mult, None, ["st1"], ["mean"])
                P.tt("dve", st1[:], mean[:], mean[:], ALU.mult, ["mean"], ["st1"])
                P.stt("dve", st2[:], st2[:], 1.0 / 64, st1[:], ALU.mult, ALU.subtract, ["st2", "st1"], ["st2"])
                P.act(st2[:], st2[:], AF.Sqrt, ["st2"], ["st2"], bias=EPS)
                P.op("dve", lambda e: e.reciprocal(st2[:], st2[:]), ["st2"], ["st2"])
                for h in range(4):
                    P.ts("dve", yn[:, h * 64:(h + 1) * 64], ob[:, h * 64:(h + 1) * 64], mean[:, h:h + 1], st2[:, h:h + 1],
                         ALU.subtract, ALU.mult, ["ob", "mean", "st2"], ["yn"])
                P.act(sg[:], rgb[s2][:, t, :], AF.Silu, ["rgb%d" % s2], ["sg"])
                P.tt("pool", mx_[:, 512:768], yn[:], sg[:], ALU.mult, ["yn", "sg"], [mxk])
                if self.kb == 3:
                    continue
                mT_, mTk = mixT[gti % 2], "mixT%d" % (gti % 2)
                for c in range(6):
                    P.tr(pT[:, c, :], mx_[:, c * 128:(c + 1) * 128], self.identb[:], [mxk, "identb"], ["n_pT"])
                P.cp("act", mT_[:, 0:6, :], pT[:, 0:6, :], ["n_pT"], [mTk])
                first = (not isctx and tcol == CT) or (isctx and tcol == 0)
                last = (not isctx and tcol == Tc - 128) or (isctx and tcol == CT - 128)
                for g in range(4):
                    gs = slice(g * 64, (g + 1) * 64)
                    if isctx:
                        Am = Actx[:, (0 if first else 4) + g, :]
                    else:
                        Am = Amain[:, (0 if first else (8 if last else 4)) + g, :]
                    ops_ = [(ppb[s2][:, t + 1, gs], Am)]
                    if not first:
                        ops_.append((ppb[s2][:, t, gs], Anb[:, g, :]))
                    elif not isctx:
                        ops_.append((Hg[:, gs], Ahalo[:, g, :]))
                    if not last:
                        ops_.append((ppb[s2][:, t + 2, gs], Anb[:, 4 + g, :]))
                    elif not isctx:
                        ops_.append((Hg[:, gs], Ahalo[:, 4 + g, :]))
                    for oi, (l_, r_) in enumerate(ops_):
                        P.mm(bM[0:64, g * 128:(g + 1) * 128], l_, r_, oi == 0, oi == len(ops_) - 1,
                             ["ppb%d" % s2, "Amain", "Anb", "Actx", "Ahalo", "Hg"], ["bM"], skip_group_check=True)
                P.cp("act", mxd[:].rearrange("p g t -> p (g t)"), bM[0:64, :], ["bM"], ["mxd"])
                for g in range(4):
                    P.mm(bY[(g % 2) * 64:(g % 2 + 1) * 64, 256 + (g // 2) * 128: 256 + (g // 2 + 1) * 128], pw_[:, g, :], mxd[:, g, :],
                         True, True, ["poolw", "mxd"], ["bYp"], skip_group_check=True)
                for c in range(2):
                    P.ts("dve", mT_[:, 6 + c, :], bY[:, 256 + c * 128: 256 + (c + 1) * 128], pscale[:, c:c + 1], None, ALU.mult, None,
                         ["bYp", "pscale"], [mTk])
                if self.kb == 4:
                    continue
                for hf in range(2):
                    for c in range(8):
                        P.mm(bS[hf][:, :], mT_[:, c, :], wo[:, c, hf * 512:(hf + 1) * 512], c == 0, c == 7, [mTk, "wo"], ["bS%d" % hf])
                x_, xk = xt[gti % 2], "xt%d" % (gti % 2)
                P.dma(x_[:], self.src_x(tcol, 128), writes=[xk])
                P.act(mjunk[:, 0:512], bS[0][:, :], AF.Square, ["bS0"], ["mjunk", "mss"], accum_out=mss[:])
                mss2 = st1[:, 0:1]
                P.act(mjunk[:, 512:1024], bS[1][:, :], AF.Square, ["bS1"], ["mjunk", "st1"], accum_out=mss2)
                P.tt("dve", mss[:], mss[:], mss2, ALU.add, ["mss", "st1"], ["mss"])
                P.ts("dve", mss[:], mss[:], 1.0 / D, EPS, ALU.mult, ALU.add, ["mss"], ["mss"])
                P.act(mss[:], mss[:], AF.Sqrt, ["mss"], ["mss"])
                P.op("dve", lambda e: e.reciprocal(mss[:], mss[:]), ["mss"], ["mss"])
                for hf in range(2):
                    P.stt("dve", mtmp[:, hf * 512:(hf + 1) * 512], bS[hf][:, :], mss[:, 0:1], self.gv[:, sset, hf * 512:(hf + 1) * 512],
                          ALU.mult, ALU.mult, ["bS%d" % hf, "mss", "gv"], ["mtmp"])
                xo_, xok = xo[gti % 2], "xo%d" % (gti % 2)
                P.tt("pool", xo_[:], mtmp[:], x_[:], ALU.add, ["mtmp", xk], [xok])
                dst = dr["cout"][tcol:tcol + 128, :] if isctx else dr["xout"][tcol - CT: tcol - CT + 128, :]
                P.dma(dst, xo_[:], reads=[xok], eng="pool")
                gti += 1

    def phase_B2(self):
        P, dr = self.P, self.dr
        E = self.n_exp
        self.alloc_norm()
        blocks = [b for b in self.blocks if not (b[0] < CT and not self.need_ctx)]
        xt = [self.sb("xt%d" % i, [128, D], F32) for i in range(2)]
        hT = [self.sb("hT0", [128, 8, 512], BF16)] * 2
        w1 = self.sb("w1", [128, 8, DFF], BF16)
        w3 = self.sb("w3", [128, 8, DFF], BF16)
        w2 = self.sb("w2", [128, NF, D], BF16)
        gT = self.sb("gT", [128, NF, 512], BF16)
        sa = self.sb("sa", [128, 512], F32)
        pa = [self.ps("pa%d" % i, [128, 512], F32) for i in range(2)]
        pb = [self.ps("pb%d" % i, [128, 512], F32) for i in range(2)]
        py = [self.ps("py%d" % i, [128, 512], F32) for i in range(2)]
        pr = self.ps("pr", [128, 8], F32)
        ntl = self.Tc // 128
        gates = self.sb("gates", [128, ntl, 8], F32)
        if E > 1:
            rtf = self.sb("rtf", [128, 8, 8], F32)
            rtb = self.sb("rtb", [128, 8, 8], BF16)
            P.dma(rtf[:], dr["router"].rearrange("(c p) e -> p c e", p=128), writes=["rtf"])
            P.cp("dve", rtb[:], rtf[:], ["rtf"], ["rtb"])
            lgt = self.sb("lgt", [128, 8], F32)
            m8 = self.sb("m8", [128, 8], F32)
            msk = self.sb("msk", [128, 8], F32)
            ex = self.sb("ex", [128, 8], F32)
            den = self.sb("den", [128, 1], F32)
        ti = 0
        for bi, (col0, ntok) in enumerate(blocks):
            nt = ntok // 128
            hb, hk = hT[0], "hT0"
            sset = 1 if col0 < CT else 0
            for t in range(nt):
                x_, xk = xt[ti % 2], "xt%d" % (ti % 2)
                src = dr["cout"][col0 + t * 128: col0 + (t + 1) * 128, :] if col0 < CT else \
                    dr["xout"][col0 - CT + t * 128: col0 - CT + (t + 1) * 128, :]
                P.dma(x_[:], src, writes=[xk])
                self.norm_tile_to_hT(x_[:], xk, hb, hk, t * 128, sset, 2, ti)
                ti += 1
                if E > 1:
                    tl = (col0 + t * 128) // 128
                    for c in range(8):
                        P.mm(pr[:, :], hb[:, c, t * 128:(t + 1) * 128], rtb[:, c, :], c == 0, c == 7, [hk, "rtb"], ["pr"])
                    P.cp("dve", lgt[:], pr[:, :], ["pr"], ["lgt"])
                    P.op("dve", lambda e: e.reduce_max(m8[:, 0:1], lgt[:], AX.X), ["lgt"], ["m8"])
                    P.ts("dve", msk[:], lgt[:], m8[:, 0:1], None, ALU.is_equal, None, ["lgt", "m8"], ["msk"])
                    P.stt("dve", ex[:], msk[:], -1e30, lgt[:], ALU.mult, ALU.add, ["msk", "lgt"], ["ex"])
                    P.op("dve", lambda e: e.reduce_max(m8[:, 1:2], ex[:], AX.X), ["ex", "m8"], ["m8"])
                    P.ts("dve", msk[:], lgt[:], m8[:, 1:2], None, ALU.is_ge, None, ["lgt", "m8"], ["msk"])
                    P.ts("dve", ex[:], lgt[:], m8[:, 0:1], None, ALU.subtract, None, ["lgt", "m8"], ["ex"])
                    P.act(ex[:], ex[:], AF.Exp, ["ex"], ["ex"])
                    P.tt("dve", ex[:], ex[:], msk[:], ALU.mult, ["ex", "msk"], ["ex"])
                    P.op("dve", lambda e: e.reduce_sum(den[:], ex[:], AX.X), ["ex"], ["den"])
                    P.op("dve", lambda e: e.reciprocal(den[:], den[:]), ["den"], ["den"])
                    P.ts("dve", gates[:, tl, :], ex[:], den[:, 0:1], None, ALU.mult, None, ["ex", "den"], ["gates"])
            P.dma(dr["hTd"][bi, :, :, 0:ntok], hb[:, :, 0:ntok], reads=[hk], writes=[("hTd", bi)], eng="pool")
        yo = [self.sb("yo%d" % i, [128, D], F32) for i in range(2)]
        ya = [self.sb("ya0", [128, D], F32)] * 2
        yi = 0
        fi = 0
        for e_ in range(E):
            for c in range(8):
                P.dma(w1[:, c, :], dr["w1"][e_, c * 128:(c + 1) * 128, :], reads=["w1"], writes=["w1"], eng="pool")
                P.dma(w3[:, c, :], dr["w3"][e_, c * 128:(c + 1) * 128, :], reads=["w3"], writes=["w3"], eng="pool")
            for f in range(NF):
                P.dma(w2[:, f, :], dr["w2"][e_, f * 128:(f + 1) * 128, :], reads=["w2"], writes=["w2"], eng="pool")
            for bi, (col0, ntok) in enumerate(blocks):
                nt = ntok // 128
                hb, hk = hT[0], "hT0"
                P.dma(hb[:, :, 0:ntok], dr["hTd"][bi, :, :, 0:ntok], writes=[hk], reads=[("hTd", bi)])
                for f in range(NF):
                    a_, ak = pa[fi % 2], "pa%d" % (fi % 2)
                    b_, bk = pb[fi % 2], "pb%d" % (fi % 2)
                    fi += 1
                    for c in range(8):
                        P.mm(a_[:, 0:ntok], w1[:, c, f * 128:(f + 1) * 128], hb[:, c, 0:ntok], c == 0, c == 7, ["w1", hk], [ak])
                    for c in range(8):
                        P.mm(b_[:, 0:ntok], w3[:, c, f * 128:(f + 1) * 128], hb[:, c, 0:ntok], c == 0, c == 7, ["w3", hk], [bk])
                    P.act(sa[:, 0:ntok], a_[:, 0:ntok], AF.Silu, [ak], ["sa"])
                    P.tt("dve", gT[:, f, 0:ntok], sa[:, 0:ntok], b_[:, 0:ntok], ALU.mult, ["sa", bk], ["gT"])
                for t in range(nt):
                    tcol = col0 + t * 128
                    tl = tcol // 128
                    for hf in range(2):
                        for f in range(NF):
                            P.mm(py[hf][:, :], gT[:, f, t * 128:(t + 1) * 128], w2[:, f, hf * 512:(hf + 1) * 512], f == 0, f == NF - 1,
                                 ["gT", "w2"], ["py%d" % hf])
                    yo_, yok = yo[yi % 2], "yo%d" % (yi % 2)
                    ya_, yak = ya[0], "ya0"
                    yi += 1
                    acc = dr["accd"][tcol:tcol + 128, :]
                    if E == 1:
                        for hf in range(2):
                            P.cp("act" if hf == 0 else "dve", yo_[:, hf * 512:(hf + 1) * 512], py[hf][:, :], ["py%d" % hf], [yok])
                    else:
                        if e_ > 0:
                            P.dma(ya_[:], acc, writes=[yak], reads=[("acc", tl)])
                        for hf in range(2):
                            if e_ == 0:
                                P.ts("dve", yo_[:, hf * 512:(hf + 1) * 512], py[hf][:, :], gates[:, tl, e_:e_ + 1], None, ALU.mult, None,
                                     ["py%d" % hf, "gates"], [yok])
                            else:
                                P.stt("dve", yo_[:, hf * 512:(hf + 1) * 512], py[hf][:, :], gates[:, tl, e_:e_ + 1],
                                      ya_[:, hf * 512:(hf + 1) * 512], ALU.mult, ALU.add, ["py%d" % hf, "gates", yak], [yok])
                    P.dma(acc, yo_[:], reads=[yok], writes=[("acc", tl)], eng="pool")
        mss = self.sb("fss", [128, 1], F32)
        ti = 0
        for bi, (col0, ntok) in enumerate(blocks):
            sset = 1 if col0 < CT else 0
            for t in range(ntok // 128):
                tcol = col0 + t * 128
                tl = tcol // 128
                x_, xk = xt[ti % 2], "xt%d" % (ti % 2)
                ya_, yak = ya[0], "ya0"
                yo_, yok = yo[ti % 2], "yo%d" % (ti % 2)
                ti += 1
                dst = dr["cout"][tcol:tcol + 128, :] if col0 < CT else dr["xout"][tcol - CT: tcol - CT + 128, :]
                P.dma(x_[:], dst, writes=[xk], reads=[("xres", tl)])
                P.dma(ya_[:], dr["accd"][tcol:tcol + 128, :], writes=[yak], reads=[("acc", tl)])
                P.act(self.n_junk[:], ya_[:], AF.Square, [yak], ["n_junk", "fss"], accum_out=mss[:])
                P.ts("dve", mss[:], mss[:], 1.0 / D, EPS, ALU.mult, ALU.add, ["fss"], ["fss"])
                P.act(mss[:], mss[:], AF.Sqrt, ["fss"], ["fss"])
                P.op("dve", lambda e: e.reciprocal(mss[:], mss[:]), ["fss"], ["fss"])
                P.stt("dve", yo_[:], ya_[:], mss[:, 0:1], self.gv[:, sset, :], ALU.mult, ALU.mult, [yak, "fss", "gv"], [yok])
                P.tt("pool", yo_[:], yo_[:], x_[:], ALU.add, [yok, xk], [yok])
                P.dma(dst, yo_[:], reads=[yok], writes=[("xres", tl)], eng="pool")


def _pool_weight(L, w, s, t):
    lo = np.clip(t - w // 2, 0, L)
    hi = np.clip(t + w // 2, 0, L)
    val = ((s >= lo) & (s < hi)).astype(np.float64) / (hi - lo)
    val = val - (s == t)
    return val


def host_consts(T, j):
    Tl = T // 4
    Tc = CT + Tl
    NCL = Tl // 128
    cst = {}
    cst["ident"] = np.eye(128, dtype=np.float32)
    bo = np.zeros((128, 128), np.float32)
    bo[0:64, 0:64] = 1
    bo[64:, 64:] = 1
    cst["bones"] = bo
    tpos = np.arange(j * Tl, (j + 1) * Tl)
    row = (tpos // 64).astype(np.float32)
    colp = (tpos % 64).astype(np.float32)
    inv = (np.float32(10000.0) ** (-np.arange(16, dtype=np.float32) / np.float32(16))).astype(np.float32)
    cosT = np.ones((64, Tc), np.float32)
    sinT = np.zeros((64, Tc), np.float32)
    for a, pos in enumerate((row, colp)):
        ang = pos[None, :] * inv[:, None]
        for b in range(2):
            rows = slice(a * 32 + b * 16, a * 32 + b * 16 + 16)
            cosT[rows, CT:] = np.cos(ang)
            sinT[rows, CT:] = np.sin(ang) * (-1.0 if b == 0 else 1.0)
    cst["cosT"] = np.concatenate([cosT, cosT], 0)
    cst["sinT"] = np.concatenate([sinT, sinT], 0)
    m = np.arange(128, dtype=np.float32)
    cst["zexp"] = np.stack([C - 1 - m, m], 1).astype(np.float32)
    cst["nC"] = np.tile((np.arange(NCL + 2, dtype=np.float32) * C)[None, :], (128, 1)).astype(np.float32)
    mm, cc = np.meshgrid(m, m, indexing="ij")
    dmask = np.stack([np.maximum(cc - mm, 0), (cc >= mm).astype(np.float32),
                      np.maximum(mm - cc, 0), (mm >= cc).astype(np.float32)], 1).astype(np.float32)
    cst["dmask"] = dmask
    cst["xiexp"] = np.tile(np.stack([m + 1, C - m], 0)[None], (128, 1, 1)).astype(np.float32)
    sel = np.zeros((128, 4), np.float32)
    sel[:, j] = 1
    cst["sel"] = sel
    wins = (2, 4, 8, 16)
    s_loc = np.arange(128)[:, None]
    t_loc = np.arange(128)[None, :]
    Amain = np.zeros((128, 12, 128), np.float32)
    Anb = np.zeros((128, 8, 128), np.float32)
    Actx = np.zeros((128, 8, 128), np.float32)
    Ahalo = np.zeros((64, 8, 128), np.float32)
    for g, w in enumerate(wins):
        base_f = j * Tl
        Amain[:, 0 + g, :] = _pool_weight(T, w, base_f + s_loc, base_f + t_loc)
        mid = T // 2 // 128 * 128 if T >= 512 else 128
        midb = 128 * (T // 256)
        Amain[:, 4 + g, :] = _pool_weight(T + 4096, w, 2048 + s_loc, 2048 + t_loc)
        base_l = (j + 1) * Tl - 128
        Amain[:, 8 + g, :] = _pool_weight(T, w, base_l + s_loc, base_l + t_loc)
        Anb[:, g, :] = _pool_weight(T + 4096, w, 2048 - 128 + s_loc, 2048 + t_loc)
        Anb[:, 4 + g, :] = _pool_weight(T + 4096, w, 2048 + 128 + s_loc, 2048 + t_loc)
        Actx[:, g, :] = _pool_weight(CT, w, s_loc, t_loc)
        Actx[:, 4 + g, :] = _pool_weight(CT, w, 128 + s_loc, 128 + t_loc)
        for i in range(4):
            for r in range(16):
                spos = i * Tl + r if r < 8 else (i + 1) * Tl - 16 + r
                if i == j:
                    continue
                Ahalo[i * 16 + r, g, :] = _pool_weight(T, w, np.array([[spos]]), base_f + t_loc)[0]
                Ahalo[i * 16 + r, 4 + g, :] = _pool_weight(T, w, np.array([[spos]]), base_l + t_loc)[0]
    cst["Amain"], cst["Anb"], cst["Actx"], cst["Ahalo"] = [a.astype(NPBF) for a in (Amain, Anb, Actx, Ahalo)]
    return cst


def _rope_perm():
    perm = np.zeros(64, np.int64)
    for a in range(2):
        for b in range(2):
            for f in range(16):
                perm[a * 32 + b * 16 + f] = a * 32 + (1 - b) * 16 + f
    return perm


def layer_weights(inp, i):
    perm = _rope_perm()
    w_in = inp["w_in"][i]
    cols = {"aq": 0, "ak": 512, "av": 640, "rq": 768, "rk": 1024, "rv": 1280, "rg": 1536, "pp": 1792}
    fm = []
    for g in range(4):
        fm += [cols["aq"] + g * 64 + d for d in range(64)] + [cols["aq"] + (4 + g) * 64 + d for d in range(64)]
    for g in range(4):
        fm += [cols["aq"] + g * 64 + perm[d] for d in range(64)] + [cols["aq"] + (4 + g) * 64 + perm[d] for d in range(64)]
    fm += [cols["ak"] + d for d in range(128)]
    fm += [cols["ak"] + kv * 64 + perm[d] for kv in range(2) for d in range(64)]
    fm += [cols["rq"] + d for d in range(256)]
    fm += [cols["rk"] + d for d in range(256)]
    tm = [cols["av"] + d for d in range(128)] + [cols["rk"] + d for d in range(256)] + [cols["rv"] + d for d in range(256)] + \
         [cols["rg"] + d for d in range(256)] + [cols["pp"] + d for d in range(256)]
    gq, gk = inp["q_norm"][i], inp["k_norm"][i]
    gains = np.stack([np.tile(gq, 2), np.tile(gq[perm], 2), np.tile(gk, 2), np.tile(gk[perm], 2)], 1).astype(np.float32)
    ps = inp["pool_scale"][i]
    out = {
        "w_fm": np.ascontiguousarray(w_in[:, fm]),
        "w_tm": np.ascontiguousarray(w_in[:, tm]),
        "gains": gains,
        "w_mod": inp["w_mod"][i], "b_mod": inp["b_mod"][i],
        "norms": np.stack([inp["norm_pre_mix"][i], inp["norm_post_mix"][i], inp["norm_pre_ffn"][i], inp["norm_post_ffn"][i]], 0),
        "decay": np.ascontiguousarray(inp["ret_decay_logit"][i].reshape(8)),
        "w_out": inp["w_out"][i],
        "pool_w": np.ascontiguousarray(np.transpose(inp["pool_w"][i], (1, 0, 2))),
        "pool_s": np.ascontiguousarray(ps.reshape(2, 128).T),
    }
    if i % 2 == 0:
        out["w1"], out["w3"], out["w2"] = inp["ffn_w1"][i // 2][None], inp["ffn_w3"][i // 2][None], inp["ffn_w2"][i // 2][None]
    else:
        out["w1"], out["w3"], out["w2"] = inp["moe_w1"][i // 2], inp["moe_w3"][i // 2], inp["moe_w2"][i // 2]
        out["router"] = inp["moe_router"][i // 2]
    return out


_NC_CACHE = {}


def get_nc(T, layer, phases, n_exp):
    key = (T, layer, phases, n_exp)
    if key not in _NC_CACHE:
        b = Builder(T, layer, phases, n_exp)
        nc = b.build()
        _NC_CACHE[key] = (nc, sorted(k for k in b.dr))
    return _NC_CACHE[key][0]


A_IN = ["xin", "cin", "cvec", "w_mod", "b_mod", "norms", "decay", "ident", "zexp", "nC", "w_fm", "w_tm", "gains", "bones",
        "cosT", "sinT"]
A_OUT = ["QT", "KT", "VA", "rqT", "rkT", "rk", "rv", "rg", "pp", "Lout"]
B_IN = ["xin", "cin", "cvec", "w_mod", "b_mod", "norms", "decay", "ident", "zexp", "nC", "QT", "KT", "VA", "rqT", "rkT", "rk",
        "rv", "rg", "pp", "KTg", "VAg", "Lg", "Hg", "w_out", "dmask", "xiexp", "sel", "Amain", "Anb", "Actx", "Ahalo", "pool_w",
        "pool_s", "w1", "w3", "w2"]


def run_model(inp, n_layers=2):
    x = np.asarray(inp["x"], np.float32)
    B, T, _ = x.shape
    Tl = T // 4
    ncore = 8
    xs = [np.ascontiguousarray(x[c // 4, (c % 4) * Tl:((c % 4) + 1) * Tl]) for c in range(ncore)]
    cs = [np.ascontiguousarray(np.asarray(inp["ctx"], np.float32)[c // 4]) for c in range(ncore)]
    csts = [host_consts(T, c % 4) for c in range(ncore)]
    cvecs = [np.stack([inp["c"][c // 4], inp["c_ctx"]], 0).astype(np.float32) for c in range(ncore)]
    for i in range(n_layers):
        lw = layer_weights(inp, i)
        n_exp = 1 if i % 2 == 0 else NE
        ncA = get_nc(T, i, "A", n_exp)
        maps = []
        for c in range(ncore):
            m = {"xin": xs[c], "cin": cs[c], "cvec": cvecs[c]}
            m.update(lw)
            m.update(csts[c])
            maps.append({k: np.ascontiguousarray(m[k]) for k in A_IN})
        resA = run_bass_kernel_spmd(ncA, maps, core_ids=list(range(ncore))).results
        ncB = get_nc(T, i, "B", n_exp)
        maps = []
        for c in range(ncore):
            b = c // 4
            grp = [resA[b * 4 + r] for r in range(4)]
            m = {"xin": xs[c], "cin": cs[c], "cvec": cvecs[c]}
            m.update(lw)
            m.update(csts[c])
            for k in A_OUT:
                m[k] = resA[c][k]
            m["KTg"] = np.concatenate([g["KT"][:, CT:] for g in grp], 0)
            m["VAg"] = np.concatenate([g["VA"][CT:] for g in grp], 0)
            m["Lg"] = np.concatenate([g["Lout"] for g in grp], 0)
            m["Hg"] = np.concatenate([np.concatenate([g["pp"][CT:CT + 8], g["pp"][-8:]], 0) for g in grp], 0)
            names = list(B_IN) + (["router"] if n_exp > 1 else [])
            maps.append({k: np.ascontiguousarray(m[k]) for k in names})
        resB = run_bass_kernel_spmd(ncB, maps, core_ids=list(range(ncore))).results
        xs = [resB[c]["xout"] for c in range(ncore)]
        if i == 0:
            cs = [resB[c]["cout"] for c in range(ncore)]
    out = np.zeros((B, T, D), np.float32)
    for c in range(ncore):
        out[c // 4, (c % 4) * Tl:((c % 4) + 1) * Tl] = xs[c]
    return out


def kernel(**inputs):
    inp = {k: np.asarray(v) for k, v in inputs.items()}
    return run_model(inp, 2)
```

```python
import contextlib
import numpy as np
import ml_dtypes
import concourse.bass as bass
import concourse.mybir as mybir
from concourse.bass_utils import run_bass_kernel_spmd

F32 = mybir.dt.float32
BF16 = mybir.dt.bfloat16
AF = mybir.ActivationFunctionType
ALU = mybir.AluOpType
AX = mybir.AxisListType
NPBF = ml_dtypes.bfloat16

D = 1024
CT = 256
DFF = 2816
NF = DFF // 128
NE = 8
EPS = 1e-6
C = 128

ENGS = ["pe", "act", "dve", "pool", "sp"]
NDMA = 8
SAME_ENG_SYNC = {"pe": False, "act": True, "dve": True, "pool": True, "sp": False}


class Prog:
    def __init__(self, nc, stack):
        self.nc = nc
        self.ops = {e: [] for e in ENGS}
        self.count = {e: 0 for e in ENGS}
        self.waited = {e: {} for e in ENGS}
        self.last_w = {}
        self.readers = {}
        self.dma_n = {e: 0 for e in ENGS}
        self.sems = {}
        for e in ENGS:
            self.sems[("c", e)] = stack.enter_context(nc.semaphore("c_" + e))
        for e in ("sp", "pool", "act"):
            for i in range(NDMA):
                self.sems[("d", e, i)] = stack.enter_context(nc.semaphore("d_%s%d" % (e, i)))
        self.semval = {k: 0 for k in self.sems}
        self.nops = 0

    def op(self, eng, fn, reads=(), writes=(), dma=False, nosame=False):
        deps = {}

        def add(d):
            if d is None:
                return
            sk, v = d
            if deps.get(sk, 0) < v:
                deps[sk] = v

        for k in reads:
            add(self.last_w.get(k))
        for k in writes:
            add(self.last_w.get(k))
            for sk, v in self.readers.get(k, {}).items():
                add((sk, v))
        if dma:
            n = self.dma_n[eng]
            idx, use = n % NDMA, n // NDMA
            semkey = ("d", eng, idx)
            val = 16 * (use + 1)
            if use > 0:
                add((semkey, 16 * use))
            self.dma_n[eng] += 1
            inc = 16
        else:
            self.count[eng] += 1
            semkey = ("c", eng)
            val = self.count[eng]
            inc = 1
        self.semval[semkey] = val
        for sk, v in deps.items():
            if sk == ("c", eng) and (nosame or not SAME_ENG_SYNC[eng]):
                continue
            if self.waited[eng].get(sk, 0) >= v:
                continue
            self.waited[eng][sk] = v
            s = self.sems[sk]
            self.ops[eng].append(lambda e, s=s, v=v: e.wait_ge(s, v))
        s = self.sems[semkey]
        self.ops[eng].append(lambda e, s=s, inc=inc: fn(e).then_inc(s, inc))
        me = (semkey, val)
        for k in reads:
            r = self.readers.setdefault(k, {})
            if r.get(semkey, 0) < val:
                r[semkey] = val
        for k in writes:
            self.last_w[k] = me
            self.readers[k] = {}
        self.nops += 1

    def new_phase(self, stack):
        self.phase_id = getattr(self, "phase_id", 0) + 1
        for e in ENGS:
            k = ("c", e)
            self.sems[k] = stack.enter_context(self.nc.semaphore("c%d_%s" % (self.phase_id, e)))
            self.semval[k] = 0
            self.count[e] = 0
            for w in self.waited.values():
                w.pop(k, None)

    def barrier(self):
        for e in ENGS:
            for sk, v in self.semval.items():
                if v == 0:
                    continue
                if sk == ("c", e) and e in ("pe", "sp"):
                    continue
                if self.waited[e].get(sk, 0) >= v:
                    continue
                self.waited[e][sk] = v
                s = self.sems[sk]
                self.ops[e].append(lambda en, s=s, v=v: en.wait_ge(s, v))
        self.last_w = {}
        self.readers = {}

    def emit(self):
        nc = self.nc
        ops = self.ops
        with nc.Block() as block:
            @block.tensor
            def _(e):
                for f in ops["pe"]:
                    f(e)

            @block.scalar
            def _(e):
                for f in ops["act"]:
                    f(e)

            @block.vector
            def _(e):
                for f in ops["dve"]:
                    f(e)

            @block.gpsimd
            def _(e):
                for f in ops["pool"]:
                    f(e)

            @block.sync
            def _(e):
                for f in ops["sp"]:
                    f(e)
        self.ops = {e: [] for e in ENGS}

    def dma(self, out, in_, reads=(), writes=(), eng="sp", **kw):
        self.op(eng, lambda e: e.dma_start(out=out, in_=in_, **kw), reads, writes, dma=True)

    def mm(self, out, lhsT, rhs, start, stop, reads=(), writes=(), **kw):
        self.op("pe", lambda e: e.matmul(out, lhsT, rhs, start=start, stop=stop, **kw), reads, writes)

    def tr(self, out, in_, ident, reads=(), writes=()):
        self.op("pe", lambda e: e.transpose(out, in_, ident), reads, writes)

    def act(self, out, in_, func, reads=(), writes=(), **kw):
        self.op("act", lambda e: e.activation(out, in_, func, **kw), reads, writes)

    def ts(self, eng, out, in0, s1, s2, op0, op1=None, reads=(), writes=()):
        if op1 is None:
            self.op(eng, lambda e: e.tensor_scalar(out, in0, s1, None, op0), reads, writes)
        else:
            self.op(eng, lambda e: e.tensor_scalar(out, in0, s1, s2, op0, op1), reads, writes)

    def tt(self, eng, out, in0, in1, op, reads=(), writes=()):
        self.op(eng, lambda e: e.tensor_tensor(out, in0, in1, op), reads, writes)

    def stt(self, eng, out, in0, scalar, in1, op0, op1, reads=(), writes=()):
        self.op(eng, lambda e: e.scalar_tensor_tensor(out, in0, scalar, in1, op0, op1), reads, writes)

    def cp(self, eng, out, in_, reads=(), writes=()):
        if eng == "act":
            self.op(eng, lambda e: e.copy(out, in_), reads, writes)
        else:
            self.op(eng, lambda e: e.tensor_copy(out, in_), reads, writes)


class Builder:
    def __init__(self, T, layer, phases, n_exp):
        self.T = T
        self.Tl = T // 4
        self.Tc = CT + self.Tl
        self.NCL = self.Tl // 128
        self.NKT = (CT + T) // 128
        self.layer = layer
        self.phases = phases
        self.n_exp = n_exp
        self.need_ctx = layer == 0
        self.nc = bass.Bass("TRN2", target_bir_lowering=False)
        self.dr = {}
        self.blocks = [(0, CT)] + [(CT + 512 * i, 512) for i in range(self.Tl // 512)]

    def din(self, name, shape, dt):
        self.dr[name] = self.nc.dram_tensor(name, list(shape), dt, kind="ExternalInput").ap()
        return self.dr[name]

    def dout(self, name, shape, dt):
        self.dr[name] = self.nc.dram_tensor(name, list(shape), dt, kind="ExternalOutput").ap()
        return self.dr[name]

    def build(self):
        nc = self.nc
        with contextlib.ExitStack() as gst:
            self.P = Prog(nc, gst)
            if "A" in self.phases:
                self.declare_A()
            if "B" in self.phases:
                self.declare_B()
            if "A" in self.phases:
                with contextlib.ExitStack() as st:
                    self.st = st
                    self.common_consts()
                    self.emit_mod()
                    import os as _os
                    self.kstop = int(_os.environ.get("KSTOP", "0"))
                    if self.kstop != 1:
                        self.phase_A()
                    self.P.barrier()
                    self.P.emit()
            if "B" in self.phases:
                with contextlib.ExitStack() as st:
                    self.st = st
                    self.common_consts()
                    self.emit_mod()
                    import os as _os
                    self.kb = int(_os.environ.get("KSTOPB", "0"))
                    self.phase_B1()
                    self.P.barrier()
                    self.P.emit()
                    self.P.new_phase(gst)
                with contextlib.ExitStack() as st:
                    self.st = st
                    self.common_consts()
                    self.emit_mod(only_ffn=True)
                    if self.kb in (0, 6, 7):
                        self.phase_B2()
                    self.P.barrier()
                    self.P.emit()
        return nc

    def sb(self, name, shape, dt):
        self.uid = getattr(self, "uid", 0) + 1
        return self.st.enter_context(self.nc.sbuf_tensor("s%d_%s" % (self.uid, name), list(shape), dt))

    def ps(self, name, shape, dt):
        self.uid = getattr(self, "uid", 0) + 1
        return self.st.enter_context(self.nc.psum_tensor("p%d_%s" % (self.uid, name), list(shape), dt))

    def declare_common(self):
        if "xin" in self.dr:
            return
        Tl, Tc = self.Tl, self.Tc
        self.din("xin", [Tl, D], F32)
        self.din("cin", [CT, D], F32)
        self.din("cvec", [2, D], F32)
        self.din("w_mod", [D, 6 * D], F32)
        self.din("b_mod", [6 * D], F32)
        self.din("norms", [4, D], F32)
        self.din("decay", [8], F32)
        self.din("ident", [128, 128], F32)
        self.din("zexp", [128, 2], F32)
        self.din("nC", [128, self.NCL + 2], F32)

    def declare_A(self):
        self.declare_common()
        Tl, Tc = self.Tl, self.Tc
        self.din("w_fm", [D, 1792], F32)
        self.din("w_tm", [D, 1152], F32)
        self.din("gains", [128, 4], F32)
        self.din("bones", [128, 128], F32)
        self.din("cosT", [128, Tc], F32)
        self.din("sinT", [128, Tc], F32)
        o = self.dout if "B" not in self.phases else self.dint
        o("QT", [128, 4, Tc], BF16)
        o("KT", [128, Tc], BF16)
        o("VA", [Tc, 132], BF16)
        o("rqT", [128, 2, Tc], BF16)
        o("rkT", [128, 2, Tc], BF16)
        o("rk", [Tc, 256], BF16)
        o("rv", [Tc, 256], BF16)
        o("rg", [Tc, 256], BF16)
        o("pp", [Tc, 256], BF16)
        o("Lout", [256, 128], F32)

    def declare_B(self):
        self.declare_common()
        Tl, Tc, T = self.Tl, self.Tc, self.T
        if "A" not in self.phases:
            i = self.din
            i("QT", [128, 4, Tc], BF16)
            i("KT", [128, Tc], BF16)
            i("VA", [Tc, 132], BF16)
            i("rqT", [128, 2, Tc], BF16)
            i("rkT", [128, 2, Tc], BF16)
            i("rk", [Tc, 256], BF16)
            i("rv", [Tc, 256], BF16)
            i("rg", [Tc, 256], BF16)
            i("pp", [Tc, 256], BF16)
            i("KTg", [4 * 128, Tl], BF16)
            i("VAg", [4 * Tl, 132], BF16)
            i("Lg", [4 * 256, 128], F32)
            i("Hg", [64, 256], BF16)
        self.din("w_out", [D, D], F32)
        self.din("dmask", [128, 4, 128], F32)
        self.din("xiexp", [128, 2, 128], F32)
        self.din("sel", [128, 4], F32)
        self.din("Amain", [128, 12, 128], BF16)
        self.din("Anb", [128, 8, 128], BF16)
        self.din("Actx", [128, 8, 128], BF16)
        self.din("Ahalo", [64, 8, 128], BF16)
        self.din("pool_w", [64, 4, 64], F32)
        self.din("pool_s", [128, 2], F32)
        E = self.n_exp
        self.din("w1", [E, D, DFF], F32)
        self.din("w3", [E, D, DFF], F32)
        self.din("w2", [E, DFF, D], F32)
        if E > 1:
            self.din("router", [D, NE], F32)
        self.dout("xout", [Tl, D], F32)
        if self.need_ctx:
            self.dout("cout", [CT, D], F32)
        self.dr["hTd"] = self.nc.dram_tensor("hTd", [len(self.blocks), 128, 8, 512], BF16, kind="Internal").ap()
        self.dr["accd"] = self.nc.dram_tensor("accd", [self.Tc, D], F32, kind="Internal").ap()

    def dint(self, name, shape, dt):
        self.dr[name] = self.nc.dram_tensor(name, list(shape), dt, kind="Internal").ap()
        return self.dr[name]

    def common_consts(self):
        P, dr = self.P, self.dr
        self.identf = self.sb("identf", [128, 128], F32)
        self.identb = self.sb("identb", [128, 128], BF16)
        P.dma(self.identf[:], dr["ident"], writes=["identf"])
        P.cp("dve", self.identb[:], self.identf[:], ["identf"], ["identb"])

    def emit_mod(self, only_ffn=False):
        P, dr = self.P, self.dr
        self.modT = self.sb("modT", [128, 2, 4, 8], F32)
        self.gv = self.sb("gv", [128, 2, D], F32)
        if "modd" not in dr:
            dr["modd"] = self.nc.dram_tensor("modd", [2, 6 * D], F32, kind="Internal").ap()
        outer = self.st
        with contextlib.ExitStack() as tmp:
            self.st = tmp
            cv = self.sb("cv", [128, 2, 8], F32)
            scv = self.sb("scv", [128, 2, 8], F32)
            for s_ in range(2):
                P.dma(cv[:, s_, :], dr["cvec"][s_].rearrange("(c p) -> p c", p=128), reads=["cv"], writes=["cv"],
                      allow_slow_non_contiguous=True)
            P.act(scv[:], cv[:], AF.Silu, ["cv"], ["scv"])
            modrow = self.sb("modrow", [2, 6 * D], F32)
            brow = self.sb("brow", [2, 6 * D], F32)
            P.dma(brow[:], dr["b_mod"].partition_broadcast(2), writes=["brow"])
            wm = [self.sb("wm%d" % i, [128, 8, 512], F32) for i in range(2)]
            pm = self.ps("pm", [128, 512], F32)
            for j in range(12):
                w = wm[j % 2]
                P.dma(w[:], dr["w_mod"][:, j * 512:(j + 1) * 512].rearrange("(c p) n -> p c n", p=128),
                      writes=["wm%d" % (j % 2)])
                for c in range(8):
                    P.mm(pm[0:2, :], scv[:, :, c], w[:, c, :], c == 0, c == 7, ["scv", "wm%d" % (j % 2)], ["pm"])
                P.tt("dve", modrow[:, j * 512:(j + 1) * 512], pm[0:2, :], brow[:, j * 512:(j + 1) * 512], ALU.add,
                     ["pm", "brow"], ["modrow"])
            P.dma(dr["modd"], modrow[:], reads=["modrow"], writes=["modd"])
            nT = self.sb("nT", [128, 4, 8], F32)
            for k_ in range(4):
                P.dma(nT[:, k_, :], dr["norms"][k_].rearrange("(c p) -> p c", p=128), reads=["nT"], writes=["nT"],
                      allow_slow_non_contiguous=True)
            mT = self.sb("mT", [128, 6, 8, 2], F32)
            pmt = self.ps("pmt", [128, 48, 2], F32)
            for v in range(6):
                for c in range(8):
                    P.tr(pmt[:, v * 8 + c, :], modrow[0:2, v * D + c * 128: v * D + (c + 1) * 128], self.identf[0:2, 0:2],
                         ["modrow", "identf"], ["pmt"])
            P.cp("dve", mT[:].rearrange("p v c s -> p (v c s)"), pmt[:].rearrange("p a s -> p (a s)"), ["pmt"], ["mT"])
            for s_ in range(2):
                P.stt("dve", self.modT[:, s_, 0, :], mT[:, 1, :, s_], 1.0, nT[:, 0, :], ALU.add, ALU.mult, ["mT", "nT"], ["modT"])
                P.cp("dve", self.modT[:, s_, 1, :], mT[:, 0, :, s_], ["mT"], ["modT"])
                P.stt("dve", self.modT[:, s_, 2, :], mT[:, 4, :, s_], 1.0, nT[:, 2, :], ALU.add, ALU.mult, ["mT", "nT"], ["modT"])
                P.cp("dve", self.modT[:, s_, 3, :], mT[:, 3, :, s_], ["mT"], ["modT"])
            nb = self.sb("nb", [128, D], F32)
            P.dma(nb[:], dr["norms"][3 if only_ffn else 1].partition_broadcast(128), writes=["nb"])
            v = 5 if only_ffn else 2
            for s_ in range(2):
                P.dma(self.gv[:, s_, :], dr["modd"][s_, v * D:(v + 1) * D].partition_broadcast(128), reads=["modd", "gv"], writes=["gv"])
            for s_ in range(2):
                P.tt("dve", self.gv[:, s_, :], self.gv[:, s_, :], nb[:], ALU.mult, ["gv", "nb"], ["gv"])
            P.barrier()
        self.st = outer

    def norm_tile_to_hT(self, xt, xkey, hT, hkey, col, sset, v, tagi):
        P = self.P
        junk, ss, xs, pT = self.n_junk, self.n_ss[tagi % 2], self.n_xs[tagi % 2], self.n_pT
        ssk, xsk = "n_ss%d" % (tagi % 2), "n_xs%d" % (tagi % 2)
        P.act(junk[:], xt, AF.Square, [xkey], ["n_junk", ssk], accum_out=ss[:])
        P.ts("dve", ss[:], ss[:], 1.0 / D, EPS, ALU.mult, ALU.add, [ssk], [ssk])
        P.act(ss[:], ss[:], AF.Sqrt, [ssk], [ssk])
        P.op("dve", lambda e: e.reciprocal(ss[:], ss[:]), [ssk], [ssk])
        P.ts("dve", xs[:], xt, ss[:, 0:1], None, ALU.mult, None, [xkey, ssk], [xsk])
        for c in range(8):
            P.tr(pT[:, c, :], xs[:, c * 128:(c + 1) * 128], self.identb[:], [xsk, "identb"], ["n_pT"])
        for c in range(8):
            eng = "dve" if c % 2 == 0 else "pool"
            if eng == "pool":
                eng = "dve"
            P.ts(eng, hT[:, c, col:col + 128], pT[:, c, :], self.modT[:, sset, v, c:c + 1], self.modT[:, sset, v + 1, c:c + 1],
                 ALU.mult, ALU.add, ["n_pT", "modT"], [hkey])

    def alloc_norm(self):
        self.n_junk = self.sb("n_junk", [128, D], BF16)
        self.n_ss = [self.sb("n_ss%d" % i, [128, 1], F32) for i in range(2)]
        self.n_xs = [self.sb("n_xs%d" % i, [128, D], BF16) for i in range(2)]
        self.n_pT = self.ps("n_pT", [128, 8, 128], BF16)

    def src_x(self, col, n):
        if col < CT:
            return self.dr["cin"][col:col + n, :]
        return self.dr["xin"][col - CT:col - CT + n, :]

    def ret_consts(self):
        P, dr = self.P, self.dr
        dl = self.sb("dl", [128, 8], F32)
        P.dma(dl[:], dr["decay"].partition_broadcast(128), writes=["dl"])
        lg = self.sb("lg", [128, 8], F32)
        P.act(lg[:], dl[:], AF.Exp, ["dl"], ["lg"], scale=-1.0)
        P.act(lg[:], lg[:], AF.Ln, ["lg"], ["lg"], bias=1.0)
        P.ts("dve", lg[:], lg[:], -1.0, None, ALU.mult, None, ["lg"], ["lg"])
        self.lg = lg
        lgs = self.sb("lgs", [128, 2, 2], F32)
        for d_ in range(2):
            for p in range(2):
                for hl in range(2):
                    P.cp("dve", lgs[hl * 64:(hl + 1) * 64, d_, p:p + 1], lg[hl * 64:(hl + 1) * 64, d_ * 4 + 2 * p + hl: d_ * 4 + 2 * p + hl + 1],
                         ["lg"], ["lgs"])
        self.lgs = lgs
        zx = self.sb("zx", [128, 2], F32)
        P.dma(zx[:], dr["zexp"], writes=["zx"])
        nCt = self.sb("nCt", [128, self.NCL + 2], F32)
        P.dma(nCt[:], dr["nC"], writes=["nCt"])
        zt = self.sb("zt", [128, 8], F32)
        for d_ in range(2):
            for h in range(4):
                P.act(zt[:, d_ * 4 + h: d_ * 4 + h + 1], zx[:, d_:d_ + 1], AF.Exp, ["zx", "lg"], ["zt"],
                      scale=lg[:, d_ * 4 + h: d_ * 4 + h + 1])
        ones = self.sb("ones64", [128, 64], F32)
        P.op("pool", lambda e: e.memset(ones[:], 1.0), (), ["ones64"])
        self.Z = self.sb("Z", [128, 2, 256], F32)
        for d_ in range(2):
            for h in range(4):
                P.ts("dve", self.Z[:, d_, h * 64:(h + 1) * 64], ones[:], zt[:, d_ * 4 + h: d_ * 4 + h + 1], None, ALU.mult, None,
                     ["ones64", "zt"], ["Z"])
        self.pw = self.sb("pw", [128, 2, 2, self.NCL + 2], F32)
        for d_ in range(2):
            for p in range(2):
                P.act(self.pw[:, d_, p, :], nCt[:], AF.Exp, ["nCt", "lgs"], ["pw"], scale=lgs[:, d_, p:p + 1])

    def emit_U(self, rk_t, rv_t, keys, pU, tag):
        P = self.P
        kz = self.u_kz
        for d_ in range(2):
            P.tt("dve", kz[:, d_, :], rk_t, self.Z[:, d_, :], ALU.mult, keys + ["Z"], ["u_kz"])
        for d_ in range(2):
            for p in range(2):
                P.mm(pU[:, d_ * 2 + p, :], kz[:, d_, p * 128:(p + 1) * 128], rv_t[:, p * 128:(p + 1) * 128], True, True,
                     ["u_kz"] + keys, [tag])

    def phase_A(self):
        P, dr = self.P, self.dr
        Tc = self.Tc
        self.alloc_norm()
        self.ret_consts()
        wfm = self.sb("wfm", [128, 8, 1792], BF16)
        wtm = self.sb("wtm", [128, 8, 1152], BF16)
        for c in range(8):
            P.dma(wfm[:, c, :], dr["w_fm"][c * 128:(c + 1) * 128, :], writes=["wfm"], reads=["wfm"], eng="pool")
            P.dma(wtm[:, c, :], dr["w_tm"][c * 128:(c + 1) * 128, :], writes=["wtm"], reads=["wtm"], eng="pool")
        gains = self.sb("gains", [128, 4], F32)
        P.dma(gains[:], dr["gains"], writes=["gains"])
        bonesf = self.sb("bonesf", [128, 128], F32)
        bones = self.sb("bones", [128, 128], BF16)
        P.dma(bonesf[:], dr["bones"], writes=["bonesf"])
        P.cp("dve", bones[:], bonesf[:], ["bonesf"], ["bones"])
        cosT = self.sb("cosT", [128, Tc], F32)
        sinT = self.sb("sinT", [128, Tc], F32)
        P.dma(cosT[:], dr["cosT"], writes=["cosT"])
        P.dma(sinT[:], dr["sinT"], writes=["sinT"])
        self.u_kz = self.sb("u_kz", [128, 2, 256], BF16)
        Lacc = self.sb("Lacc", [128, 2, 2, 64], F32)
        P.op("pool", lambda e: e.memset(Lacc[:], 0.0), (), ["Lacc"])

        xt = [self.sb("xt%d" % i, [128, D], F32) for i in range(2)]
        hT = [self.sb("hT%d" % i, [128, 8, 512], BF16) for i in range(2)]
        pa = self.ps("pa", [128, 512], F32)
        pb = self.ps("pb", [128, 512], F32)
        pss = self.ps("pss", [128, 512], F32)
        ptm = [self.ps("ptm%d" % i, [128, 512], F32) for i in range(2)]
        pU = self.ps("pU", [128, 4, 128], F32)
        sq = self.sb("sq", [128, 512], BF16)
        rr = self.sb("rr", [128, 512], F32)
        t1 = self.sb("t1", [128, 512], F32)
        t2 = self.sb("t2", [128, 512], F32)
        qo = [self.sb("qo%d" % i, [128, 4, 512], BF16) for i in range(2)]
        ko = [self.sb("ko%d" % i, [128, 512], BF16) for i in range(2)]
        rqo = [self.sb("rqo%d" % i, [128, 2, 512], BF16) for i in range(2)]
        rko = [self.sb("rko%d" % i, [128, 2, 512], BF16) for i in range(2)]
        va = [self.sb("va%d" % i, [128, 132], BF16) for i in range(2)]
        tko = [self.sb("tko%d" % i, [128, 256], BF16) for i in range(2)]
        tvo = [self.sb("tvo%d" % i, [128, 256], BF16) for i in range(2)]
        tgo = [self.sb("tgo%d" % i, [128, 256], BF16) for i in range(2)]
        tpo = [self.sb("tpo%d" % i, [128, 256], BF16) for i in range(2)]
        for i in range(2):
            P.op("pool", lambda e, i=i: e.memset(va[i][:], 1.0), (), ["va%d" % i])

        ti = 0
        if self.kstop == 2:
            return
        for bi, (col0, ntok) in enumerate(self.blocks):
            nt = ntok // 128
            hb = hT[bi % 2]
            hk = "hT%d" % (bi % 2)
            sset = 1 if col0 < CT else 0
            if self.kstop == 3 and bi > 0:
                return
            for t in range(nt):
                x_ = xt[ti % 2]
                xk = "xt%d" % (ti % 2)
                P.dma(x_[:], self.src_x(col0 + t * 128, 128), writes=[xk])
                self.norm_tile_to_hT(x_[:], xk, hb, hk, t * 128, sset, 0, ti)
                ti += 1
            if self.kstop == 4:
                return
            qb_, qk_ = qo[bi % 2], "qo%d" % (bi % 2)
            kb_, kk_ = ko[bi % 2], "ko%d" % (bi % 2)

            def fm(ps_, m):
                for c in range(8):
                    P.mm(ps_[:, 0:ntok], wfm[:, c, m * 128:(m + 1) * 128], hb[:, c, 0:ntok], c == 0, c == 7,
                         ["wfm", hk], [ps_.name if hasattr(ps_, "name") else "px"])

            for g in range(5):
                ma, mb = (g, g + 4) if g < 4 else (8, 9)
                gcol = 0 if g < 4 else 2
                for c in range(8):
                    P.mm(pa[:, 0:ntok], wfm[:, c, ma * 128:(ma + 1) * 128], hb[:, c, 0:ntok], c == 0, c == 7, ["wfm", hk], ["pa"])
                for c in range(8):
                    P.mm(pb[:, 0:ntok], wfm[:, c, mb * 128:(mb + 1) * 128], hb[:, c, 0:ntok], c == 0, c == 7, ["wfm", hk], ["pb"])
                P.act(sq[:, 0:ntok], pa[:, 0:ntok], AF.Square, ["pa"], ["sq"])
                P.mm(pss[:, 0:ntok], bones[:], sq[:, 0:ntok], True, True, ["bones", "sq"], ["pss"])
                P.act(rr[:, 0:ntok], pss[:, 0:ntok], AF.Sqrt, ["pss"], ["rr"], scale=1.0 / 64, bias=EPS)
                P.op("dve", lambda e, n=ntok: e.reciprocal(rr[:, 0:n], rr[:, 0:n]), ["rr"], ["rr"])
                P.stt("dve", t1[:, 0:ntok], pa[:, 0:ntok], gains[:, gcol:gcol + 1], cosT[:, col0:col0 + ntok], ALU.mult, ALU.mult,
                      ["pa", "gains", "cosT"], ["t1"])
                P.stt("dve", t2[:, 0:ntok], pb[:, 0:ntok], gains[:, gcol + 1:gcol + 2], sinT[:, col0:col0 + ntok], ALU.mult, ALU.mult,
                      ["pb", "gains", "sinT"], ["t2"])
                P.tt("pool", t1[:, 0:ntok], t1[:, 0:ntok], t2[:, 0:ntok], ALU.add, ["t1", "t2"], ["t1"])
                if g < 4:
                    P.tt("pool", qb_[:, g, 0:ntok], t1[:, 0:ntok], rr[:, 0:ntok], ALU.mult, ["t1", "rr"], [qk_])
                else:
                    P.tt("pool", kb_[:, 0:ntok], t1[:, 0:ntok], rr[:, 0:ntok], ALU.mult, ["t1", "rr"], [kk_])
            if self.kstop == 5:
                return
            P.dma(dr["QT"][:, :, col0:col0 + ntok], qb_[:, :, 0:ntok], reads=[qk_])
            P.dma(dr["KT"][:, col0:col0 + ntok], kb_[:, 0:ntok], reads=[kk_])
            rq_, rqk = rqo[bi % 2], "rqo%d" % (bi % 2)
            rk_, rkk = rko[bi % 2], "rko%d" % (bi % 2)
            for p in range(2):
                for c in range(8):
                    P.mm(pa[:, 0:ntok], wfm[:, c, (10 + p) * 128:(11 + p) * 128], hb[:, c, 0:ntok], c == 0, c == 7, ["wfm", hk], ["pa"])
                P.cp("act", rq_[:, p, 0:ntok], pa[:, 0:ntok], ["pa"], [rqk])
                for c in range(8):
                    P.mm(pb[:, 0:ntok], wfm[:, c, (12 + p) * 128:(13 + p) * 128], hb[:, c, 0:ntok], c == 0, c == 7, ["wfm", hk], ["pb"])
                P.ts("dve", rk_[:, p, 0:ntok], pb[:, 0:ntok], 0.125, None, ALU.mult, None, ["pb"], [rkk])
            P.dma(dr["rqT"][:, :, col0:col0 + ntok], rq_[:, :, 0:ntok], reads=[rqk])
            P.dma(dr["rkT"][:, :, col0:col0 + ntok], rk_[:, :, 0:ntok], reads=[rkk])
            if self.kstop == 6:
                return
            for t in range(nt):
                tcol = col0 + t * 128
                s = (bi * 4 + t) % 2
                p0 = ptm[0]
                for c in range(8):
                    P.mm(p0[:, 0:384], hb[:, c, t * 128:(t + 1) * 128], wtm[:, c, 0:384], c == 0, c == 7, [hk, "wtm"], ["ptm0"])
                if self.kstop == 9:
                    continue
                P.cp("dve", va[s][:, 0:64], p0[:, 0:64], ["ptm0"], ["va%d" % s])
                P.cp("dve", va[s][:, 66:130], p0[:, 64:128], ["ptm0"], ["va%d" % s])
                P.ts("dve", tko[s][:], p0[:, 128:384], 0.125, None, ALU.mult, None, ["ptm0"], ["tko%d" % s])
                if self.kstop == 8:
                    continue
                p1 = ptm[1]
                for c in range(8):
                    P.mm(p1[:, 0:512], hb[:, c, t * 128:(t + 1) * 128], wtm[:, c, 384:896], c == 0, c == 7, [hk, "wtm"], ["ptm1"])
                P.cp("dve", tvo[s][:], p1[:, 0:256], ["ptm1"], ["tvo%d" % s])
                P.cp("dve", tgo[s][:], p1[:, 256:512], ["ptm1"], ["tgo%d" % s])
                for c in range(8):
                    P.mm(p0[:, 0:256], hb[:, c, t * 128:(t + 1) * 128], wtm[:, c, 896:1152], c == 0, c == 7, [hk, "wtm"], ["ptm0"])
                P.cp("dve", tpo[s][:], p0[:, 0:256], ["ptm0"], ["tpo%d" % s])
                if self.kstop == 7:
                    continue
                P.dma(dr["VA"][tcol:tcol + 128, :], va[s][:], reads=["va%d" % s], eng="sp")
                P.dma(dr["rk"][tcol:tcol + 128, :], tko[s][:], reads=["tko%d" % s], eng="sp")
                P.dma(dr["rv"][tcol:tcol + 128, :], tvo[s][:], reads=["tvo%d" % s], eng="sp")
                P.dma(dr["rg"][tcol:tcol + 128, :], tgo[s][:], reads=["tgo%d" % s], eng="sp")
                P.dma(dr["pp"][tcol:tcol + 128, :], tpo[s][:], reads=["tpo%d" % s], eng="sp")
                if col0 >= CT and self.kstop != 10:
                    n = (tcol - CT) // 128
                    if self.kstop == 12:
                        continue
                    self.emit_U(tko[s][:], tvo[s][:], ["tko%d" % s, "tvo%d" % s], pU, "pU")
                    for d_ in range(2):
                        pwi = (self.NCL - 1 - n) if d_ == 0 else n
                        for p in range(2):
                            for hl in range(2):
                                r = slice(hl * 64, (hl + 1) * 64)
                                P.stt("dve", Lacc[r, d_, p, :], pU[r, d_ * 2 + p, hl * 64:(hl + 1) * 64],
                                      self.pw[r, d_, p, pwi:pwi + 1], Lacc[r, d_, p, :], ALU.mult, ALU.add,
                                      ["pU", "pw", "Lacc"], ["Lacc"])
        for d_ in range(2):
            P.dma(dr["Lout"][d_ * 128:(d_ + 1) * 128, :], Lacc[:, d_, :, :].rearrange("p a e -> p (a e)"), reads=["Lacc"])

    def phase_B1(self):
        P, dr = self.P, self.dr
        T, Tl, Tc, NCL, NKT = self.T, self.Tl, self.Tc, self.NCL, self.NKT
        self.ret_consts()
        lg, lgs = self.lg, self.lgs
        wo = self.sb("wo", [128, 8, D], BF16)
        for c in range(8):
            P.dma(wo[:, c, :], dr["w_out"][c * 128:(c + 1) * 128, :], writes=["wo"], reads=["wo"], eng="pool")
        KTa = self.sb("KTa", [128, CT + T], BF16)
        VAa = self.sb("VAa", [128, NKT, 132], BF16)
        P.dma(KTa[:, 0:CT], dr["KT"][:, 0:CT], writes=["KTa"])
        for r in range(4):
            P.dma(KTa[:, CT + r * Tl: CT + (r + 1) * Tl], dr["KTg"][r * 128:(r + 1) * 128, :], reads=["KTa"], writes=["KTa"])
        P.dma(VAa[:, 0:2, :], dr["VA"][0:CT, :].rearrange("(k p) c -> p k c", p=128), writes=["VAa"])
        for k0 in range(0, 4 * NCL, 8):
            k1 = min(k0 + 8, 4 * NCL)
            P.dma(VAa[:, 2 + k0: 2 + k1, :], dr["VAg"][k0 * 128:k1 * 128, :].rearrange("(k p) c -> p k c", p=128),
                  reads=["VAa"], writes=["VAa"])
        dm = self.sb("dm", [128, 4, 128], F32)
        P.dma(dm[:], dr["dmask"], writes=["dm"])
        DT = self.sb("DT", [128, 4, 128], F32)
        dtmp = self.sb("dtmp", [128, 128], F32)
        for h in range(4):
            P.act(DT[:, h, :], dm[:, 0, :], AF.Exp, ["dm", "lg"], ["DT"], scale=lg[:, h:h + 1])
            P.tt("dve", DT[:, h, :], DT[:, h, :], dm[:, 1, :], ALU.mult, ["DT", "dm"], ["DT"])
            P.act(dtmp[:], dm[:, 2, :], AF.Exp, ["dm", "lg"], ["dtmp"], scale=lg[:, 4 + h:5 + h])
            P.tt("dve", dtmp[:], dtmp[:], dm[:, 3, :], ALU.mult, ["dtmp", "dm"], ["dtmp"])
            P.tt("dve", DT[:, h, :], DT[:, h, :], dtmp[:], ALU.add, ["DT", "dtmp"], ["DT"])
        xe = self.sb("xe", [128, 2, 128], F32)
        P.dma(xe[:], dr["xiexp"], writes=["xe"])
        XiT = self.sb("XiT", [128, 2, 2, 128], BF16)
        for d_ in range(2):
            for p in range(2):
                P.act(XiT[:, d_, p, :], xe[:, d_, :], AF.Exp, ["xe", "lgs"], ["XiT"], scale=lgs[:, d_, p:p + 1])
        sel = self.sb("sel", [128, 4], F32)
        P.dma(sel[:], dr["sel"], writes=["sel"])
        def ldc(name, src, shape, eng="sp"):
            b = self.sb(name, shape, BF16)
            P.dma(b[:], src, writes=[name], eng=eng)
            return b
        Amain = ldc("Amain", dr["Amain"], [128, 12, 128])
        Anb = ldc("Anb", dr["Anb"], [128, 8, 128])
        Actx = ldc("Actx", dr["Actx"], [128, 8, 128])
        Ahalo = ldc("Ahalo", dr["Ahalo"], [64, 8, 128])
        pw_ = ldc("poolw", dr["pool_w"], [64, 4, 64], eng="pool")
        pscale = self.sb("pscale", [128, 2], F32)
        P.dma(pscale[:], dr["pool_s"], writes=["pscale"])
        Hg = self.sb("Hg", [64, 256], BF16)
        P.dma(Hg[:], dr["Hg"], writes=["Hg"])

        bS = [self.ps("bS%d" % i, [128, 512], F32) for i in range(2)]
        bO = self.ps("bO", [128, 512], F32)
        bR = self.ps("bR", [128, 512], F32)
        bY = self.ps("bY", [128, 512], F32)
        bU = self.ps("bU", [128, 4, 128], F32)
        bM = self.ps("bM", [128, 512], F32)
        self.n_pT = self.ps("n_pT", [128, 8, 128], BF16)
        pT = self.n_pT

        NCc = NCL + 2
        Sb = self.sb("Sb", [128, NCc, 2, 2, 64], BF16)
        outer = self.st
        tmpstack = contextlib.ExitStack()
        self.st = tmpstack
        self.u_kz = self.sb("u_kz", [128, 2, 256], BF16)
        rk_all = self.sb("rk_all", [128, NCc, 256], BF16)
        rv_all = self.sb("rv_all", [128, NCc, 256], BF16)
        for k0 in range(0, NCc, 8):
            k1 = min(k0 + 8, NCc)
            P.dma(rk_all[:, k0:k1, :], dr["rk"][k0 * 128:k1 * 128, :].rearrange("(k p) c -> p k c", p=128), reads=["rk_all"], writes=["rk_all"])
            P.dma(rv_all[:, k0:k1, :], dr["rv"][k0 * 128:k1 * 128, :].rearrange("(k p) c -> p k c", p=128), reads=["rv_all"], writes=["rv_all"])
        Uall = self.sb("Uall", [128, NCc, 2, 2, 64], F32)
        for n in range(NCc):
            self.emit_U(rk_all[:, n, :], rv_all[:, n, :], ["rk_all", "rv_all"], bU, "bU")
            for d_ in range(2):
                for p in range(2):
                    for hl in range(2):
                        r = slice(hl * 64, (hl + 1) * 64)
                        eng = "dve"
                        P.cp(eng, Uall[r, n, d_, p, :], bU[r, d_ * 2 + p, hl * 64:(hl + 1) * 64], ["bU"], [("Uall", n)])
        gC = self.pw[:, :, :, 1]
        sctx = self.sb("sctx", [128, 2, 2, 64], F32)
        for p in range(2):
            P.stt("dve", sctx[:, 0, p, :], Uall[:, 0, 0, p, :], self.pw[:, 0, p, 1:2], Uall[:, 1, 0, p, :], ALU.mult, ALU.add,
                  [("Uall", 0), ("Uall", 1), "pw"], ["sctx"])
            P.stt("dve", sctx[:, 1, p, :], Uall[:, 1, 1, p, :], self.pw[:, 1, p, 1:2], Uall[:, 0, 1, p, :], ALU.mult, ALU.add,
                  [("Uall", 0), ("Uall", 1), "pw"], ["sctx"])
        Lg = self.sb("Lg", [128, 4, 2, 128], F32)
        P.dma(Lg[:], dr["Lg"].rearrange("(r d p) c -> p r d c", r=4, d=2, p=128), writes=["Lg"])
        G = self.sb("G", [128, 2, 4, 2, 64], F32)
        for p in range(2):
            P.cp("dve", G[:, 0, 0, p, :], sctx[:, 0, p, :], ["sctx"], ["G"])
            for i in range(1, 4):
                P.stt("dve", G[:, 0, i, p, :], G[:, 0, i - 1, p, :], self.pw[:, 0, p, NCL:NCL + 1], Lg[:, i - 1, 0, p * 64:(p + 1) * 64],
                      ALU.mult, ALU.add, ["G", "pw", "Lg"], ["G"])
            P.cp("dve", G[:, 1, 3, p, :], sctx[:, 1, p, :], ["sctx"], ["G"])
            for i in (2, 1, 0):
                P.stt("dve", G[:, 1, i, p, :], G[:, 1, i + 1, p, :], self.pw[:, 1, p, NCL:NCL + 1], Lg[:, i + 1, 1, p * 64:(p + 1) * 64],
                      ALU.mult, ALU.add, ["G", "pw", "Lg"], ["G"])
        Gown = self.sb("Gown", [128, 2, 2, 64], F32)
        for d_ in range(2):
            P.ts("dve", Gown[:, d_, :, :], G[:, d_, 0, :, :], sel[:, 0:1], None, ALU.mult, None, ["G", "sel"], ["Gown"])
            for i in range(1, 4):
                P.stt("dve", Gown[:, d_, :, :], G[:, d_, i, :, :], sel[:, i:i + 1], Gown[:, d_, :, :], ALU.mult, ALU.add,
                      ["G", "sel", "Gown"], ["Gown"])
        run = self.sb("run", [128, 2, 2, 64], F32)
        P.op("pool", lambda e: e.memset(Sb[:, 0:2, :, :, :], 0.0), (), [("Sb", 0), ("Sb", 1)])
        P.cp("dve", Sb[:, 1, 0, :, :], Uall[:, 0, 0, :, :], [("Uall", 0), ("Sb", 1)], [("Sb", 1)])
        P.cp("dve", Sb[:, 0, 1, :, :], Uall[:, 1, 1, :, :], [("Uall", 1), ("Sb", 0)], [("Sb", 0)])
        P.cp("dve", run[:, 0, :, :], Gown[:, 0, :, :], ["Gown"], ["run"])
        for n in range(NCL):
            P.cp("pool", Sb[:, 2 + n, 0, :, :], run[:, 0, :, :], ["run"], [("Sb", 2 + n)])
            if n < NCL - 1:
                for p in range(2):
                    P.stt("dve", run[:, 0, p, :], run[:, 0, p, :], self.pw[:, 0, p, 1:2], Uall[:, 2 + n, 0, p, :], ALU.mult, ALU.add,
                          ["run", "pw", ("Uall", 2 + n)], ["run"])
        P.cp("dve", run[:, 1, :, :], Gown[:, 1, :, :], ["Gown"], ["run"])
        for n in range(NCL - 1, -1, -1):
            P.cp("pool", Sb[:, 2 + n, 1, :, :], run[:, 1, :, :], ["run", ("Sb", 2 + n)], [("Sb", 2 + n)])
            if n > 0:
                for p in range(2):
                    P.stt("dve", run[:, 1, p, :], run[:, 1, p, :], self.pw[:, 1, p, 1:2], Uall[:, 2 + n, 1, p, :], ALU.mult, ALU.add,
                          ["run", "pw", ("Uall", 2 + n)], ["run"])

        P.barrier()
        tmpstack.close()
        self.st = outer

        QTb = [self.sb("QTb%d" % i, [128, 4, 512], BF16) for i in range(2)]
        rqb = [self.sb("rqb%d" % i, [128, 2, 512], BF16) for i in range(2)]
        rkb = [self.sb("rkb%d" % i, [128, 2, 512], BF16) for i in range(2)]
        rgb = [self.sb("rgb%d" % i, [128, 4, 256], BF16) for i in range(2)]
        rvb = [self.sb("rvb%d" % i, [128, 4, 256], BF16) for i in range(2)]
        ppb = [self.sb("ppb%d" % i, [128, 6, 256], BF16) for i in range(2)]
        PT = [self.sb("PT%d" % i, [128, 512], BF16) for i in range(3)]
        zeros = self.sb("zeros", [128, 260], BF16)
        P.op("pool", lambda e: e.memset(zeros[:], 0.0), (), ["zeros"])
        mix = [self.sb("mix%d" % i, [128, D], BF16) for i in range(2)]
        mixT = [self.sb("mixT%d" % i, [128, 8, 128], BF16) for i in range(2)]
        rden = self.sb("rden", [128, 4], F32)
        sT = self.sb("sT", [128, 4, 128], BF16)
        qx = self.sb("qx", [128, 2, 2, 128], BF16)
        osq = self.sb("osq", [128, 256], F32)
        ob = self.sb("ob", [128, 256], F32)
        st1 = self.sb("st1", [128, 4], F32)
        st2 = self.sb("st2", [128, 4], F32)
        mean = self.sb("mean", [128, 4], F32)
        yn = self.sb("yn", [128, 256], F32)
        sg = self.sb("sg", [128, 256], F32)
        mxd = self.sb("mxd", [64, 4, 128], BF16)
        xt = [self.sb("xt%d" % i, [128, D], F32) for i in range(2)]
        mjunk = self.sb("mjunk", [128, D], F32)
        mss = self.sb("mss", [128, 1], F32)
        mtmp = self.sb("mtmp", [128, D], F32)
        xo = [self.sb("xo%d" % i, [128, D], F32) for i in range(2)]

        ptc = [0]
        gti = 0
        if self.kb == 1:
            return
        for bi, (col0, ntok) in enumerate(self.blocks):
            isctx = col0 < CT
            if isctx and not self.need_ctx:
                continue
            nt = ntok // 128
            sset = 1 if isctx else 0
            s2 = bi % 2
            Qb, Qk = QTb[s2], "QTb%d" % s2
            P.dma(Qb[:, :, 0:ntok], dr["QT"][:, :, col0:col0 + ntok], writes=[Qk])
            P.dma(rqb[s2][:, :, 0:ntok], dr["rqT"][:, :, col0:col0 + ntok], writes=["rqb%d" % s2])
            P.dma(rkb[s2][:, :, 0:ntok], dr["rkT"][:, :, col0:col0 + ntok], writes=["rkb%d" % s2])
            P.dma(rgb[s2][:, 0:nt, :], dr["rg"][col0:col0 + ntok, :].rearrange("(k p) c -> p k c", p=128), writes=["rgb%d" % s2])
            P.dma(rvb[s2][:, 0:nt, :], dr["rv"][col0:col0 + ntok, :].rearrange("(k p) c -> p k c", p=128), writes=["rvb%d" % s2])
            lo_t = col0 - 128 if (col0 > CT or (isctx and col0 > 0)) else col0
            hi_t = col0 + ntok + 128
            if isctx:
                hi_t = min(hi_t, CT)
            else:
                hi_t = min(hi_t, Tc)
            k0 = (lo_t - (col0 - 128)) // 128
            nk = (hi_t - lo_t) // 128
            P.dma(ppb[s2][:, k0:k0 + nk, :], dr["pp"][lo_t:hi_t, :].rearrange("(k p) c -> p k c", p=128), writes=["ppb%d" % s2])
            kts = list(range(2)) if isctx else list(range(NKT))
            for t in range(nt):
                tcol = col0 + t * 128
                mx_, mxk = mix[gti % 2], "mix%d" % (gti % 2)
                for kvh in range(2):
                    r = slice(kvh * 64, (kvh + 1) * 64)
                    P.mm(bO[:, 0:260], zeros[:, 0:128], zeros[:, 0:260], True, True, ["zeros"], ["bO"])
                    def issue_S(ki_):
                        kt_ = kts[ki_]
                        S_ = bS[ptc[0] % 2]
                        Sk = "bS%d" % (ptc[0] % 2)
                        Pt_, Pk_ = PT[ptc[0] % 3], "PT%d" % (ptc[0] % 3)
                        ptc[0] += 1
                        P.mm(S_[:, :].rearrange("p (g q) -> p g q", g=4), KTa[r, kt_ * 128:(kt_ + 1) * 128],
                             Qb[r, :, t * 128:(t + 1) * 128], True, True, ["KTa", Qk], [Sk])
                        P.op("act", lambda e, Pt_=Pt_, S_=S_: e.activation(Pt_[:], S_[:], AF.Exp, scale=0.125),
                             [Sk], [Pk_], nosame=True)
                        return Pt_, Pk_
                    cur = issue_S(0)
                    for ki, kt in enumerate(kts):
                        nxt = issue_S(ki + 1) if ki + 1 < len(kts) else None
                        Pt, Pk = cur
                        for g in range(4):
                            P.mm(bO[:, g * 65:(g + 1) * 65], Pt[:, g * 128:(g + 1) * 128], VAa[:, kt, kvh * 66:kvh * 66 + 65],
                                 False, ki == len(kts) - 1, [Pk, "VAa"], ["bO"], skip_group_check=True)
                        cur = nxt
                    for g in range(4):
                        P.op("dve", lambda e, g=g: e.reciprocal(rden[:, g:g + 1], bO[:, g * 65 + 64: g * 65 + 65]), ["bO"], ["rden"])
                    for g in range(4):
                        h = kvh * 4 + g
                        P.ts("dve", mx_[:, h * 64:(h + 1) * 64], bO[:, g * 65: g * 65 + 64], rden[:, g:g + 1], None, ALU.mult, None,
                             ["bO", "rden"], [mxk])
                if self.kb == 2:
                    continue
                n = tcol // 128
                for d_ in range(2):
                    for p in range(2):
                        P.tt("pool", qx[:, d_, p, :], rqb[s2][:, p, t * 128:(t + 1) * 128], XiT[:, d_, p, :], ALU.mult,
                             ["rqb%d" % s2, "XiT"], ["qx"])
                bUf = bU[:, :, :].rearrange("p a b -> p (a b)")
                for h in range(4):
                    p, hl = h // 2, h % 2
                    r = slice(hl * 64, (hl + 1) * 64)
                    bank, bkey = (bR, "bR") if hl == 0 else (bUf, "bU")
                    P.mm(bank[:, p * 128:(p + 1) * 128], rkb[s2][r, p, t * 128:(t + 1) * 128], rqb[s2][r, p, t * 128:(t + 1) * 128],
                         True, True, ["rkb%d" % s2, "rqb%d" % s2], [bkey])
                for h in range(4):
                    p, hl = h // 2, h % 2
                    bank, bkey = (bR, "bR") if hl == 0 else (bUf, "bU")
                    P.tt("dve", sT[:, h, :], bank[:, p * 128:(p + 1) * 128], DT[:, h, :], ALU.mult, [bkey, "DT"], ["sT"])
                for h in range(4):
                    p, hl = h // 2, h % 2
                    r = slice(hl * 64, (hl + 1) * 64)
                    if hl == 0:
                        oreg, okey = bY[:, p * 64:(p + 1) * 64], "bY"
                    else:
                        oreg, okey = bO[:, 320 + p * 64: 320 + (p + 1) * 64], "bO"
                    P.mm(oreg, sT[:, h, :], rvb[s2][:, t, h * 64:(h + 1) * 64], True, False,
                         ["sT", "rvb%d" % s2], [okey], skip_group_check=True)
                    P.mm(oreg, qx[r, 0, p, :], Sb[r, n, 0, p, :], False, False,
                         ["qx", ("Sb", n)], [okey], skip_group_check=True)
                    P.mm(oreg, qx[r, 1, p, :], Sb[r, n, 1, p, :], False, True,
                         ["qx", ("Sb", n)], [okey], skip_group_check=True)
                ob4 = ob[:].rearrange("p (a b e) -> p a b e", a=2, b=2)
                P.cp("dve", ob4[:, :, 0, :], bY[:, 0:128].rearrange("p (a e) -> p a e", a=2), ["bY"], ["ob"])
                P.cp("dve", ob4[:, :, 1, :], bO[:, 320:448].rearrange("p (a e) -> p a e", a=2), ["bO"], ["ob"])
                P.op("dve", lambda e: e.reduce_sum(st1[:], ob[:].rearrange("p (h e) -> p h e", h=4), AX.X), ["ob"], ["st1"])
                P.act(osq[:], ob[:], AF.Square, ["ob"], ["osq"])
                P.op("dve", lambda e: e.reduce_sum(st2[:], osq[:].rearrange("p (h e) -> p h e", h=4), AX.X), ["osq"], ["st2"])
                P.ts("dve", mean[:], st1[:], 1.0 / 64, None, ALU.mult, None, ["st1"], ["mean"])
                P.tt("dve", st1[:], mean[:], mean[:], ALU.mult, ["mean"], ["st1"])
                P.stt("dve", st2[:], st2[:], 1.0 / 64, st1[:], ALU.mult, ALU.subtract, ["st2", "st1"], ["st2"])
                P.act(st2[:], st2[:], AF.Sqrt, ["st2"], ["st2"], bias=EPS)
                P.op("dve", lambda e: e.reciprocal(st2[:], st2[:]), ["st2"], ["st2"])
                for h in range(4):
                    P.ts("dve", yn[:, h * 64:(h + 1) * 64], ob[:, h * 64:(h + 1) * 64], mean[:, h:h + 1], st2[:, h:h + 1],
                         ALU.subtract, ALU.mult, ["ob", "mean", "st2"], ["yn"])
                P.act(sg[:], rgb[s2][:, t, :], AF.Silu, ["rgb%d" % s2], ["sg"])
                P.tt("pool", mx_[:, 512:768], yn[:], sg[:], ALU.mult, ["yn", "sg"], [mxk])
                if self.kb == 3:
                    continue
                mT_, mTk = mixT[gti % 2], "mixT%d" % (gti % 2)
                for c in range(6):
                    P.tr(pT[:, c, :], mx_[:, c * 128:(c + 1) * 128], self.identb[:], [mxk, "identb"], ["n_pT"])
                P.cp("act", mT_[:, 0:6, :], pT[:, 0:6, :], ["n_pT"], [mTk])
                first = (not isctx and tcol == CT) or (isctx and tcol == 0)
                last = (not isctx and tcol == Tc - 128) or (isctx and tcol == CT - 128)
                for g in range(4):
                    gs = slice(g * 64, (g + 1) * 64)
                    if isctx:
                        Am = Actx[:, (0 if first else 4) + g, :]
                    else:
                        Am = Amain[:, (0 if first else (8 if last else 4)) + g, :]
                    ops_ = [(ppb[s2][:, t + 1, gs], Am)]
                    if not first:
                        ops_.append((ppb[s2][:, t, gs], Anb[:, g, :]))
                    elif not isctx:
                        ops_.append((Hg[:, gs], Ahalo[:, g, :]))
                    if not last:
                        ops_.append((ppb[s2][:, t + 2, gs], Anb[:, 4 + g, :]))
                    elif not isctx:
                        ops_.append((Hg[:, gs], Ahalo[:, 4 + g, :]))
                    for oi, (l_, r_) in enumerate(ops_):
                        P.mm(bM[0:64, g * 128:(g + 1) * 128], l_, r_, oi == 0, oi == len(ops_) - 1,
                             ["ppb%d" % s2, "Amain", "Anb", "Actx", "Ahalo", "Hg"], ["bM"], skip_group_check=True)
                P.cp("act", mxd[:].rearrange("p g t -> p (g t)"), bM[0:64, :], ["bM"], ["mxd"])
                for g in range(4):
                    P.mm(bY[(g % 2) * 64:(g % 2 + 1) * 64, 256 + (g // 2) * 128: 256 + (g // 2 + 1) * 128], pw_[:, g, :], mxd[:, g, :],
                         True, True, ["poolw", "mxd"], ["bYp"], skip_group_check=True)
                for c in range(2):
                    P.ts("dve", mT_[:, 6 + c, :], bY[:, 256 + c * 128: 256 + (c + 1) * 128], pscale[:, c:c + 1], None, ALU.mult, None,
                         ["bYp", "pscale"], [mTk])
                if self.kb == 4:
                    continue
                for hf in range(2):
                    for c in range(8):
                        P.mm(bS[hf][:, :], mT_[:, c, :], wo[:, c, hf * 512:(hf + 1) * 512], c == 0, c == 7, [mTk, "wo"], ["bS%d" % hf])
                x_, xk = xt[gti % 2], "xt%d" % (gti % 2)
                P.dma(x_[:], self.src_x(tcol, 128), writes=[xk])
                P.act(mjunk[:, 0:512], bS[0][:, :], AF.Square, ["bS0"], ["mjunk", "mss"], accum_out=mss[:])
                mss2 = st1[:, 0:1]
                P.act(mjunk[:, 512:1024], bS[1][:, :], AF.Square, ["bS1"], ["mjunk", "st1"], accum_out=mss2)
                P.tt("dve", mss[:], mss[:], mss2, ALU.add, ["mss", "st1"], ["mss"])
                P.ts("dve", mss[:], mss[:], 1.0 / D, EPS, ALU.mult, ALU.add, ["mss"], ["mss"])
                P.act(mss[:], mss[:], AF.Sqrt, ["mss"], ["mss"])
                P.op("dve", lambda e: e.reciprocal(mss[:], mss[:]), ["mss"], ["mss"])
                for hf in range(2):
                    P.stt("dve", mtmp[:, hf * 512:(hf + 1) * 512], bS[hf][:, :], mss[:, 0:1], self.gv[:, sset, hf * 512:(hf + 1) * 512],
                          ALU.mult, ALU.mult, ["bS%d" % hf, "mss", "gv"], ["mtmp"])
                xo_, xok = xo[gti % 2], "xo%d" % (gti % 2)
                P.tt("pool", xo_[:], mtmp[:], x_[:], ALU.add, ["mtmp", xk], [xok])
                dst = dr["cout"][tcol:tcol + 128, :] if isctx else dr["xout"][tcol - CT: tcol - CT + 128, :]
                P.dma(dst, xo_[:], reads=[xok], eng="pool")
                gti += 1

    def phase_B2(self):
        P, dr = self.P, self.dr
        E = self.n_exp
        self.alloc_norm()
        blocks = [b for b in self.blocks if not (b[0] < CT and not self.need_ctx)]
        xt = [self.sb("xt%d" % i, [128, D], F32) for i in range(2)]
        hT = [self.sb("hT0", [128, 8, 512], BF16)] * 2
        w1 = self.sb("w1", [128, 8, DFF], BF16)
        w3 = self.sb("w3", [128, 8, DFF], BF16)
        w2 = self.sb("w2", [128, NF, D], BF16)
        gT = self.sb("gT", [128, NF, 512], BF16)
        sa = self.sb("sa", [128, 512], F32)
        pa = [self.ps("pa%d" % i, [128, 512], F32) for i in range(2)]
        pb = [self.ps("pb%d" % i, [128, 512], F32) for i in range(2)]
        py = [self.ps("py%d" % i, [128, 512], F32) for i in range(2)]
        pr = self.ps("pr", [128, 8], F32)
        ntl = self.Tc // 128
        gates = self.sb("gates", [128, ntl, 8], F32)
        if E > 1:
            rtf = self.sb("rtf", [128, 8, 8], F32)
            rtb = self.sb("rtb", [128, 8, 8], BF16)
            P.dma(rtf[:], dr["router"].rearrange("(c p) e -> p c e", p=128), writes=["rtf"])
            P.cp("dve", rtb[:], rtf[:], ["rtf"], ["rtb"])
            lgt = self.sb("lgt", [128, 8], F32)
            m8 = self.sb("m8", [128, 8], F32)
            msk = self.sb("msk", [128, 8], F32)
            ex = self.sb("ex", [128, 8], F32)
            den = self.sb("den", [128, 1], F32)
        ti = 0
        for bi, (col0, ntok) in enumerate(blocks):
            nt = ntok // 128
            hb, hk = hT[0], "hT0"
            sset = 1 if col0 < CT else 0
            for t in range(nt):
                x_, xk = xt[ti % 2], "xt%d" % (ti % 2)
                src = dr["cout"][col0 + t * 128: col0 + (t + 1) * 128, :] if col0 < CT else \
                    dr["xout"][col0 - CT + t * 128: col0 - CT + (t + 1) * 128, :]
                P.dma(x_[:], src, writes=[xk])
                self.norm_tile_to_hT(x_[:], xk, hb, hk, t * 128, sset, 2, ti)
                ti += 1
                if E > 1:
                    tl = (col0 + t * 128) // 128
                    for c in range(8):
                        P.mm(pr[:, :], hb[:, c, t * 128:(t + 1) * 128], rtb[:, c, :], c == 0, c == 7, [hk, "rtb"], ["pr"])
                    P.cp("dve", lgt[:], pr[:, :], ["pr"], ["lgt"])
                    P.op("dve", lambda e: e.reduce_max(m8[:, 0:1], lgt[:], AX.X), ["lgt"], ["m8"])
                    P.ts("dve", msk[:], lgt[:], m8[:, 0:1], None, ALU.is_equal, None, ["lgt", "m8"], ["msk"])
                    P.stt("dve", ex[:], msk[:], -1e30, lgt[:], ALU.mult, ALU.add, ["msk", "lgt"], ["ex"])
                    P.op("dve", lambda e: e.reduce_max(m8[:, 1:2], ex[:], AX.X), ["ex", "m8"], ["m8"])
                    P.ts("dve", msk[:], lgt[:], m8[:, 1:2], None, ALU.is_ge, None, ["lgt", "m8"], ["msk"])
                    P.ts("dve", ex[:], lgt[:], m8[:, 0:1], None, ALU.subtract, None, ["lgt", "m8"], ["ex"])
                    P.act(ex[:], ex[:], AF.Exp, ["ex"], ["ex"])
                    P.tt("dve", ex[:], ex[:], msk[:], ALU.mult, ["ex", "msk"], ["ex"])
                    P.op("dve", lambda e: e.reduce_sum(den[:], ex[:], AX.X), ["ex"], ["den"])
                    P.op("dve", lambda e: e.reciprocal(den[:], den[:]), ["den"], ["den"])
                    P.ts("dve", gates[:, tl, :], ex[:], den[:, 0:1], None, ALU.mult, None, ["ex", "den"], ["gates"])
            P.dma(dr["hTd"][bi, :, :, 0:ntok], hb[:, :, 0:ntok], reads=[hk], writes=[("hTd", bi)], eng="pool")
        yo = [self.sb("yo%d" % i, [128, D], F32) for i in range(2)]
        ya = [self.sb("ya0", [128, D], F32)] * 2
        yi = 0
        fi = 0
        for e_ in range(E):
            for c in range(8):
                P.dma(w1[:, c, :], dr["w1"][e_, c * 128:(c + 1) * 128, :], reads=["w1"], writes=["w1"], eng="pool")
                P.dma(w3[:, c, :], dr["w3"][e_, c * 128:(c + 1) * 128, :], reads=["w3"], writes=["w3"], eng="pool")
            for f in range(NF):
                P.dma(w2[:, f, :], dr["w2"][e_, f * 128:(f + 1) * 128, :], reads=["w2"], writes=["w2"], eng="pool")
            for bi, (col0, ntok) in enumerate(blocks):
                nt = ntok // 128
                hb, hk = hT[0], "hT0"
                P.dma(hb[:, :, 0:ntok], dr["hTd"][bi, :, :, 0:ntok], writes=[hk], reads=[("hTd", bi)])
                for f in range(NF):
                    a_, ak = pa[fi % 2], "pa%d" % (fi % 2)
                    b_, bk = pb[fi % 2], "pb%d" % (fi % 2)
                    fi += 1
                    for c in range(8):
                        P.mm(a_[:, 0:ntok], w1[:, c, f * 128:(f + 1) * 128], hb[:, c, 0:ntok], c == 0, c == 7, ["w1", hk], [ak])
                    for c in range(8):
                        P.mm(b_[:, 0:ntok], w3[:, c, f * 128:(f + 1) * 128], hb[:, c, 0:ntok], c == 0, c == 7, ["w3", hk], [bk])
                    P.act(sa[:, 0:ntok], a_[:, 0:ntok], AF.Silu, [ak], ["sa"])
                    P.tt("dve", gT[:, f, 0:ntok], sa[:, 0:ntok], b_[:, 0:ntok], ALU.mult, ["sa", bk], ["gT"])
                for t in range(nt):
                    tcol = col0 + t * 128
                    tl = tcol // 128
                    for hf in range(2):
                        for f in range(NF):
                            P.mm(py[hf][:, :], gT[:, f, t * 128:(t + 1) * 128], w2[:, f, hf * 512:(hf + 1) * 512], f == 0, f == NF - 1,
                                 ["gT", "w2"], ["py%d" % hf])
                    yo_, yok = yo[yi % 2], "yo%d" % (yi % 2)
                    ya_, yak = ya[0], "ya0"
                    yi += 1
                    acc = dr["accd"][tcol:tcol + 128, :]
                    if E == 1:
                        for hf in range(2):
                            P.cp("act" if hf == 0 else "dve", yo_[:, hf * 512:(hf + 1) * 512], py[hf][:, :], ["py%d" % hf], [yok])
                    else:
                        if e_ > 0:
                            P.dma(ya_[:], acc, writes=[yak], reads=[("acc", tl)])
                        for hf in range(2):
                            if e_ == 0:
                                P.ts("dve", yo_[:, hf * 512:(hf + 1) * 512], py[hf][:, :], gates[:, tl, e_:e_ + 1], None, ALU.mult, None,
                                     ["py%d" % hf, "gates"], [yok])
                            else:
                                P.stt("dve", yo_[:, hf * 512:(hf + 1) * 512], py[hf][:, :], gates[:, tl, e_:e_ + 1],
                                      ya_[:, hf * 512:(hf + 1) * 512], ALU.mult, ALU.add, ["py%d" % hf, "gates", yak], [yok])
                    P.dma(acc, yo_[:], reads=[yok], writes=[("acc", tl)], eng="pool")
        mss = self.sb("fss", [128, 1], F32)
        ti = 0
        for bi, (col0, ntok) in enumerate(blocks):
            sset = 1 if col0 < CT else 0
            for t in range(ntok // 128):
                tcol = col0 + t * 128
                tl = tcol // 128
                x_, xk = xt[ti % 2], "xt%d" % (ti % 2)
                ya_, yak = ya[0], "ya0"
                yo_, yok = yo[ti % 2], "yo%d" % (ti % 2)
                ti += 1
                dst = dr["cout"][tcol:tcol + 128, :] if col0 < CT else dr["xout"][tcol - CT: tcol - CT + 128, :]
                P.dma(x_[:], dst, writes=[xk], reads=[("xres", tl)])
                P.dma(ya_[:], dr["accd"][tcol:tcol + 128, :], writes=[yak], reads=[("acc", tl)])
                P.act(self.n_junk[:], ya_[:], AF.Square, [yak], ["n_junk", "fss"], accum_out=mss[:])
                P.ts("dve", mss[:], mss[:], 1.0 / D, EPS, ALU.mult, ALU.add, ["fss"], ["fss"])
                P.act(mss[:], mss[:], AF.Sqrt, ["fss"], ["fss"])
                P.op("dve", lambda e: e.reciprocal(mss[:], mss[:]), ["fss"], ["fss"])
                P.stt("dve", yo_[:], ya_[:], mss[:, 0:1], self.gv[:, sset, :], ALU.mult, ALU.mult, [yak, "fss", "gv"], [yok])
                P.tt("pool", yo_[:], yo_[:], x_[:], ALU.add, [yok, xk], [yok])
                P.dma(dst, yo_[:], reads=[yok], writes=[("xres", tl)], eng="pool")


def _pool_weight(L, w, s, t):
    lo = np.clip(t - w // 2, 0, L)
    hi = np.clip(t + w // 2, 0, L)
    val = ((s >= lo) & (s < hi)).astype(np.float64) / (hi - lo)
    val = val - (s == t)
    return val


def host_consts(T, j):
    Tl = T // 4
    Tc = CT + Tl
    NCL = Tl // 128
    cst = {}
    cst["ident"] = np.eye(128, dtype=np.float32)
    bo = np.zeros((128, 128), np.float32)
    bo[0:64, 0:64] = 1
    bo[64:, 64:] = 1
    cst["bones"] = bo
    tpos = np.arange(j * Tl, (j + 1) * Tl)
    row = (tpos // 64).astype(np.float32)
    colp = (tpos % 64).astype(np.float32)
    inv = (np.float32(10000.0) ** (-np.arange(16, dtype=np.float32) / np.float32(16))).astype(np.float32)
    cosT = np.ones((64, Tc), np.float32)
    sinT = np.zeros((64, Tc), np.float32)
    for a, pos in enumerate((row, colp)):
        ang = pos[None, :] * inv[:, None]
        for b in range(2):
            rows = slice(a * 32 + b * 16, a * 32 + b * 16 + 16)
            cosT[rows, CT:] = np.cos(ang)
            sinT[rows, CT:] = np.sin(ang) * (-1.0 if b == 0 else 1.0)
    cst["cosT"] = np.concatenate([cosT, cosT], 0)
    cst["sinT"] = np.concatenate([sinT, sinT], 0)
    m = np.arange(128, dtype=np.float32)
    cst["zexp"] = np.stack([C - 1 - m, m], 1).astype(np.float32)
    cst["nC"] = np.tile((np.arange(NCL + 2, dtype=np.float32) * C)[None, :], (128, 1)).astype(np.float32)
    mm, cc = np.meshgrid(m, m, indexing="ij")
    dmask = np.stack([np.maximum(cc - mm, 0), (cc >= mm).astype(np.float32),
                      np.maximum(mm - cc, 0), (mm >= cc).astype(np.float32)], 1).astype(np.float32)
    cst["dmask"] = dmask
    cst["xiexp"] = np.tile(np.stack([m + 1, C - m], 0)[None], (128, 1, 1)).astype(np.float32)
    sel = np.zeros((128, 4), np.float32)
    sel[:, j] = 1
    cst["sel"] = sel
    wins = (2, 4, 8, 16)
    s_loc = np.arange(128)[:, None]
    t_loc = np.arange(128)[None, :]
    Amain = np.zeros((128, 12, 128), np.float32)
    Anb = np.zeros((128, 8, 128), np.float32)
    Actx = np.zeros((128, 8, 128), np.float32)
    Ahalo = np.zeros((64, 8, 128), np.float32)
    for g, w in enumerate(wins):
        base_f = j * Tl
        Amain[:, 0 + g, :] = _pool_weight(T, w, base_f + s_loc, base_f + t_loc)
        mid = T // 2 // 128 * 128 if T >= 512 else 128
        midb = 128 * (T // 256)
        Amain[:, 4 + g, :] = _pool_weight(T + 4096, w, 2048 + s_loc, 2048 + t_loc)
        base_l = (j + 1) * Tl - 128
        Amain[:, 8 + g, :] = _pool_weight(T, w, base_l + s_loc, base_l + t_loc)
        Anb[:, g, :] = _pool_weight(T + 4096, w, 2048 - 128 + s_loc, 2048 + t_loc)
        Anb[:, 4 + g, :] = _pool_weight(T + 4096, w, 2048 + 128 + s_loc, 2048 + t_loc)
        Actx[:, g, :] = _pool_weight(CT, w, s_loc, t_loc)
        Actx[:, 4 + g, :] = _pool_weight(CT, w, 128 + s_loc, 128 + t_loc)
        for i in range(4):
            for r in range(16):
                spos = i * Tl + r if r < 8 else (i + 1) * Tl - 16 + r
                if i == j:
                    continue
                Ahalo[i * 16 + r, g, :] = _pool_weight(T, w, np.array([[spos]]), base_f + t_loc)[0]
                Ahalo[i * 16 + r, 4 + g, :] = _pool_weight(T, w, np.array([[spos]]), base_l + t_loc)[0]
    cst["Amain"], cst["Anb"], cst["Actx"], cst["Ahalo"] = [a.astype(NPBF) for a in (Amain, Anb, Actx, Ahalo)]
    return cst


def _rope_perm():
    perm = np.zeros(64, np.int64)
    for a in range(2):
        for b in range(2):
            for f in range(16):
                perm[a * 32 + b * 16 + f] = a * 32 + (1 - b) * 16 + f
    return perm


def layer_weights(inp, i):
    perm = _rope_perm()
    w_in = inp["w_in"][i]
    cols = {"aq": 0, "ak": 512, "av": 640, "rq": 768, "rk": 1024, "rv": 1280, "rg": 1536, "pp": 1792}
    fm = []
    for g in range(4):
        fm += [cols["aq"] + g * 64 + d for d in range(64)] + [cols["aq"] + (4 + g) * 64 + d for d in range(64)]
    for g in range(4):
        fm += [cols["aq"] + g * 64 + perm[d] for d in range(64)] + [cols["aq"] + (4 + g) * 64 + perm[d] for d in range(64)]
    fm += [cols["ak"] + d for d in range(128)]
    fm += [cols["ak"] + kv * 64 + perm[d] for kv in range(2) for d in range(64)]
    fm += [cols["rq"] + d for d in range(256)]
    fm += [cols["rk"] + d for d in range(256)]
    tm = [cols["av"] + d for d in range(128)] + [cols["rk"] + d for d in range(256)] + [cols["rv"] + d for d in range(256)] + \
         [cols["rg"] + d for d in range(256)] + [cols["pp"] + d for d in range(256)]
    gq, gk = inp["q_norm"][i], inp["k_norm"][i]
    gains = np.stack([np.tile(gq, 2), np.tile(gq[perm], 2), np.tile(gk, 2), np.tile(gk[perm], 2)], 1).astype(np.float32)
    ps = inp["pool_scale"][i]
    out = {
        "w_fm": np.ascontiguousarray(w_in[:, fm]),
        "w_tm": np.ascontiguousarray(w_in[:, tm]),
        "gains": gains,
        "w_mod": inp["w_mod"][i], "b_mod": inp["b_mod"][i],
        "norms": np.stack([inp["norm_pre_mix"][i], inp["norm_post_mix"][i], inp["norm_pre_ffn"][i], inp["norm_post_ffn"][i]], 0),
        "decay": np.ascontiguousarray(inp["ret_decay_logit"][i].reshape(8)),
        "w_out": inp["w_out"][i],
        "pool_w": np.ascontiguousarray(np.transpose(inp["pool_w"][i], (1, 0, 2))),
        "pool_s": np.ascontiguousarray(ps.reshape(2, 128).T),
    }
    if i % 2 == 0:
        out["w1"], out["w3"], out["w2"] = inp["ffn_w1"][i // 2][None], inp["ffn_w3"][i // 2][None], inp["ffn_w2"][i // 2][None]
    else:
        out["w1"], out["w3"], out["w2"] = inp["moe_w1"][i // 2], inp["moe_w3"][i // 2], inp["moe_w2"][i // 2]
        out["router"] = inp["moe_router"][i // 2]
    return out


_NC_CACHE = {}


def get_nc(T, layer, phases, n_exp):
    key = (T, layer, phases, n_exp)
    if key not in _NC_CACHE:
        b = Builder(T, layer, phases, n_exp)
        nc = b.build()
        _NC_CACHE[key] = (nc, sorted(k for k in b.dr))
    return _NC_CACHE[key][0]


A_IN = ["xin", "cin", "cvec", "w_mod", "b_mod", "norms", "decay", "ident", "zexp", "nC", "w_fm", "w_tm", "gains", "bones",
        "cosT", "sinT"]
A_OUT = ["QT", "KT", "VA", "rqT", "rkT", "rk", "rv", "rg", "pp", "Lout"]
B_IN = ["xin", "cin", "cvec", "w_mod", "b_mod", "norms", "decay", "ident", "zexp", "nC", "QT", "KT", "VA", "rqT", "rkT", "rk",
        "rv", "rg", "pp", "KTg", "VAg", "Lg", "Hg", "w_out", "dmask", "xiexp", "sel", "Amain", "Anb", "Actx", "Ahalo", "pool_w",
        "pool_s", "w1", "w3", "w2"]


def run_model(inp, n_layers=2):
    x = np.asarray(inp["x"], np.float32)
    B, T, _ = x.shape
    Tl = T // 4
    ncore = 8
    xs = [np.ascontiguousarray(x[c // 4, (c % 4) * Tl:((c % 4) + 1) * Tl]) for c in range(ncore)]
    cs = [np.ascontiguousarray(np.asarray(inp["ctx"], np.float32)[c // 4]) for c in range(ncore)]
    csts = [host_consts(T, c % 4) for c in range(ncore)]
    cvecs = [np.stack([inp["c"][c // 4], inp["c_ctx"]], 0).astype(np.float32) for c in range(ncore)]
    for i in range(n_layers):
        lw = layer_weights(inp, i)
        n_exp = 1 if i % 2 == 0 else NE
        ncA = get_nc(T, i, "A", n_exp)
        maps = []
        for c in range(ncore):
            m = {"xin": xs[c], "cin": cs[c], "cvec": cvecs[c]}
            m.update(lw)
            m.update(csts[c])
            maps.append({k: np.ascontiguousarray(m[k]) for k in A_IN})
        resA = run_bass_kernel_spmd(ncA, maps, core_ids=list(range(ncore))).results
        ncB = get_nc(T, i, "B", n_exp)
        maps = []
        for c in range(ncore):
            b = c // 4
            grp = [resA[b * 4 + r] for r in range(4)]
            m = {"xin": xs[c], "cin": cs[c], "cvec": cvecs[c]}
            m.update(lw)
            m.update(csts[c])
            for k in A_OUT:
                m[k] = resA[c][k]
            m["KTg"] = np.concatenate([g["KT"][:, CT:] for g in grp], 0)
            m["VAg"] = np.concatenate([g["VA"][CT:] for g in grp], 0)
            m["Lg"] = np.concatenate([g["Lout"] for g in grp], 0)
            m["Hg"] = np.concatenate([np.concatenate([g["pp"][CT:CT + 8], g["pp"][-8:]], 0) for g in grp], 0)
            names = list(B_IN) + (["router"] if n_exp > 1 else [])
            maps.append({k: np.ascontiguousarray(m[k]) for k in names})
        resB = run_bass_kernel_spmd(ncB, maps, core_ids=list(range(ncore))).results
        xs = [resB[c]["xout"] for c in range(ncore)]
        if i == 0:
            cs = [resB[c]["cout"] for c in range(ncore)]
    out = np.zeros((B, T, D), np.float32)
    for c in range(ncore):
        out[c // 4, (c % 4) * Tl:((c % 4) + 1) * Tl] = xs[c]
    return out


def kernel(**inputs):
    inp = {k: np.asarray(v) for k, v in inputs.items()}
    return run_model(inp, 2)
```

```python
import contextlib
import numpy as np
import ml_dtypes
import concourse.bass as bass
import concourse.mybir as mybir
from concourse.bass_utils import run_bass_kernel_spmd

F32 = mybir.dt.float32
BF16 = mybir.dt.bfloat16
AF = mybir.ActivationFunctionType
ALU = mybir.AluOpType
AX = mybir.AxisListType
NPBF = ml_dtypes.bfloat16

D = 1024
CT = 256
DFF = 2816
NF = DFF // 128
NE = 8
EPS = 1e-6
C = 128

ENGS = ["pe", "act", "dve", "pool", "sp"]
NDMA = 8
SAME_ENG_SYNC = {"pe": False, "act": True, "dve": True, "pool": True, "sp": False}


class Prog:
    def __init__(self, nc, stack):
        self.nc = nc
        self.ops = {e: [] for e in ENGS}
        self.count = {e: 0 for e in ENGS}
        self.waited = {e: {} for e in ENGS}
        self.last_w = {}
        self.readers = {}
        self.dma_n = {e: 0 for e in ENGS}
        self.sems = {}
        for e in ENGS:
            self.sems[("c", e)] = stack.enter_context(nc.semaphore("c_" + e))
        for e in ("sp", "pool", "act"):
            for i in range(NDMA):
                self.sems[("d", e, i)] = stack.enter_context(nc.semaphore("d_%s%d" % (e, i)))
        self.semval = {k: 0 for k in self.sems}
        self.nops = 0

    def op(self, eng, fn, reads=(), writes=(), dma=False, nosame=False):
        deps = {}

        def add(d):
            if d is None:
                return
            sk, v = d
            if deps.get(sk, 0) < v:
                deps[sk] = v

        for k in reads:
            add(self.last_w.get(k))
        for k in writes:
            add(self.last_w.get(k))
            for sk, v in self.readers.get(k, {}).items():
                add((sk, v))
        if dma:
            n = self.dma_n[eng]
            idx, use = n % NDMA, n // NDMA
            semkey = ("d", eng, idx)
            val = 16 * (use + 1)
            if use > 0:
                add((semkey, 16 * use))
            self.dma_n[eng] += 1
            inc = 16
        else:
            self.count[eng] += 1
            semkey = ("c", eng)
            val = self.count[eng]
            inc = 1
        self.semval[semkey] = val
        for sk, v in deps.items():
            if sk == ("c", eng) and (nosame or not SAME_ENG_SYNC[eng]):
                continue
            if self.waited[eng].get(sk, 0) >= v:
                continue
            self.waited[eng][sk] = v
            s = self.sems[sk]
            self.ops[eng].append(lambda e, s=s, v=v: e.wait_ge(s, v))
        s = self.sems[semkey]
        self.ops[eng].append(lambda e, s=s, inc=inc: fn(e).then_inc(s, inc))
        me = (semkey, val)
        for k in reads:
            r = self.readers.setdefault(k, {})
            if r.get(semkey, 0) < val:
                r[semkey] = val
        for k in writes:
            self.last_w[k] = me
            self.readers[k] = {}
        self.nops += 1

    def new_phase(self, stack):
        self.phase_id = getattr(self, "phase_id", 0) + 1
        for e in ENGS:
            k = ("c", e)
            self.sems[k] = stack.enter_context(self.nc.semaphore("c%d_%s" % (self.phase_id, e)))
            self.semval[k] = 0
            self.count[e] = 0
            for w in self.waited.values():
                w.pop(k, None)

    def barrier(self):
        for e in ENGS:
            for sk, v in self.semval.items():
                if v == 0:
                    continue
                if sk == ("c", e) and e in ("pe", "sp"):
                    continue
                if self.waited[e].get(sk, 0) >= v:
                    continue
                self.waited[e][sk] = v
                s = self.sems[sk]
                self.ops[e].append(lambda en, s=s, v=v: en.wait_ge(s, v))
        self.last_w = {}
        self.readers = {}

    def emit(self):
        nc = self.nc
        ops = self.ops
        with nc.Block() as block:
            @block.tensor
            def _(e):
                for f in ops["pe"]:
                    f(e)

            @block.scalar
            def _(e):
                for f in ops["act"]:
                    f(e)

            @block.vector
            def _(e):
                for f in ops["dve"]:
                    f(e)

            @block.gpsimd
            def _(e):
                for f in ops["pool"]:
                    f(e)

            @block.sync
            def _(e):
                for f in ops["sp"]:
                    f(e)
        self.ops = {e: [] for e in ENGS}

    def dma(self, out, in_, reads=(), writes=(), eng="sp", **kw):
        self.op(eng, lambda e: e.dma_start(out=out, in_=in_, **kw), reads, writes, dma=True)

    def mm(self, out, lhsT, rhs, start, stop, reads=(), writes=(), **kw):
        self.op("pe", lambda e: e.matmul(out, lhsT, rhs, start=start, stop=stop, **kw), reads, writes)

    def tr(self, out, in_, ident, reads=(), writes=()):
        self.op("pe", lambda e: e.transpose(out, in_, ident), reads, writes)

    def act(self, out, in_, func, reads=(), writes=(), **kw):
        self.op("act", lambda e: e.activation(out, in_, func, **kw), reads, writes)

    def ts(self, eng, out, in0, s1, s2, op0, op1=None, reads=(), writes=()):
        if op1 is None:
            self.op(eng, lambda e: e.tensor_scalar(out, in0, s1, None, op0), reads, writes)
        else:
            self.op(eng, lambda e: e.tensor_scalar(out, in0, s1, s2, op0, op1), reads, writes)

    def tt(self, eng, out, in0, in1, op, reads=(), writes=()):
        self.op(eng, lambda e: e.tensor_tensor(out, in0, in1, op), reads, writes)

    def stt(self, eng, out, in0, scalar, in1, op0, op1, reads=(), writes=()):
        self.op(eng, lambda e: e.scalar_tensor_tensor(out, in0, scalar, in1, op0, op1), reads, writes)

    def cp(self, eng, out, in_, reads=(), writes=()):
        if eng == "act":
            self.op(eng, lambda e: e.copy(out, in_), reads, writes)
        else:
            self.op(eng, lambda e: e.tensor_copy(out, in_), reads, writes)


class Builder:
    def __init__(self, T, layer, phases, n_exp):
        self.T = T
        self.Tl = T // 4
        self.Tc = CT + self.Tl
        self.NCL = self.Tl // 128
        self.NKT = (CT + T) // 128
        self.layer = layer
        self.phases = phases
        self.n_exp = n_exp
        self.need_ctx = layer == 0
        self.nc = bass.Bass("TRN2", target_bir_lowering=False)
        self.dr = {}
        self.blocks = [(0, CT)] + [(CT + 512 * i, 512) for i in range(self.Tl // 512)]

    def din(self, name, shape, dt):
        self.dr[name] = self.nc.dram_tensor(name, list(shape), dt, kind="ExternalInput").ap()
        return self.dr[name]

    def dout(self, name, shape, dt):
        self.dr[name] = self.nc.dram_tensor(name, list(shape), dt, kind="ExternalOutput").ap()
        return self.dr[name]

    def build(self):
        nc = self.nc
        with contextlib.ExitStack() as gst:
            self.P = Prog(nc, gst)
            if "A" in self.phases:
                self.declare_A()
            if "B" in self.phases:
                self.declare_B()
            if "A" in self.phases:
                with contextlib.ExitStack() as st:
                    self.st = st
                    self.common_consts()
                    self.emit_mod()
                    import os as _os
                    self.kstop = int(_os.environ.get("KSTOP", "0"))
                    if self.kstop != 1:
                        self.phase_A()
                    self.P.barrier()
                    self.P.emit()
            if "B" in self.phases:
                with contextlib.ExitStack() as st:
                    self.st = st
                    self.common_consts()
                    self.emit_mod()
                    import os as _os
                    self.kb = int(_os.environ.get("KSTOPB", "0"))
                    self.phase_B1()
                    self.P.barrier()
                    self.P.emit()
                    self.P.new_phase(gst)
                with contextlib.ExitStack() as st:
                    self.st = st
                    self.common_consts()
                    self.emit_mod(only_ffn=True)
                    if self.kb in (0, 6, 7):
                        self.phase_B2()
                    self.P.barrier()
                    self.P.emit()
        return nc

    def sb(self, name, shape, dt):
        self.uid = getattr(self, "uid", 0) + 1
        return self.st.enter_context(self.nc.sbuf_tensor("s%d_%s" % (self.uid, name), list(shape), dt))

    def ps(self, name, shape, dt):
        self.uid = getattr(self, "uid", 0) + 1
        return self.st.enter_context(self.nc.psum_tensor("p%d_%s" % (self.uid, name), list(shape), dt))

    def declare_common(self):
        if "xin" in self.dr:
            return
        Tl, Tc = self.Tl, self.Tc
        self.din("xin", [Tl, D], F32)
        self.din("cin", [CT, D], F32)
        self.din("cvec", [2, D], F32)
        self.din("w_mod", [D, 6 * D], F32)
        self.din("b_mod", [6 * D], F32)
        self.din("norms", [4, D], F32)
        self.din("decay", [8], F32)
        self.din("ident", [128, 128], F32)
        self.din("zexp", [128, 2], F32)
        self.din("nC", [128, self.NCL + 2], F32)

    def declare_A(self):
        self.declare_common()
        Tl, Tc = self.Tl, self.Tc
        self.din("w_fm", [D, 1792], F32)
        self.din("w_tm", [D, 1152], F32)
        self.din("gains", [128, 4], F32)
        self.din("bones", [128, 128], F32)
        self.din("cosT", [128, Tc], F32)
        self.din("sinT", [128, Tc], F32)
        o = self.dout if "B" not in self.phases else self.dint
        o("QT", [128, 4, Tc], BF16)
        o("KT", [128, Tc], BF16)
        o("VA", [Tc, 132], BF16)
        o("rqT", [128, 2, Tc], BF16)
        o("rkT", [128, 2, Tc], BF16)
        o("rk", [Tc, 256], BF16)
        o("rv", [Tc, 256], BF16)
        o("rg", [Tc, 256], BF16)
        o("pp", [Tc, 256], BF16)
        o("Lout", [256, 128], F32)

    def declare_B(self):
        self.declare_common()
        Tl, Tc, T = self.Tl, self.Tc, self.T
        if "A" not in self.phases:
            i = self.din
            i("QT", [128, 4, Tc], BF16)
            i("KT", [128, Tc], BF16)
            i("VA", [Tc, 132], BF16)
            i("rqT", [128, 2, Tc], BF16)
            i("rkT", [128, 2, Tc], BF16)
            i("rk", [Tc, 256], BF16)
            i("rv", [Tc, 256], BF16)
            i("rg", [Tc, 256], BF16)
            i("pp", [Tc, 256], BF16)
            i("KTg", [4 * 128, Tl], BF16)
            i("VAg", [4 * Tl, 132], BF16)
            i("Lg", [4 * 256, 128], F32)
            i("Hg", [64, 256], BF16)
        self.din("w_out", [D, D], F32)
        self.din("dmask", [128, 4, 128], F32)
        self.din("xiexp", [128, 2, 128], F32)
        self.din("sel", [128, 4], F32)
        self.din("Amain", [128, 12, 128], BF16)
        self.din("Anb", [128, 8, 128], BF16)
        self.din("Actx", [128, 8, 128], BF16)
        self.din("Ahalo", [64, 8, 128], BF16)
        self.din("pool_w", [64, 4, 64], F32)
        self.din("pool_s", [128, 2], F32)
        E = self.n_exp
        self.din("w1", [E, D, DFF], F32)
        self.din("w3", [E, D, DFF], F32)
        self.din("w2", [E, DFF, D], F32)
        if E > 1:
            self.din("router", [D, NE], F32)
        self.dout("xout", [Tl, D], F32)
        if self.need_ctx:
            self.dout("cout", [CT, D], F32)
        self.dr["hTd"] = self.nc.dram_tensor("hTd", [len(self.blocks), 128, 8, 512], BF16, kind="Internal").ap()
        self.dr["accd"] = self.nc.dram_tensor("accd", [self.Tc, D], F32, kind="Internal").ap()

    def dint(self, name, shape, dt):
        self.dr[name] = self.nc.dram_tensor(name, list(shape), dt, kind="Internal").ap()
        return self.dr[name]

    def common_consts(self):
        P, dr = self.P, self.dr
        self.identf = self.sb("identf", [128, 128], F32)
        self.identb = self.sb("identb", [128, 128], BF16)
        P.dma(self.identf[:], dr["ident"], writes=["identf"])
        P.cp("dve", self.identb[:], self.identf[:], ["identf"], ["identb"])

    def emit_mod(self, only_ffn=False):
        P, dr = self.P, self.dr
        self.modT = self.sb("modT", [128, 2, 4, 8], F32)
        self.gv = self.sb("gv", [128, 2, D], F32)
        if "modd" not in dr:
            dr["modd"] = self.nc.dram_tensor("modd", [2, 6 * D], F32, kind="Internal").ap()
        outer = self.st
        with contextlib.ExitStack() as tmp:
            self.st = tmp
            cv = self.sb("cv", [128, 2, 8], F32)
            scv = self.sb("scv", [128, 2, 8], F32)
            for s_ in range(2):
                P.dma(cv[:, s_, :], dr["cvec"][s_].rearrange("(c p) -> p c", p=128), reads=["cv"], writes=["cv"],
                      allow_slow_non_contiguous=True)
            P.act(scv[:], cv[:], AF.Silu, ["cv"], ["scv"])
            modrow = self.sb("modrow", [2, 6 * D], F32)
            brow = self.sb("brow", [2, 6 * D], F32)
            P.dma(brow[:], dr["b_mod"].partition_broadcast(2), writes=["brow"])
            wm = [self.sb("wm%d" % i, [128, 8, 512], F32) for i in range(2)]
            pm = self.ps("pm", [128, 512], F32)
            for j in range(12):
                w = wm[j % 2]
                P.dma(w[:], dr["w_mod"][:, j * 512:(j + 1) * 512].rearrange("(c p) n -> p c n", p=128),
                      writes=["wm%d" % (j % 2)])
                for c in range(8):
                    P.mm(pm[0:2, :], scv[:, :, c], w[:, c, :], c == 0, c == 7, ["scv", "wm%d" % (j % 2)], ["pm"])
                P.tt("dve", modrow[:, j * 512:(j + 1) * 512], pm[0:2, :], brow[:, j * 512:(j + 1) * 512], ALU.add,
                     ["pm", "brow"], ["modrow"])
            P.dma(dr["modd"], modrow[:], reads=["modrow"], writes=["modd"])
            nT = self.sb("nT", [128, 4, 8], F32)
            for k_ in range(4):
                P.dma(nT[:, k_, :], dr["norms"][k_].rearrange("(c p) -> p c", p=128), reads=["nT"], writes=["nT"],
                      allow_slow_non_contiguous=True)
            mT = self.sb("mT", [128, 6, 8, 2], F32)
            pmt = self.ps("pmt", [128, 48, 2], F32)
            for v in range(6):
                for c in range(8):
                    P.tr(pmt[:, v * 8 + c, :], modrow[0:2, v * D + c * 128: v * D + (c + 1) * 128], self.identf[0:2, 0:2],
                         ["modrow", "identf"], ["pmt"])
            P.cp("dve", mT[:].rearrange("p v c s -> p (v c s)"), pmt[:].rearrange("p a s -> p (a s)"), ["pmt"], ["mT"])
            for s_ in range(2):
                P.stt("dve", self.modT[:, s_, 0, :], mT[:, 1, :, s_], 1.0, nT[:, 0, :], ALU.add, ALU.mult, ["mT", "nT"], ["modT"])
                P.cp("dve", self.modT[:, s_, 1, :], mT[:, 0, :, s_], ["mT"], ["modT"])
                P.stt("dve", self.modT[:, s_, 2, :], mT[:, 4, :, s_], 1.0, nT[:, 2, :], ALU.add, ALU.mult, ["mT", "nT"], ["modT"])
                P.cp("dve", self.modT[:, s_, 3, :], mT[:, 3, :, s_], ["mT"], ["modT"])
            nb = self.sb("nb", [128, D], F32)
            P.dma(nb[:], dr["norms"][3 if only_ffn else 1].partition_broadcast(128), writes=["nb"])
            v = 5 if only_ffn else 2
            for s_ in range(2):
                P.dma(self.gv[:, s_, :], dr["modd"][s_, v * D:(v + 1) * D].partition_broadcast(128), reads=["modd", "gv"], writes=["gv"])
            for s_ in range(2):
                P.tt("dve", self.gv[:, s_, :], self.gv[:, s_, :], nb[:], ALU.mult, ["gv", "nb"], ["gv"])
            P.barrier()
        self.st = outer

    def norm_tile_to_hT(self, xt, xkey, hT, hkey, col, sset, v, tagi):
        P = self.P
        junk, ss, xs, pT = self.n_junk, self.n_ss[tagi % 2], self.n_xs[tagi % 2], self.n_pT
        ssk, xsk = "n_ss%d" % (tagi % 2), "n_xs%d" % (tagi % 2)
        P.act(junk[:], xt, AF.Square, [xkey], ["n_junk", ssk], accum_out=ss[:])
        P.ts("dve", ss[:], ss[:], 1.0 / D, EPS, ALU.mult, ALU.add, [ssk], [ssk])
        P.act(ss[:], ss[:], AF.Sqrt, [ssk], [ssk])
        P.op("dve", lambda e: e.reciprocal(ss[:], ss[:]), [ssk], [ssk])
        P.ts("dve", xs[:], xt, ss[:, 0:1], None, ALU.mult, None, [xkey, ssk], [xsk])
        for c in range(8):
            P.tr(pT[:, c, :], xs[:, c * 128:(c + 1) * 128], self.identb[:], [xsk, "identb"], ["n_pT"])
        for c in range(8):
            eng = "dve" if c % 2 == 0 else "pool"
            if eng == "pool":
                eng = "dve"
            P.ts(eng, hT[:, c, col:col + 128], pT[:, c, :], self.modT[:, sset, v, c:c + 1], self.modT[:, sset, v + 1, c:c + 1],
                 ALU.mult, ALU.add, ["n_pT", "modT"], [hkey])

    def alloc_norm(self):
        self.n_junk = self.sb("n_junk", [128, D], BF16)
        self.n_ss = [self.sb("n_ss%d" % i, [128, 1], F32) for i in range(2)]
        self.n_xs = [self.sb("n_xs%d" % i, [128, D], BF16) for i in range(2)]
        self.n_pT = self.ps("n_pT", [128, 8, 128], BF16)

    def src_x(self, col, n):
        if col < CT:
            return self.dr["cin"][col:col + n, :]
        return self.dr["xin"][col - CT:col - CT + n, :]

    def ret_consts(self):
        P, dr = self.P, self.dr
        dl = self.sb("dl", [128, 8], F32)
        P.dma(dl[:], dr["decay"].partition_broadcast(128), writes=["dl"])
        lg = self.sb("lg", [128, 8], F32)
        P.act(lg[:], dl[:], AF.Exp, ["dl"], ["lg"], scale=-1.0)
        P.act(lg[:], lg[:], AF.Ln, ["lg"], ["lg"], bias=1.0)
        P.ts("dve", lg[:], lg[:], -1.0, None, ALU.mult, None, ["lg"], ["lg"])
        self.lg = lg
        lgs = self.sb("lgs", [128, 2, 2], F32)
        for d_ in range(2):
            for p in range(2):
                for hl in range(2):
                    P.cp("dve", lgs[hl * 64:(hl + 1) * 64, d_, p:p + 1], lg[hl * 64:(hl + 1) * 64, d_ * 4 + 2 * p + hl: d_ * 4 + 2 * p + hl + 1],
                         ["lg"], ["lgs"])
        self.lgs = lgs
        zx = self.sb("zx", [128, 2], F32)
        P.dma(zx[:], dr["zexp"], writes=["zx"])
        nCt = self.sb("nCt", [128, self.NCL + 2], F32)
        P.dma(nCt[:], dr["nC"], writes=["nCt"])
        zt = self.sb("zt", [128, 8], F32)
        for d_ in range(2):
            for h in range(4):
                P.act(zt[:, d_ * 4 + h: d_ * 4 + h + 1], zx[:, d_:d_ + 1], AF.Exp, ["zx", "lg"], ["zt"],
                      scale=lg[:, d_ * 4 + h: d_ * 4 + h + 1])
        ones = self.sb("ones64", [128, 64], F32)
        P.op("pool", lambda e: e.memset(ones[:], 1.0), (), ["ones64"])
        self.Z = self.sb("Z", [128, 2, 256], F32)
        for d_ in range(2):
            for h in range(4):
                P.ts("dve", self.Z[:, d_, h * 64:(h + 1) * 64], ones[:], zt[:, d_ * 4 + h: d_ * 4 + h + 1], None, ALU.mult, None,
                     ["ones64", "zt"], ["Z"])
        self.pw = self.sb("pw", [128, 2, 2, self.NCL + 2], F32)
        for d_ in range(2):
            for p in range(2):
                P.act(self.pw[:, d_, p, :], nCt[:], AF.Exp, ["nCt", "lgs"], ["pw"], scale=lgs[:, d_, p:p + 1])

    def emit_U(self, rk_t, rv_t, keys, pU, tag):
        P = self.P
        kz = self.u_kz
        for d_ in range(2):
            P.tt("dve", kz[:, d_, :], rk_t, self.Z[:, d_, :], ALU.mult, keys + ["Z"], ["u_kz"])
        for d_ in range(2):
            for p in range(2):
                P.mm(pU[:, d_ * 2 + p, :], kz[:, d_, p * 128:(p + 1) * 128], rv_t[:, p * 128:(p + 1) * 128], True, True,
                     ["u_kz"] + keys, [tag])

    def phase_A(self):
        P, dr = self.P, self.dr
        Tc = self.Tc
        self.alloc_norm()
        self.ret_consts()
        wfm = self.sb("wfm", [128, 8, 1792], BF16)
        wtm = self.sb("wtm", [128, 8, 1152], BF16)
        for c in range(8):
            P.dma(wfm[:, c, :], dr["w_fm"][c * 128:(c + 1) * 128, :], writes=["wfm"], reads=["wfm"], eng="pool")
            P.dma(wtm[:, c, :], dr["w_tm"][c * 128:(c + 1) * 128, :], writes=["wtm"], reads=["wtm"], eng="pool")
        gains = self.sb("gains", [128, 4], F32)
        P.dma(gains[:], dr["gains"], writes=["gains"])
        bonesf = self.sb("bonesf", [128, 128], F32)
        bones = self.sb("bones", [128, 128], BF16)
        P.dma(bonesf[:], dr["bones"], writes=["bonesf"])
        P.cp("dve", bones[:], bonesf[:], ["bonesf"], ["bones"])
        cosT = self.sb("cosT", [128, Tc], F32)
        sinT = self.sb("sinT", [128, Tc], F32)
        P.dma(cosT[:], dr["cosT"], writes=["cosT"])
        P.dma(sinT[:], dr["sinT"], writes=["sinT"])
        self.u_kz = self.sb("u_kz", [128, 2, 256], BF16)
        Lacc = self.sb("Lacc", [128, 2, 2, 64], F32)
        P.op("pool", lambda e: e.memset(Lacc[:], 0.0), (), ["Lacc"])

        xt = [self.sb("xt%d" % i, [128, D], F32) for i in range(2)]
        hT = [self.sb("hT%d" % i, [128, 8, 512], BF16) for i in range(2)]
        pa = self.ps("pa", [128, 512], F32)
        pb = self.ps("pb", [128, 512], F32)
        pss = self.ps("pss", [128, 512], F32)
        ptm = [self.ps("ptm%d" % i, [128, 512], F32) for i in range(2)]
        pU = self.ps("pU", [128, 4, 128], F32)
        sq = self.sb("sq", [128, 512], BF16)
        rr = self.sb("rr", [128, 512], F32)
        t1 = self.sb("t1", [128, 512], F32)
        t2 = self.sb("t2", [128, 512], F32)
        qo = [self.sb("qo%d" % i, [128, 4, 512], BF16) for i in range(2)]
        ko = [self.sb("ko%d" % i, [128, 512], BF16) for i in range(2)]
        rqo = [self.sb("rqo%d" % i, [128, 2, 512], BF16) for i in range(2)]
        rko = [self.sb("rko%d" % i, [128, 2, 512], BF16) for i in range(2)]
        va = [self.sb("va%d" % i, [128, 132], BF16) for i in range(2)]
        tko = [self.sb("tko%d" % i, [128, 256], BF16) for i in range(2)]
        tvo = [self.sb("tvo%d" % i, [128, 256], BF16) for i in range(2)]
        tgo = [self.sb("tgo%d" % i, [128, 256], BF16) for i in range(2)]
        tpo = [self.sb("tpo%d" % i, [128, 256], BF16) for i in range(2)]
        for i in range(2):
            P.op("pool", lambda e, i=i: e.memset(va[i][:], 1.0), (), ["va%d" % i])

        ti = 0
        if self.kstop == 2:
            return
        for bi, (col0, ntok) in enumerate(self.blocks):
            nt = ntok // 128
            hb = hT[bi % 2]
            hk = "hT%d" % (bi % 2)
            sset = 1 if col0 < CT else 0
            if self.kstop == 3 and bi > 0:
                return
            for t in range(nt):
                x_ = xt[ti % 2]
                xk = "xt%d" % (ti % 2)
                P.dma(x_[:], self.src_x(col0 + t * 128, 128), writes=[xk])
                self.norm_tile_to_hT(x_[:], xk, hb, hk, t * 128, sset, 0, ti)
                ti += 1
            if self.kstop == 4:
                return
            qb_, qk_ = qo[bi % 2], "qo%d" % (bi % 2)
            kb_, kk_ = ko[bi % 2], "ko%d" % (bi % 2)

            def fm(ps_, m):
                for c in range(8):
                    P.mm(ps_[:, 0:ntok], wfm[:, c, m * 128:(m + 1) * 128], hb[:, c, 0:ntok], c == 0, c == 7,
                         ["wfm", hk], [ps_.name if hasattr(ps_, "name") else "px"])

            for g in range(5):
                ma, mb = (g, g + 4) if g < 4 else (8, 9)
                gcol = 0 if g < 4 else 2
                for c in range(8):
                    P.mm(pa[:, 0:ntok], wfm[:, c, ma * 128:(ma + 1) * 128], hb[:, c, 0:ntok], c == 0, c == 7, ["wfm", hk], ["pa"])
                for c in range(8):
                    P.mm(pb[:, 0:ntok], wfm[:, c, mb * 128:(mb + 1) * 128], hb[:, c, 0:ntok], c == 0, c == 7, ["wfm", hk], ["pb"])
                P.act(sq[:, 0:ntok], pa[:, 0:ntok], AF.Square, ["pa"], ["sq"])
                P.mm(pss[:, 0:ntok], bones[:], sq[:, 0:ntok], True, True, ["bones", "sq"], ["pss"])
                P.act(rr[:, 0:ntok], pss[:, 0:ntok], AF.Sqrt, ["pss"], ["rr"], scale=1.0 / 64, bias=EPS)
                P.op("dve", lambda e, n=ntok: e.reciprocal(rr[:, 0:n], rr[:, 0:n]), ["rr"], ["rr"])
                P.stt("dve", t1[:, 0:ntok], pa[:, 0:ntok], gains[:, gcol:gcol + 1], cosT[:, col0:col0 + ntok], ALU.mult, ALU.mult,
                      ["pa", "gains", "cosT"], ["t1"])
                P.stt("dve", t2[:, 0:ntok], pb[:, 0:ntok], gains[:, gcol + 1:gcol + 2], sinT[:, col0:col0 + ntok], ALU.mult, ALU.mult,
                      ["pb", "gains", "sinT"], ["t2"])
                P.tt("pool", t1[:, 0:ntok], t1[:, 0:ntok], t2[:, 0:ntok], ALU.add, ["t1", "t2"], ["t1"])
                if g < 4:
                    P.tt("pool", qb_[:, g, 0:ntok], t1[:, 0:ntok], rr[:, 0:ntok], ALU.mult, ["t1", "rr"], [qk_])
                else:
                    P.tt("pool", kb_[:, 0:ntok], t1[:, 0:ntok], rr[:, 0:ntok], ALU.mult, ["t1", "rr"], [kk_])
            if self.kstop == 5:
                return
            P.dma(dr["QT"][:, :, col0:col0 + ntok], qb_[:, :, 0:ntok], reads=[qk_])
            P.dma(dr["KT"][:, col0:col0 + ntok], kb_[:, 0:ntok], reads=[kk_])
            rq_, rqk = rqo[bi % 2], "rqo%d" % (bi % 2)
            rk_, rkk = rko[bi % 2], "rko%d" % (bi % 2)
            for p in range(2):
                for c in range(8):
                    P.mm(pa[:, 0:ntok], wfm[:, c, (10 + p) * 128:(11 + p) * 128], hb[:, c, 0:ntok], c == 0, c == 7, ["wfm", hk], ["pa"])
                P.cp("act", rq_[:, p, 0:ntok], pa[:, 0:ntok], ["pa"], [rqk])
                for c in range(8):
                    P.mm(pb[:, 0:ntok], wfm[:, c, (12 + p) * 128:(13 + p) * 128], hb[:, c, 0:ntok], c == 0, c == 7, ["wfm", hk], ["pb"])
                P.ts("dve", rk_[:, p, 0:ntok], pb[:, 0:ntok], 0.125, None, ALU.mult, None, ["pb"], [rkk])
            P.dma(dr["rqT"][:, :, col0:col0 + ntok], rq_[:, :, 0:ntok], reads=[rqk])
            P.dma(dr["rkT"][:, :, col0:col0 + ntok], rk_[:, :, 0:ntok], reads=[rkk])
            if self.kstop == 6:
                return
            for t in range(nt):
                tcol = col0 + t * 128
                s = (bi * 4 + t) % 2
                p0 = ptm[0]
                for c in range(8):
                    P.mm(p0[:, 0:384], hb[:, c, t * 128:(t + 1) * 128], wtm[:, c, 0:384], c == 0, c == 7, [hk, "wtm"], ["ptm0"])
                if self.kstop == 9:
                    continue
                P.cp("dve", va[s][:, 0:64], p0[:, 0:64], ["ptm0"], ["va%d" % s])
                P.cp("dve", va[s][:, 66:130], p0[:, 64:128], ["ptm0"], ["va%d" % s])
                P.ts("dve", tko[s][:], p0[:, 128:384], 0.125, None, ALU.mult, None, ["ptm0"], ["tko%d" % s])
                if self.kstop == 8:
                    continue
                p1 = ptm[1]
                for c in range(8):
                    P.mm(p1[:, 0:512], hb[:, c, t * 128:(t + 1) * 128], wtm[:, c, 384:896], c == 0, c == 7, [hk, "wtm"], ["ptm1"])
                P.cp("dve", tvo[s][:], p1[:, 0:256], ["ptm1"], ["tvo%d" % s])
                P.cp("dve", tgo[s][:], p1[:, 256:512], ["ptm1"], ["tgo%d" % s])
                for c in range(8):
                    P.mm(p0[:, 0:256], hb[:, c, t * 128:(t + 1) * 128], wtm[:, c, 896:1152], c == 0, c == 7, [hk, "wtm"], ["ptm0"])
                P.cp("dve", tpo[s][:], p0[:, 0:256], ["ptm0"], ["tpo%d" % s])
                if self.kstop == 7:
                    continue
                P.dma(dr["VA"][tcol:tcol + 128, :], va[s][:], reads=["va%d" % s], eng="sp")
                P.dma(dr["rk"][tcol:tcol + 128, :], tko[s][:], reads=["tko%d" % s], eng="sp")
                P.dma(dr["rv"][tcol:tcol + 128, :], tvo[s][:], reads=["tvo%d" % s], eng="sp")
                P.dma(dr["rg"][tcol:tcol + 128, :], tgo[s][:], reads=["tgo%d" % s], eng="sp")
                P.dma(dr["pp"][tcol:tcol + 128, :], tpo[s][:], reads=["tpo%d" % s], eng="sp")
                if col0 >= CT and self.kstop != 10:
                    n = (tcol - CT) // 128
                    if self.kstop == 12:
                        continue
                    self.emit_U(tko[s][:], tvo[s][:], ["tko%d" % s, "tvo%d" % s], pU, "pU")
                    for d_ in range(2):
                        pwi = (self.NCL - 1 - n) if d_ == 0 else n
                        for p in range(2):
                            for hl in range(2):
                                r = slice(hl * 64, (hl + 1) * 64)
                                P.stt("dve", Lacc[r, d_, p, :], pU[r, d_ * 2 + p, hl * 64:(hl + 1) * 64],
                                      self.pw[r, d_, p, pwi:pwi + 1], Lacc[r, d_, p, :], ALU.mult, ALU.add,
                                      ["pU", "pw", "Lacc"], ["Lacc"])
        for d_ in range(2):
            P.dma(dr["Lout"][d_ * 128:(d_ + 1) * 128, :], Lacc[:, d_, :, :].rearrange("p a e -> p (a e)"), reads=["Lacc"])

    def phase_B1(self):
        P, dr = self.P, self.dr
        T, Tl, Tc, NCL, NKT = self.T, self.Tl, self.Tc, self.NCL, self.NKT
        self.ret_consts()
        lg, lgs = self.lg, self.lgs
        wo = self.sb("wo", [128, 8, D], BF16)
        for c in range(8):
            P.dma(wo[:, c, :], dr["w_out"][c * 128:(c + 1) * 128, :], writes=["wo"], reads=["wo"], eng="pool")
        KTa = self.sb("KTa", [128, CT + T], BF16)
        VAa = self.sb("VAa", [128, NKT, 132], BF16)
        P.dma(KTa[:, 0:CT], dr["KT"][:, 0:CT], writes=["KTa"])
        for r in range(4):
            P.dma(KTa[:, CT + r * Tl: CT + (r + 1) * Tl], dr["KTg"][r * 128:(r + 1) * 128, :], reads=["KTa"], writes=["KTa"])
        P.dma(VAa[:, 0:2, :], dr["VA"][0:CT, :].rearrange("(k p) c -> p k c", p=128), writes=["VAa"])
        for k0 in range(0, 4 * NCL, 8):
            k1 = min(k0 + 8, 4 * NCL)
            P.dma(VAa[:, 2 + k0: 2 + k1, :], dr["VAg"][k0 * 128:k1 * 128, :].rearrange("(k p) c -> p k c", p=128),
                  reads=["VAa"], writes=["VAa"])
        dm = self.sb("dm", [128, 4, 128], F32)
        P.dma(dm[:], dr["dmask"], writes=["dm"])
        DT = self.sb("DT", [128, 4, 128], F32)
        dtmp = self.sb("dtmp", [128, 128], F32)
        for h in range(4):
            P.act(DT[:, h, :], dm[:, 0, :], AF.Exp, ["dm", "lg"], ["DT"], scale=lg[:, h:h + 1])
            P.tt("dve", DT[:, h, :], DT[:, h, :], dm[:, 1, :], ALU.mult, ["DT", "dm"], ["DT"])
            P.act(dtmp[:], dm[:, 2, :], AF.Exp, ["dm", "lg"], ["dtmp"], scale=lg[:, 4 + h:5 + h])
            P.tt("dve", dtmp[:], dtmp[:], dm[:, 3, :], ALU.mult, ["dtmp", "dm"], ["dtmp"])
            P.tt("dve", DT[:, h, :], DT[:, h, :], dtmp[:], ALU.add, ["DT", "dtmp"], ["DT"])
        xe = self.sb("xe", [128, 2, 128], F32)
        P.dma(xe[:], dr["xiexp"], writes=["xe"])
        XiT = self.sb("XiT", [128, 2, 2, 128], BF16)
        for d_ in range(2):
            for p in range(2):
                P.act(XiT[:, d_, p, :], xe[:, d_, :], AF.Exp, ["xe", "lgs"], ["XiT"], scale=lgs[:, d_, p:p + 1])
        sel = self.sb("sel", [128, 4], F32)
        P.dma(sel[:], dr["sel"], writes=["sel"])
        def ldc(name, src, shape, eng="sp"):
            b = self.sb(name, shape, BF16)
            P.dma(b[:], src, writes=[name], eng=eng)
            return b
        Amain = ldc("Amain", dr["Amain"], [128, 12, 128])
        Anb = ldc("Anb", dr["Anb"], [128, 8, 128])
        Actx = ldc("Actx", dr["Actx"], [128, 8, 128])
        Ahalo = ldc("Ahalo", dr["Ahalo"], [64, 8, 128])
        pw_ = ldc("poolw", dr["pool_w"], [64, 4, 64], eng="pool")
        pscale = self.sb("pscale", [128, 2], F32)
        P.dma(pscale[:], dr["pool_s"], writes=["pscale"])
        Hg = self.sb("Hg", [64, 256], BF16)
        P.dma(Hg[:], dr["Hg"], writes=["Hg"])

        bS = [self.ps("bS%d" % i, [128, 512], F32) for i in range(3)]
        bO = self.ps("bO", [128, 512], F32)
        bR = self.ps("bR", [128, 512], F32)
        bY = self.ps("bY", [128, 512], F32)
        bU = self.ps("bU", [128, 4, 128], F32)
        self.n_pT = self.ps("n_pT", [128, 8, 128], BF16)
        pT = self.n_pT

        NCc = NCL + 2
        Sb = self.sb("Sb", [128, NCc, 2, 2, 64], BF16)
        outer = self.st
        tmpstack = contextlib.ExitStack()
        self.st = tmpstack
        self.u_kz = self.sb("u_kz", [128, 2, 256], BF16)
        rk_all = self.sb("rk_all", [128, NCc, 256], BF16)
        rv_all = self.sb("rv_all", [128, NCc, 256], BF16)
        for k0 in range(0, NCc, 8):
            k1 = min(k0 + 8, NCc)
            P.dma(rk_all[:, k0:k1, :], dr["rk"][k0 * 128:k1 * 128, :].rearrange("(k p) c -> p k c", p=128), reads=["rk_all"], writes=["rk_all"])
            P.dma(rv_all[:, k0:k1, :], dr["rv"][k0 * 128:k1 * 128, :].rearrange("(k p) c -> p k c", p=128), reads=["rv_all"], writes=["rv_all"])
        Uall = self.sb("Uall", [128, NCc, 2, 2, 64], F32)
        for n in range(NCc):
            self.emit_U(rk_all[:, n, :], rv_all[:, n, :], ["rk_all", "rv_all"], bU, "bU")
            for d_ in range(2):
                for p in range(2):
                    for hl in range(2):
                        r = slice(hl * 64, (hl + 1) * 64)
                        eng = "dve"
                        P.cp(eng, Uall[r, n, d_, p, :], bU[r, d_ * 2 + p, hl * 64:(hl + 1) * 64], ["bU"], [("Uall", n)])
        gC = self.pw[:, :, :, 1]
        sctx = self.sb("sctx", [128, 2, 2, 64], F32)
        for p in range(2):
            P.stt("dve", sctx[:, 0, p, :], Uall[:, 0, 0, p, :], self.pw[:, 0, p, 1:2], Uall[:, 1, 0, p, :], ALU.mult, ALU.add,
                  [("Uall", 0), ("Uall", 1), "pw"], ["sctx"])
            P.stt("dve", sctx[:, 1, p, :], Uall[:, 1, 1, p, :], self.pw[:, 1, p, 1:2], Uall[:, 0, 1, p, :], ALU.mult, ALU.add,
                  [("Uall", 0), ("Uall", 1), "pw"], ["sctx"])
        Lg = self.sb("Lg", [128, 4, 2, 128], F32)
        P.dma(Lg[:], dr["Lg"].rearrange("(r d p) c -> p r d c", r=4, d=2, p=128), writes=["Lg"])
        G = self.sb("G", [128, 2, 4, 2, 64], F32)
        for p in range(2):
            P.cp("dve", G[:, 0, 0, p, :], sctx[:, 0, p, :], ["sctx"], ["G"])
            for i in range(1, 4):
                P.stt("dve", G[:, 0, i, p, :], G[:, 0, i - 1, p, :], self.pw[:, 0, p, NCL:NCL + 1], Lg[:, i - 1, 0, p * 64:(p + 1) * 64],
                      ALU.mult, ALU.add, ["G", "pw", "Lg"], ["G"])
            P.cp("dve", G[:, 1, 3, p, :], sctx[:, 1, p, :], ["sctx"], ["G"])
            for i in (2, 1, 0):
                P.stt("dve", G[:, 1, i, p, :], G[:, 1, i + 1, p, :], self.pw[:, 1, p, NCL:NCL + 1], Lg[:, i + 1, 1, p * 64:(p + 1) * 64],
                      ALU.mult, ALU.add, ["G", "pw", "Lg"], ["G"])
        Gown = self.sb("Gown", [128, 2, 2, 64], F32)
        for d_ in range(2):
            P.ts("dve", Gown[:, d_, :, :], G[:, d_, 0, :, :], sel[:, 0:1], None, ALU.mult, None, ["G", "sel"], ["Gown"])
            for i in range(1, 4):
                P.stt("dve", Gown[:, d_, :, :], G[:, d_, i, :, :], sel[:, i:i + 1], Gown[:, d_, :, :], ALU.mult, ALU.add,
                      ["G", "sel", "Gown"], ["Gown"])
        run = self.sb("run", [128, 2, 2, 64], F32)
        P.op("pool", lambda e: e.memset(Sb[:, 0:2, :, :, :], 0.0), (), [("Sb", 0), ("Sb", 1)])
        P.cp("dve", Sb[:, 1, 0, :, :], Uall[:, 0, 0, :, :], [("Uall", 0), ("Sb", 1)], [("Sb", 1)])
        P.cp("dve", Sb[:, 0, 1, :, :], Uall[:, 1, 1, :, :], [("Uall", 1), ("Sb", 0)], [("Sb", 0)])
        P.cp("dve", run[:, 0, :, :], Gown[:, 0, :, :], ["Gown"], ["run"])
        for n in range(NCL):
            P.cp("pool", Sb[:, 2 + n, 0, :, :], run[:, 0, :, :], ["run"], [("Sb", 2 + n)])
            if n < NCL - 1:
                for p in range(2):
                    P.stt("dve", run[:, 0, p, :], run[:, 0, p, :], self.pw[:, 0, p, 1:2], Uall[:, 2 + n, 0, p, :], ALU.mult, ALU.add,
                          ["run", "pw", ("Uall", 2 + n)], ["run"])
        P.cp("dve", run[:, 1, :, :], Gown[:, 1, :, :], ["Gown"], ["run"])
        for n in range(NCL - 1, -1, -1):
            P.cp("pool", Sb[:, 2 + n, 1, :, :], run[:, 1, :, :], ["run", ("Sb", 2 + n)], [("Sb", 2 + n)])
            if n > 0:
                for p in range(2):
                    P.stt("dve", run[:, 1, p, :], run[:, 1, p, :], self.pw[:, 1, p, 1:2], Uall[:, 2 + n, 1, p, :], ALU.mult, ALU.add,
                          ["run", "pw", ("Uall", 2 + n)], ["run"])

        P.barrier()
        tmpstack.close()
        self.st = outer

        QTb = [self.sb("QTb%d" % i, [128, 4, 512], BF16) for i in range(2)]
        rqb = [self.sb("rqb%d" % i, [128, 2, 512], BF16) for i in range(2)]
        rkb = [self.sb("rkb%d" % i, [128, 2, 512], BF16) for i in range(2)]
        rgb = [self.sb("rgb%d" % i, [128, 4, 256], BF16) for i in range(2)]
        rvb = [self.sb("rvb%d" % i, [128, 4, 256], BF16) for i in range(2)]
        ppb = [self.sb("ppb%d" % i, [128, 6, 256], BF16) for i in range(2)]
        PT = [self.sb("PT%d" % i, [128, 512], BF16) for i in range(4)]
        zeros = self.sb("zeros", [128, 260], BF16)
        P.op("pool", lambda e: e.memset(zeros[:], 0.0), (), ["zeros"])
        mix = [self.sb("mix%d" % i, [128, D], BF16) for i in range(2)]
        mixT = [self.sb("mixT%d" % i, [128, 8, 128], BF16) for i in range(2)]
        rden = self.sb("rden", [128, 4], F32)
        sT = self.sb("sT", [128, 4, 128], BF16)
        qx = self.sb("qx", [128, 2, 2, 128], BF16)
        osq = self.sb("osq", [128, 256], F32)
        ob = self.sb("ob", [128, 256], F32)
        st1 = self.sb("st1", [128, 4], F32)
        st2 = self.sb("st2", [128, 4], F32)
        mean = self.sb("mean", [128, 4], F32)
        yn = self.sb("yn", [128, 256], F32)
        sg = self.sb("sg", [128, 256], F32)
        mxd = self.sb("mxd", [64, 4, 128], BF16)
        xt = [self.sb("xt%d" % i, [128, D], F32) for i in range(2)]
        mjunk = self.sb("mjunk", [128, D], F32)
        mss = self.sb("mss", [128, 1], F32)
        mtmp = self.sb("mtmp", [128, D], F32)
        xo = [self.sb("xo%d" % i, [128, D], F32) for i in range(2)]

        ptc = [0]
        gti = 0
        if self.kb == 1:
            return
        for bi, (col0, ntok) in enumerate(self.blocks):
            isctx = col0 < CT
            if isctx and not self.need_ctx:
                continue
            nt = ntok // 128
            sset = 1 if isctx else 0
            s2 = bi % 2
            Qb, Qk = QTb[s2], "QTb%d" % s2
            P.dma(Qb[:, :, 0:ntok], dr["QT"][:, :, col0:col0 + ntok], writes=[Qk])
            P.dma(rqb[s2][:, :, 0:ntok], dr["rqT"][:, :, col0:col0 + ntok], writes=["rqb%d" % s2])
            P.dma(rkb[s2][:, :, 0:ntok], dr["rkT"][:, :, col0:col0 + ntok], writes=["rkb%d" % s2])
            P.dma(rgb[s2][:, 0:nt, :], dr["rg"][col0:col0 + ntok, :].rearrange("(k p) c -> p k c", p=128), writes=["rgb%d" % s2])
            P.dma(rvb[s2][:, 0:nt, :], dr["rv"][col0:col0 + ntok, :].rearrange("(k p) c -> p k c", p=128), writes=["rvb%d" % s2])
            lo_t = col0 - 128 if (col0 > CT or (isctx and col0 > 0)) else col0
            hi_t = col0 + ntok + 128
            if isctx:
                hi_t = min(hi_t, CT)
            else:
                hi_t = min(hi_t, Tc)
            k0 = (lo_t - (col0 - 128)) // 128
            nk = (hi_t - lo_t) // 128
            P.dma(ppb[s2][:, k0:k0 + nk, :], dr["pp"][lo_t:hi_t, :].rearrange("(k p) c -> p k c", p=128), writes=["ppb%d" % s2])
            kts = list(range(2)) if isctx else list(range(NKT))
            for t in range(nt):
                tcol = col0 + t * 128
                mx_, mxk = mix[gti % 2], "mix%d" % (gti % 2)
                for kvh in range(2):
                    r = slice(kvh * 64, (kvh + 1) * 64)
                    P.mm(bO[:, 0:260], zeros[:, 0:128], zeros[:, 0:260], True, True, ["zeros"], ["bO"])
                    def issue_S(ki_):
                        kt_ = kts[ki_]
                        S_ = bS[ptc[0] % 3]
                        Sk = "bS%d" % (ptc[0] % 3)
                        Pt_, Pk_ = PT[ptc[0] % 4], "PT%d" % (ptc[0] % 4)
                        ptc[0] += 1
                        P.mm(S_[:, :].rearrange("p (g q) -> p g q", g=4), KTa[r, kt_ * 128:(kt_ + 1) * 128],
                             Qb[r, :, t * 128:(t + 1) * 128], True, True, ["KTa", Qk], [Sk])
                        P.op("act", lambda e, Pt_=Pt_, S_=S_: e.activation(Pt_[:], S_[:], AF.Exp, scale=0.125),
                             [Sk], [Pk_], nosame=True)
                        return Pt_, Pk_
                    pend = [issue_S(0)]
                    if len(kts) > 1:
                        pend.append(issue_S(1))
                    for ki, kt in enumerate(kts):
                        if ki + 2 < len(kts):
                            pend.append(issue_S(ki + 2))
                        Pt, Pk = pend.pop(0)
                        for g in range(4):
                            P.mm(bO[:, g * 65:(g + 1) * 65], Pt[:, g * 128:(g + 1) * 128], VAa[:, kt, kvh * 66:kvh * 66 + 65],
                                 False, ki == len(kts) - 1, [Pk, "VAa"], ["bO"], skip_group_check=True)
                    for g in range(4):
                        P.op("dve", lambda e, g=g: e.reciprocal(rden[:, g:g + 1], bO[:, g * 65 + 64: g * 65 + 65]), ["bO"], ["rden"])
                    for g in range(4):
                        h = kvh * 4 + g
                        P.ts("dve", mx_[:, h * 64:(h + 1) * 64], bO[:, g * 65: g * 65 + 64], rden[:, g:g + 1], None, ALU.mult, None,
                             ["bO", "rden"], [mxk])
                if self.kb == 2:
                    continue
                n = tcol // 128
                for d_ in range(2):
                    for p in range(2):
                        P.tt("pool", qx[:, d_, p, :], rqb[s2][:, p, t * 128:(t + 1) * 128], XiT[:, d_, p, :], ALU.mult,
                             ["rqb%d" % s2, "XiT"], ["qx"])
                bUf = bU[:, :, :].rearrange("p a b -> p (a b)")
                for h in range(4):
                    p, hl = h // 2, h % 2
                    r = slice(hl * 64, (hl + 1) * 64)
                    bank, bkey = (bR, "bR") if hl == 0 else (bUf, "bU")
                    P.mm(bank[:, p * 128:(p + 1) * 128], rkb[s2][r, p, t * 128:(t + 1) * 128], rqb[s2][r, p, t * 128:(t + 1) * 128],
                         True, True, ["rkb%d" % s2, "rqb%d" % s2], [bkey])
                for h in range(4):
                    p, hl = h // 2, h % 2
                    bank, bkey = (bR, "bR") if hl == 0 else (bUf, "bU")
                    P.tt("dve", sT[:, h, :], bank[:, p * 128:(p + 1) * 128], DT[:, h, :], ALU.mult, [bkey, "DT"], ["sT"])
                for h in range(4):
                    p, hl = h // 2, h % 2
                    r = slice(hl * 64, (hl + 1) * 64)
                    if hl == 0:
                        oreg, okey = bY[:, p * 64:(p + 1) * 64], "bY"
                    else:
                        oreg, okey = bO[:, 320 + p * 64: 320 + (p + 1) * 64], "bO"
                    P.mm(oreg, sT[:, h, :], rvb[s2][:, t, h * 64:(h + 1) * 64], True, False,
                         ["sT", "rvb%d" % s2], [okey], skip_group_check=True)
                    P.mm(oreg, qx[r, 0, p, :], Sb[r, n, 0, p, :], False, False,
                         ["qx", ("Sb", n)], [okey], skip_group_check=True)
                    P.mm(oreg, qx[r, 1, p, :], Sb[r, n, 1, p, :], False, True,
                         ["qx", ("Sb", n)], [okey], skip_group_check=True)
                ob4 = ob[:].rearrange("p (a b e) -> p a b e", a=2, b=2)
                P.cp("dve", ob4[:, :, 0, :], bY[:, 0:128].rearrange("p (a e) -> p a e", a=2), ["bY"], ["ob"])
                P.cp("dve", ob4[:, :, 1, :], bO[:, 320:448].rearrange("p (a e) -> p a e", a=2), ["bO"], ["ob"])
                P.op("dve", lambda e: e.reduce_sum(st1[:], ob[:].rearrange("p (h e) -> p h e", h=4), AX.X), ["ob"], ["st1"])
                P.act(osq[:], ob[:], AF.Square, ["ob"], ["osq"])
                P.op("dve", lambda e: e.reduce_sum(st2[:], osq[:].rearrange("p (h e) -> p h e", h=4), AX.X), ["osq"], ["st2"])
                P.ts("dve", mean[:], st1[:], 1.0 / 64, None, ALU.mult, None, ["st1"], ["mean"])
                P.tt("dve", st1[:], mean[:], mean[:], ALU.mult, ["mean"], ["st1"])
                P.stt("dve", st2[:], st2[:], 1.0 / 64, st1[:], ALU.mult, ALU.subtract, ["st2", "st1"], ["st2"])
                P.act(st2[:], st2[:], AF.Sqrt, ["st2"], ["st2"], bias=EPS)
                P.op("dve", lambda e: e.reciprocal(st2[:], st2[:]), ["st2"], ["st2"])
                for h in range(4):
                    P.ts("dve", yn[:, h * 64:(h + 1) * 64], ob[:, h * 64:(h + 1) * 64], mean[:, h:h + 1], st2[:, h:h + 1],
                         ALU.subtract, ALU.mult, ["ob", "mean", "st2"], ["yn"])
                P.act(sg[:], rgb[s2][:, t, :], AF.Silu, ["rgb%d" % s2], ["sg"])
                P.tt("pool", mx_[:, 512:768], yn[:], sg[:], ALU.mult, ["yn", "sg"], [mxk])
                if self.kb == 3:
                    continue
                mT_, mTk = mixT[gti % 2], "mixT%d" % (gti % 2)
                for c in range(6):
                    P.tr(pT[:, c, :], mx_[:, c * 128:(c + 1) * 128], self.identb[:], [mxk, "identb"], ["n_pT"])
                P.cp("act", mT_[:, 0:6, :], pT[:, 0:6, :], ["n_pT"], [mTk])
                first = (not isctx and tcol == CT) or (isctx and tcol == 0)
                last = (not isctx and tcol == Tc - 128) or (isctx and tcol == CT - 128)
                for g in range(4):
                    gs = slice(g * 64, (g + 1) * 64)
                    if isctx:
                        Am = Actx[:, (0 if first else 4) + g, :]
                    else:
                        Am = Amain[:, (0 if first else (8 if last else 4)) + g, :]
                    ops_ = [(ppb[s2][:, t + 1, gs], Am)]
                    if not first:
                        ops_.append((ppb[s2][:, t, gs], Anb[:, g, :]))
                    elif not isctx:
                        ops_.append((Hg[:, gs], Ahalo[:, g, :]))
                    if not last:
                        ops_.append((ppb[s2][:, t + 2, gs], Anb[:, 4 + g, :]))
                    elif not isctx:
                        ops_.append((Hg[:, gs], Ahalo[:, 4 + g, :]))
                    mbank, mkey = (bR, "bR") if g < 2 else (bUf, "bU")
                    mcol = 256 + (g % 2) * 128
                    for oi, (l_, r_) in enumerate(ops_):
                        P.mm(mbank[0:64, mcol:mcol + 128], l_, r_, oi == 0, oi == len(ops_) - 1,
                             ["ppb%d" % s2, "Amain", "Anb", "Actx", "Ahalo", "Hg"], [mkey], skip_group_check=True)
                P.cp("act", mxd[:, 0:2, :].rearrange("p g t -> p (g t)"), bR[0:64, 256:512], ["bR"], ["mxd"])
                P.cp("act", mxd[:, 2:4, :].rearrange("p g t -> p (g t)"), bUf[0:64, 256:512], ["bU"], ["mxd"])
                for g in range(4):
                    P.mm(bY[(g % 2) * 64:(g % 2 + 1) * 64, 256 + (g // 2) * 128: 256 + (g // 2 + 1) * 128], pw_[:, g, :], mxd[:, g, :],
                         True, True, ["poolw", "mxd"], ["bYp"], skip_group_check=True)
                for c in range(2):
                    P.ts("dve", mT_[:, 6 + c, :], bY[:, 256 + c * 128: 256 + (c + 1) * 128], pscale[:, c:c + 1], None, ALU.mult, None,
                         ["bYp", "pscale"], [mTk])
                if self.kb == 4:
                    continue
                for hf in range(2):
                    for c in range(8):
                        P.mm(bS[hf][:, :], mT_[:, c, :], wo[:, c, hf * 512:(hf + 1) * 512], c == 0, c == 7, [mTk, "wo"], ["bS%d" % hf])
                x_, xk = xt[gti % 2], "xt%d" % (gti % 2)
                P.dma(x_[:], self.src_x(tcol, 128), writes=[xk])
                P.act(mjunk[:, 0:512], bS[0][:, :], AF.Square, ["bS0"], ["mjunk", "mss"], accum_out=mss[:])
                mss2 = st1[:, 0:1]
                P.act(mjunk[:, 512:1024], bS[1][:, :], AF.Square, ["bS1"], ["mjunk", "st1"], accum_out=mss2)
                P.tt("dve", mss[:], mss[:], mss2, ALU.add, ["mss", "st1"], ["mss"])
                P.ts("dve", mss[:], mss[:], 1.0 / D, EPS, ALU.mult, ALU.add, ["mss"], ["mss"])
                P.act(mss[:], mss[:], AF.Sqrt, ["mss"], ["mss"])
                P.op("dve", lambda e: e.reciprocal(mss[:], mss[:]), ["mss"], ["mss"])
                for hf in range(2):
                    P.stt("dve", mtmp[:, hf * 512:(hf + 1) * 512], bS[hf][:, :], mss[:, 0:1], self.gv[:, sset, hf * 512:(hf + 1) * 512],
                          ALU.mult, ALU.mult, ["bS%d" % hf, "mss", "gv"], ["mtmp"])
                xo_, xok = xo[gti % 2], "xo%d" % (gti % 2)
                P.tt("pool", xo_[:], mtmp[:], x_[:], ALU.add, ["mtmp", xk], [xok])
                dst = dr["cout"][tcol:tcol + 128, :] if isctx else dr["xout"][tcol - CT: tcol - CT + 128, :]
                P.dma(dst, xo_[:], reads=[xok], eng="pool")
                gti += 1

    def phase_B2(self):
        P, dr = self.P, self.dr
        E = self.n_exp
        self.alloc_norm()
        blocks = [b for b in self.blocks if not (b[0] < CT and not self.need_ctx)]
        xt = [self.sb("xt%d" % i, [128, D], F32) for i in range(2)]
        hT = [self.sb("hT0", [128, 8, 512], BF16)] * 2
        w1 = self.sb("w1", [128, 8, DFF], BF16)
        w3 = self.sb("w3", [128, 8, DFF], BF16)
        w2 = self.sb("w2", [128, NF, D], BF16)
        gT = self.sb("gT", [128, NF, 512], BF16)
        sa = self.sb("sa", [128, 512], F32)
        pa = [self.ps("pa%d" % i, [128, 512], F32) for i in range(2)]
        pb = [self.ps("pb%d" % i, [128, 512], F32) for i in range(2)]
        py = [self.ps("py%d" % i, [128, 512], F32) for i in range(2)]
        pr = self.ps("pr", [128, 8], F32)
        ntl = self.Tc // 128
        gates = self.sb("gates", [128, ntl, 8], F32)
        if E > 1:
            rtf = self.sb("rtf", [128, 8, 8], F32)
            rtb = self.sb("rtb", [128, 8, 8], BF16)
            P.dma(rtf[:], dr["router"].rearrange("(c p) e -> p c e", p=128), writes=["rtf"])
            P.cp("dve", rtb[:], rtf[:], ["rtf"], ["rtb"])
            lgt = self.sb("lgt", [128, 8], F32)
            m8 = self.sb("m8", [128, 8], F32)
            msk = self.sb("msk", [128, 8], F32)
            ex = self.sb("ex", [128, 8], F32)
            den = self.sb("den", [128, 1], F32)
        ti = 0
        for bi, (col0, ntok) in enumerate(blocks):
            nt = ntok // 128
            hb, hk = hT[0], "hT0"
            sset = 1 if col0 < CT else 0
            for t in range(nt):
                x_, xk = xt[ti % 2], "xt%d" % (ti % 2)
                src = dr["cout"][col0 + t * 128: col0 + (t + 1) * 128, :] if col0 < CT else \
                    dr["xout"][col0 - CT + t * 128: col0 - CT + (t + 1) * 128, :]
                P.dma(x_[:], src, writes=[xk])
                self.norm_tile_to_hT(x_[:], xk, hb, hk, t * 128, sset, 2, ti)
                ti += 1
                if E > 1:
                    tl = (col0 + t * 128) // 128
                    for c in range(8):
                        P.mm(pr[:, :], hb[:, c, t * 128:(t + 1) * 128], rtb[:, c, :], c == 0, c == 7, [hk, "rtb"], ["pr"])
                    P.cp("dve", lgt[:], pr[:, :], ["pr"], ["lgt"])
                    P.op("dve", lambda e: e.reduce_max(m8[:, 0:1], lgt[:], AX.X), ["lgt"], ["m8"])
                    P.ts("dve", msk[:], lgt[:], m8[:, 0:1], None, ALU.is_equal, None, ["lgt", "m8"], ["msk"])
                    P.stt("dve", ex[:], msk[:], -1e30, lgt[:], ALU.mult, ALU.add, ["msk", "lgt"], ["ex"])
                    P.op("dve", lambda e: e.reduce_max(m8[:, 1:2], ex[:], AX.X), ["ex", "m8"], ["m8"])
                    P.ts("dve", msk[:], lgt[:], m8[:, 1:2], None, ALU.is_ge, None, ["lgt", "m8"], ["msk"])
                    P.ts("dve", ex[:], lgt[:], m8[:, 0:1], None, ALU.subtract, None, ["lgt", "m8"], ["ex"])
                    P.act(ex[:], ex[:], AF.Exp, ["ex"], ["ex"])
                    P.tt("dve", ex[:], ex[:], msk[:], ALU.mult, ["ex", "msk"], ["ex"])
                    P.op("dve", lambda e: e.reduce_sum(den[:], ex[:], AX.X), ["ex"], ["den"])
                    P.op("dve", lambda e: e.reciprocal(den[:], den[:]), ["den"], ["den"])
                    P.ts("dve", gates[:, tl, :], ex[:], den[:, 0:1], None, ALU.mult, None, ["ex", "den"], ["gates"])
            P.dma(dr["hTd"][bi, :, :, 0:ntok], hb[:, :, 0:ntok], reads=[hk], writes=[("hTd", bi)], eng="pool")
        yo = [self.sb("yo%d" % i, [128, D], F32) for i in range(2)]
        ya = [self.sb("ya0", [128, D], F32)] * 2
        yi = 0
        fi = 0
        for e_ in range(E):
            for c in range(8):
                P.dma(w1[:, c, :], dr["w1"][e_, c * 128:(c + 1) * 128, :], reads=["w1"], writes=["w1"], eng="pool")
                P.dma(w3[:, c, :], dr["w3"][e_, c * 128:(c + 1) * 128, :], reads=["w3"], writes=["w3"], eng="pool")
            for f in range(NF):
                P.dma(w2[:, f, :], dr["w2"][e_, f * 128:(f + 1) * 128, :], reads=["w2"], writes=["w2"], eng="pool")
            for bi, (col0, ntok) in enumerate(blocks):
                nt = ntok // 128
                hb, hk = hT[0], "hT0"
                P.dma(hb[:, :, 0:ntok], dr["hTd"][bi, :, :, 0:ntok], writes=[hk], reads=[("hTd", bi)])
                for f in range(NF):
                    a_, ak = pa[fi % 2], "pa%d" % (fi % 2)
                    b_, bk = pb[fi % 2], "pb%d" % (fi % 2)
                    fi += 1
                    for c in range(8):
                        P.mm(a_[:, 0:ntok], w1[:, c, f * 128:(f + 1) * 128], hb[:, c, 0:ntok], c == 0, c == 7, ["w1", hk], [ak])
                    for c in range(8):
                        P.mm(b_[:, 0:ntok], w3[:, c, f * 128:(f + 1) * 128], hb[:, c, 0:ntok], c == 0, c == 7, ["w3", hk], [bk])
                    P.act(sa[:, 0:ntok], a_[:, 0:ntok], AF.Silu, [ak], ["sa"])
                    P.tt("dve", gT[:, f, 0:ntok], sa[:, 0:ntok], b_[:, 0:ntok], ALU.mult, ["sa", bk], ["gT"])
                for t in range(nt):
                    tcol = col0 + t * 128
                    tl = tcol // 128
                    for hf in range(2):
                        for f in range(NF):
                            P.mm(py[hf][:, :], gT[:, f, t * 128:(t + 1) * 128], w2[:, f, hf * 512:(hf + 1) * 512], f == 0, f == NF - 1,
                                 ["gT", "w2"], ["py%d" % hf])
                    yo_, yok = yo[yi % 2], "yo%d" % (yi % 2)
                    ya_, yak = ya[0], "ya0"
                    yi += 1
                    acc = dr["accd"][tcol:tcol + 128, :]
                    if E == 1:
                        for hf in range(2):
                            P.cp("act" if hf == 0 else "dve", yo_[:, hf * 512:(hf + 1) * 512], py[hf][:, :], ["py%d" % hf], [yok])
                    else:
                        if e_ > 0:
                            P.dma(ya_[:], acc, writes=[yak], reads=[("acc", tl)])
                        for hf in range(2):
                            if e_ == 0:
                                P.ts("dve", yo_[:, hf * 512:(hf + 1) * 512], py[hf][:, :], gates[:, tl, e_:e_ + 1], None, ALU.mult, None,
                                     ["py%d" % hf, "gates"], [yok])
                            else:
                                P.stt("dve", yo_[:, hf * 512:(hf + 1) * 512], py[hf][:, :], gates[:, tl, e_:e_ + 1],
                                      ya_[:, hf * 512:(hf + 1) * 512], ALU.mult, ALU.add, ["py%d" % hf, "gates", yak], [yok])
                    P.dma(acc, yo_[:], reads=[yok], writes=[("acc", tl)], eng="pool")
        mss = self.sb("fss", [128, 1], F32)
        ti = 0
        for bi, (col0, ntok) in enumerate(blocks):
            sset = 1 if col0 < CT else 0
            for t in range(ntok // 128):
                tcol = col0 + t * 128
                tl = tcol // 128
                x_, xk = xt[ti % 2], "xt%d" % (ti % 2)
                ya_, yak = ya[0], "ya0"
                yo_, yok = yo[ti % 2], "yo%d" % (ti % 2)
                ti += 1
                dst = dr["cout"][tcol:tcol + 128, :] if col0 < CT else dr["xout"][tcol - CT: tcol - CT + 128, :]
                P.dma(x_[:], dst, writes=[xk], reads=[("xres", tl)])
                P.dma(ya_[:], dr["accd"][tcol:tcol + 128, :], writes=[yak], reads=[("acc", tl)])
                P.act(self.n_junk[:], ya_[:], AF.Square, [yak], ["n_junk", "fss"], accum_out=mss[:])
                P.ts("dve", mss[:], mss[:], 1.0 / D, EPS, ALU.mult, ALU.add, ["fss"], ["fss"])
                P.act(mss[:], mss[:], AF.Sqrt, ["fss"], ["fss"])
                P.op("dve", lambda e: e.reciprocal(mss[:], mss[:]), ["fss"], ["fss"])
                P.stt("dve", yo_[:], ya_[:], mss[:, 0:1], self.gv[:, sset, :], ALU.mult, ALU.mult, [yak, "fss", "gv"], [yok])
                P.tt("pool", yo_[:], yo_[:], x_[:], ALU.add, [yok, xk], [yok])
                P.dma(dst, yo_[:], reads=[yok], writes=[("xres", tl)], eng="pool")


def _pool_weight(L, w, s, t):
    lo = np.clip(t - w // 2, 0, L)
    hi = np.clip(t + w // 2, 0, L)
    val = ((s >= lo) & (s < hi)).astype(np.float64) / (hi - lo)
    val = val - (s == t)
    return val


def host_consts(T, j):
    Tl = T // 4
    Tc = CT + Tl
    NCL = Tl // 128
    cst = {}
    cst["ident"] = np.eye(128, dtype=np.float32)
    bo = np.zeros((128, 128), np.float32)
    bo[0:64, 0:64] = 1
    bo[64:, 64:] = 1
    cst["bones"] = bo
    tpos = np.arange(j * Tl, (j + 1) * Tl)
    row = (tpos // 64).astype(np.float32)
    colp = (tpos % 64).astype(np.float32)
    inv = (np.float32(10000.0) ** (-np.arange(16, dtype=np.float32) / np.float32(16))).astype(np.float32)
    cosT = np.ones((64, Tc), np.float32)
    sinT = np.zeros((64, Tc), np.float32)
    for a, pos in enumerate((row, colp)):
        ang = pos[None, :] * inv[:, None]
        for b in range(2):
            rows = slice(a * 32 + b * 16, a * 32 + b * 16 + 16)
            cosT[rows, CT:] = np.cos(ang)
            sinT[rows, CT:] = np.sin(ang) * (-1.0 if b == 0 else 1.0)
    cst["cosT"] = np.concatenate([cosT, cosT], 0)
    cst["sinT"] = np.concatenate([sinT, sinT], 0)
    m = np.arange(128, dtype=np.float32)
    cst["zexp"] = np.stack([C - 1 - m, m], 1).astype(np.float32)
    cst["nC"] = np.tile((np.arange(NCL + 2, dtype=np.float32) * C)[None, :], (128, 1)).astype(np.float32)
    mm, cc = np.meshgrid(m, m, indexing="ij")
    dmask = np.stack([np.maximum(cc - mm, 0), (cc >= mm).astype(np.float32),
                      np.maximum(mm - cc, 0), (mm >= cc).astype(np.float32)], 1).astype(np.float32)
    cst["dmask"] = dmask
    cst["xiexp"] = np.tile(np.stack([m + 1, C - m], 0)[None], (128, 1, 1)).astype(np.float32)
    sel = np.zeros((128, 4), np.float32)
    sel[:, j] = 1
    cst["sel"] = sel
    wins = (2, 4, 8, 16)
    s_loc = np.arange(128)[:, None]
    t_loc = np.arange(128)[None, :]
    Amain = np.zeros((128, 12, 128), np.float32)
    Anb = np.zeros((128, 8, 128), np.float32)
    Actx = np.zeros((128, 8, 128), np.float32)
    Ahalo = np.zeros((64, 8, 128), np.float32)
    for g, w in enumerate(wins):
        base_f = j * Tl
        Amain[:, 0 + g, :] = _pool_weight(T, w, base_f + s_loc, base_f + t_loc)
        mid = T // 2 // 128 * 128 if T >= 512 else 128
        midb = 128 * (T // 256)
        Amain[:, 4 + g, :] = _pool_weight(T + 4096, w, 2048 + s_loc, 2048 + t_loc)
        base_l = (j + 1) * Tl - 128
        Amain[:, 8 + g, :] = _pool_weight(T, w, base_l + s_loc, base_l + t_loc)
        Anb[:, g, :] = _pool_weight(T + 4096, w, 2048 - 128 + s_loc, 2048 + t_loc)
        Anb[:, 4 + g, :] = _pool_weight(T + 4096, w, 2048 + 128 + s_loc, 2048 + t_loc)
        Actx[:, g, :] = _pool_weight(CT, w, s_loc, t_loc)
        Actx[:, 4 + g, :] = _pool_weight(CT, w, 128 + s_loc, 128 + t_loc)
        for i in range(4):
            for r in range(16):
                spos = i * Tl + r if r < 8 else (i + 1) * Tl - 16 + r
                if i == j:
                    continue
                Ahalo[i * 16 + r, g, :] = _pool_weight(T, w, np.array([[spos]]), base_f + t_loc)[0]
                Ahalo[i * 16 + r, 4 + g, :] = _pool_weight(T, w, np.array([[spos]]), base_l + t_loc)[0]
    cst["Amain"], cst["Anb"], cst["Actx"], cst["Ahalo"] = [a.astype(NPBF) for a in (Amain, Anb, Actx, Ahalo)]
    return cst


def _rope_perm():
    perm = np.zeros(64, np.int64)
    for a in range(2):
        for b in range(2):
            for f in range(16):
                perm[a * 32 + b * 16 + f] = a * 32 + (1 - b) * 16 + f
    return perm


def layer_weights(inp, i):
    perm = _rope_perm()
    w_in = inp["w_in"][i]
    cols = {"aq": 0, "ak": 512, "av": 640, "rq": 768, "rk": 1024, "rv": 1280, "rg": 1536, "pp": 1792}
    fm = []
    for g in range(4):
        fm += [cols["aq"] + g * 64 + d for d in range(64)] + [cols["aq"] + (4 + g) * 64 + d for d in range(64)]
    for g in range(4):
        fm += [cols["aq"] + g * 64 + perm[d] for d in range(64)] + [cols["aq"] + (4 + g) * 64 + perm[d] for d in range(64)]
    fm += [cols["ak"] + d for d in range(128)]
    fm += [cols["ak"] + kv * 64 + perm[d] for kv in range(2) for d in range(64)]
    fm += [cols["rq"] + d for d in range(256)]
    fm += [cols["rk"] + d for d in range(256)]
    tm = [cols["av"] + d for d in range(128)] + [cols["rk"] + d for d in range(256)] + [cols["rv"] + d for d in range(256)] + \
         [cols["rg"] + d for d in range(256)] + [cols["pp"] + d for d in range(256)]
    gq, gk = inp["q_norm"][i], inp["k_norm"][i]
    gains = np.stack([np.tile(gq, 2), np.tile(gq[perm], 2), np.tile(gk, 2), np.tile(gk[perm], 2)], 1).astype(np.float32)
    ps = inp["pool_scale"][i]
    out = {
        "w_fm": np.ascontiguousarray(w_in[:, fm]),
        "w_tm": np.ascontiguousarray(w_in[:, tm]),
        "gains": gains,
        "w_mod": inp["w_mod"][i], "b_mod": inp["b_mod"][i],
        "norms": np.stack([inp["norm_pre_mix"][i], inp["norm_post_mix"][i], inp["norm_pre_ffn"][i], inp["norm_post_ffn"][i]], 0),
        "decay": np.ascontiguousarray(inp["ret_decay_logit"][i].reshape(8)),
        "w_out": inp["w_out"][i],
        "pool_w": np.ascontiguousarray(np.transpose(inp["pool_w"][i], (1, 0, 2))),
        "pool_s": np.ascontiguousarray(ps.reshape(2, 128).T),
    }
    if i % 2 == 0:
        out["w1"], out["w3"], out["w2"] = inp["ffn_w1"][i // 2][None], inp["ffn_w3"][i // 2][None], inp["ffn_w2"][i // 2][None]
    else:
        out["w1"], out["w3"], out["w2"] = inp["moe_w1"][i // 2], inp["moe_w3"][i // 2], inp["moe_w2"][i // 2]
        out["router"] = inp["moe_router"][i // 2]
    return out


_NC_CACHE = {}


def get_nc(T, layer, phases, n_exp):
    key = (T, layer, phases, n_exp)
    if key not in _NC_CACHE:
        b = Builder(T, layer, phases, n_exp)
        nc = b.build()
        _NC_CACHE[key] = (nc, sorted(k for k in b.dr))
    return _NC_CACHE[key][0]


A_IN = ["xin", "cin", "cvec", "w_mod", "b_mod", "norms", "decay", "ident", "zexp", "nC", "w_fm", "w_tm", "gains", "bones",
        "cosT", "sinT"]
A_OUT = ["QT", "KT", "VA", "rqT", "rkT", "rk", "rv", "rg", "pp", "Lout"]
B_IN = ["xin", "cin", "cvec", "w_mod", "b_mod", "norms", "decay", "ident", "zexp", "nC", "QT", "KT", "VA", "rqT", "rkT", "rk",
        "rv", "rg", "pp", "KTg", "VAg", "Lg", "Hg", "w_out", "dmask", "xiexp", "sel", "Amain", "Anb", "Actx", "Ahalo", "pool_w",
        "pool_s", "w1", "w3", "w2"]


def run_model(inp, n_layers=2):
    x = np.asarray(inp["x"], np.float32)
    B, T, _ = x.shape
    Tl = T // 4
    ncore = 8
    xs = [np.ascontiguousarray(x[c // 4, (c % 4) * Tl:((c % 4) + 1) * Tl]) for c in range(ncore)]
    cs = [np.ascontiguousarray(np.asarray(inp["ctx"], np.float32)[c // 4]) for c in range(ncore)]
    csts = [host_consts(T, c % 4) for c in range(ncore)]
    cvecs = [np.stack([inp["c"][c // 4], inp["c_ctx"]], 0).astype(np.float32) for c in range(ncore)]
    for i in range(n_layers):
        lw = layer_weights(inp, i)
        n_exp = 1 if i % 2 == 0 else NE
        ncA = get_nc(T, i, "A", n_exp)
        maps = []
        for c in range(ncore):
            m = {"xin": xs[c], "cin": cs[c], "cvec": cvecs[c]}
            m.update(lw)
            m.update(csts[c])
            maps.append({k: np.ascontiguousarray(m[k]) for k in A_IN})
        resA = run_bass_kernel_spmd(ncA, maps, core_ids=list(range(ncore))).results
        ncB = get_nc(T, i, "B", n_exp)
        maps = []
        for c in range(ncore):
            b = c // 4
            grp = [resA[b * 4 + r] for r in range(4)]
            m = {"xin": xs[c], "cin": cs[c], "cvec": cvecs[c]}
            m.update(lw)
            m.update(csts[c])
            for k in A_OUT:
                m[k] = resA[c][k]
            m["KTg"] = np.concatenate([g["KT"][:, CT:] for g in grp], 0)
            m["VAg"] = np.concatenate([g["VA"][CT:] for g in grp], 0)
            m["Lg"] = np.concatenate([g["Lout"] for g in grp], 0)
            m["Hg"] = np.concatenate([np.concatenate([g["pp"][CT:CT + 8], g["pp"][-8:]], 0) for g in grp], 0)
            names = list(B_IN) + (["router"] if n_exp > 1 else [])
            maps.append({k: np.ascontiguousarray(m[k]) for k in names})
        resB = run_bass_kernel_spmd(ncB, maps, core_ids=list(range(ncore))).results
        xs = [resB[c]["xout"] for c in range(ncore)]
        if i == 0:
            cs = [resB[c]["cout"] for c in range(ncore)]
    out = np.zeros((B, T, D), np.float32)
    for c in range(ncore):
        out[c // 4, (c % 4) * Tl:((c % 4) + 1) * Tl] = xs[c]
    return out


def kernel(**inputs):
    inp = {k: np.asarray(v) for k, v in inputs.items()}
    return run_model(inp, 2)
```

```python
import contextlib
import numpy as np
import ml_dtypes
import concourse.bass as bass
import concourse.mybir as mybir
from concourse.bass_utils import run_bass_kernel_spmd

F32 = mybir.dt.float32
BF16 = mybir.dt.bfloat16
AF = mybir.ActivationFunctionType
ALU = mybir.AluOpType
AX = mybir.AxisListType
NPBF = ml_dtypes.bfloat16

D = 1024
CT = 256
DFF = 2816
NF = DFF // 128
NE = 8
EPS = 1e-6
C = 128

ENGS = ["pe", "act", "dve", "pool", "sp"]
NDMA = 8
SAME_ENG_SYNC = {"pe": False, "act": True, "dve": True, "pool": True, "sp": False}


class Prog:
    def __init__(self, nc, stack):
        self.nc = nc
        self.ops = {e: [] for e in ENGS}
        self.count = {e: 0 for e in ENGS}
        self.waited = {e: {} for e in ENGS}
        self.last_w = {}
        self.readers = {}
        self.dma_n = {e: 0 for e in ENGS}
        self.sems = {}
        for e in ENGS:
            self.sems[("c", e)] = stack.enter_context(nc.semaphore("c_" + e))
        for e in ("sp", "pool", "act"):
            for i in range(NDMA):
                self.sems[("d", e, i)] = stack.enter_context(nc.semaphore("d_%s%d" % (e, i)))
        self.semval = {k: 0 for k in self.sems}
        self.nops = 0

    def op(self, eng, fn, reads=(), writes=(), dma=False, nosame=False):
        deps = {}

        def add(d):
            if d is None:
                return
            sk, v = d
            if deps.get(sk, 0) < v:
                deps[sk] = v

        for k in reads:
            add(self.last_w.get(k))
        for k in writes:
            add(self.last_w.get(k))
            for sk, v in self.readers.get(k, {}).items():
                add((sk, v))
        if dma:
            n = self.dma_n[eng]
            idx, use = n % NDMA, n // NDMA
            semkey = ("d", eng, idx)
            val = 16 * (use + 1)
            if use > 0:
                add((semkey, 16 * use))
            self.dma_n[eng] += 1
            inc = 16
        else:
            self.count[eng] += 1
            semkey = ("c", eng)
            val = self.count[eng]
            inc = 1
        self.semval[semkey] = val
        for sk, v in deps.items():
            if sk == ("c", eng) and (nosame or not SAME_ENG_SYNC[eng]):
                continue
            if self.waited[eng].get(sk, 0) >= v:
                continue
            self.waited[eng][sk] = v
            s = self.sems[sk]
            self.ops[eng].append(lambda e, s=s, v=v: e.wait_ge(s, v))
        s = self.sems[semkey]
        self.ops[eng].append(lambda e, s=s, inc=inc: fn(e).then_inc(s, inc))
        me = (semkey, val)
        for k in reads:
            r = self.readers.setdefault(k, {})
            if r.get(semkey, 0) < val:
                r[semkey] = val
        for k in writes:
            self.last_w[k] = me
            self.readers[k] = {}
        self.nops += 1

    def new_phase(self, stack):
        self.phase_id = getattr(self, "phase_id", 0) + 1
        for e in ENGS:
            k = ("c", e)
            self.sems[k] = stack.enter_context(self.nc.semaphore("c%d_%s" % (self.phase_id, e)))
            self.semval[k] = 0
            self.count[e] = 0
            for w in self.waited.values():
                w.pop(k, None)

    def barrier(self):
        for e in ENGS:
            for sk, v in self.semval.items():
                if v == 0:
                    continue
                if sk == ("c", e) and e in ("pe", "sp"):
                    continue
                if self.waited[e].get(sk, 0) >= v:
                    continue
                self.waited[e][sk] = v
                s = self.sems[sk]
                self.ops[e].append(lambda en, s=s, v=v: en.wait_ge(s, v))
        self.last_w = {}
        self.readers = {}

    def emit(self):
        nc = self.nc
        ops = self.ops
        with nc.Block() as block:
            @block.tensor
            def _(e):
                for f in ops["pe"]:
                    f(e)

            @block.scalar
            def _(e):
                for f in ops["act"]:
                    f(e)

            @block.vector
            def _(e):
                for f in ops["dve"]:
                    f(e)

            @block.gpsimd
            def _(e):
                for f in ops["pool"]:
                    f(e)

            @block.sync
            def _(e):
                for f in ops["sp"]:
                    f(e)
        self.ops = {e: [] for e in ENGS}

    def dma(self, out, in_, reads=(), writes=(), eng="sp", **kw):
        self.op(eng, lambda e: e.dma_start(out=out, in_=in_, **kw), reads, writes, dma=True)

    def mm(self, out, lhsT, rhs, start, stop, reads=(), writes=(), **kw):
        self.op("pe", lambda e: e.matmul(out, lhsT, rhs, start=start, stop=stop, **kw), reads, writes)

    def tr(self, out, in_, ident, reads=(), writes=()):
        self.op("pe", lambda e: e.transpose(out, in_, ident), reads, writes)

    def act(self, out, in_, func, reads=(), writes=(), **kw):
        self.op("act", lambda e: e.activation(out, in_, func, **kw), reads, writes)

    def ts(self, eng, out, in0, s1, s2, op0, op1=None, reads=(), writes=()):
        if op1 is None:
            self.op(eng, lambda e: e.tensor_scalar(out, in0, s1, None, op0), reads, writes)
        else:
            self.op(eng, lambda e: e.tensor_scalar(out, in0, s1, s2, op0, op1), reads, writes)

    def tt(self, eng, out, in0, in1, op, reads=(), writes=()):
        self.op(eng, lambda e: e.tensor_tensor(out, in0, in1, op), reads, writes)

    def stt(self, eng, out, in0, scalar, in1, op0, op1, reads=(), writes=()):
        self.op(eng, lambda e: e.scalar_tensor_tensor(out, in0, scalar, in1, op0, op1), reads, writes)

    def cp(self, eng, out, in_, reads=(), writes=()):
        if eng == "act":
            self.op(eng, lambda e: e.copy(out, in_), reads, writes)
        else:
            self.op(eng, lambda e: e.tensor_copy(out, in_), reads, writes)


class Builder:
    def __init__(self, T, layer, phases, n_exp):
        self.T = T
        self.Tl = T // 4
        self.Tc = CT + self.Tl
        self.NCL = self.Tl // 128
        self.NKT = (CT + T) // 128
        self.layer = layer
        self.phases = phases
        self.n_exp = n_exp
        self.need_ctx = layer == 0
        self.nc = bass.Bass("TRN2", target_bir_lowering=False)
        self.dr = {}
        self.blocks = [(0, CT)] + [(CT + 512 * i, 512) for i in range(self.Tl // 512)]

    def din(self, name, shape, dt):
        self.dr[name] = self.nc.dram_tensor(name, list(shape), dt, kind="ExternalInput").ap()
        return self.dr[name]

    def dout(self, name, shape, dt):
        self.dr[name] = self.nc.dram_tensor(name, list(shape), dt, kind="ExternalOutput").ap()
        return self.dr[name]

    def build(self):
        nc = self.nc
        with contextlib.ExitStack() as gst:
            self.P = Prog(nc, gst)
            if "A" in self.phases:
                self.declare_A()
            if "B" in self.phases:
                self.declare_B()
            if "A" in self.phases:
                with contextlib.ExitStack() as st:
                    self.st = st
                    self.common_consts()
                    self.emit_mod()
                    import os as _os
                    self.kstop = int(_os.environ.get("KSTOP", "0"))
                    if self.kstop != 1:
                        self.phase_A()
                    self.P.barrier()
                    self.P.emit()
            if "B" in self.phases:
                with contextlib.ExitStack() as st:
                    self.st = st
                    self.common_consts()
                    self.emit_mod()
                    import os as _os
                    self.kb = int(_os.environ.get("KSTOPB", "0"))
                    self.phase_B1()
                    self.P.barrier()
                    self.P.emit()
                    self.P.new_phase(gst)
                with contextlib.ExitStack() as st:
                    self.st = st
                    self.common_consts()
                    self.emit_mod(only_ffn=True)
                    if self.kb in (0, 6, 7):
                        self.phase_B2()
                    self.P.barrier()
                    self.P.emit()
        return nc

    def sb(self, name, shape, dt):
        self.uid = getattr(self, "uid", 0) + 1
        return self.st.enter_context(self.nc.sbuf_tensor("s%d_%s" % (self.uid, name), list(shape), dt))

    def ps(self, name, shape, dt):
        self.uid = getattr(self, "uid", 0) + 1
        return self.st.enter_context(self.nc.psum_tensor("p%d_%s" % (self.uid, name), list(shape), dt))

    def declare_common(self):
        if "xin" in self.dr:
            return
        Tl, Tc = self.Tl, self.Tc
        self.din("xin", [Tl, D], F32)
        self.din("cin", [CT, D], F32)
        self.din("cvec", [2, D], F32)
        self.din("w_mod", [D, 6 * D], F32)
        self.din("b_mod", [6 * D], F32)
        self.din("norms", [4, D], F32)
        self.din("decay", [8], F32)
        self.din("ident", [128, 128], F32)
        self.din("zexp", [128, 2], F32)
        self.din("nC", [128, self.NCL + 2], F32)

    def declare_A(self):
        self.declare_common()
        Tl, Tc = self.Tl, self.Tc
        self.din("w_fm", [D, 1792], F32)
        self.din("w_tm", [D, 1152], F32)
        self.din("gains", [128, 4], F32)
        self.din("bones", [128, 128], F32)
        self.din("cosT", [128, Tc], F32)
        self.din("sinT", [128, Tc], F32)
        o = self.dout if "B" not in self.phases else self.dint
        o("QT", [128, 4, Tc], BF16)
        o("KT", [128, Tc], BF16)
        o("VA", [Tc, 132], BF16)
        o("rqT", [128, 2, Tc], BF16)
        o("rkT", [128, 2, Tc], BF16)
        o("rk", [Tc, 256], BF16)
        o("rv", [Tc, 256], BF16)
        o("rg", [Tc, 256], BF16)
        o("pp", [Tc, 256], BF16)
        o("Lout", [256, 128], F32)

    def declare_B(self):
        self.declare_common()
        Tl, Tc, T = self.Tl, self.Tc, self.T
        if "A" not in self.phases:
            i = self.din
            i("QT", [128, 4, Tc], BF16)
            i("KT", [128, Tc], BF16)
            i("VA", [Tc, 132], BF16)
            i("rqT", [128, 2, Tc], BF16)
            i("rkT", [128, 2, Tc], BF16)
            i("rk", [Tc, 256], BF16)
            i("rv", [Tc, 256], BF16)
            i("rg", [Tc, 256], BF16)
            i("pp", [Tc, 256], BF16)
            i("KTg", [4 * 128, Tl], BF16)
            i("VAg", [4 * Tl, 132], BF16)
            i("Lg", [4 * 256, 128], F32)
            i("Hg", [64, 256], BF16)
        self.din("w_out", [D, D], F32)
        self.din("dmask", [128, 4, 128], F32)
        self.din("xiexp", [128, 2, 128], F32)
        self.din("sel", [128, 4], F32)
        self.din("Amain", [128, 12, 128], BF16)
        self.din("Anb", [128, 8, 128], BF16)
        self.din("Actx", [128, 8, 128], BF16)
        self.din("Ahalo", [64, 8, 128], BF16)
        self.din("pool_w", [64, 4, 64], F32)
        self.din("pool_s", [128, 2], F32)
        E = self.n_exp
        self.din("w1", [E, D, DFF], F32)
        self.din("w3", [E, D, DFF], F32)
        self.din("w2", [E, DFF, D], F32)
        if E > 1:
            self.din("router", [D, NE], F32)
        self.dout("xout", [Tl, D], F32)
        if self.need_ctx:
            self.dout("cout", [CT, D], F32)
        self.dr["hTd"] = self.nc.dram_tensor("hTd", [len(self.blocks), 128, 8, 512], BF16, kind="Internal").ap()
        self.dr["accd"] = self.nc.dram_tensor("accd", [self.Tc, D], F32, kind="Internal").ap()

    def dint(self, name, shape, dt):
        self.dr[name] = self.nc.dram_tensor(name, list(shape), dt, kind="Internal").ap()
        return self.dr[name]

    def common_consts(self):
        P, dr = self.P, self.dr
        self.identf = self.sb("identf", [128, 128], F32)
        self.identb = self.sb("identb", [128, 128], BF16)
        P.dma(self.identf[:], dr["ident"], writes=["identf"])
        P.cp("dve", self.identb[:], self.identf[:], ["identf"], ["identb"])

    def emit_mod(self, only_ffn=False):
        P, dr = self.P, self.dr
        self.modT = self.sb("modT", [128, 2, 4, 8], F32)
        self.gv = self.sb("gv", [128, 2, D], F32)
        if "modd" not in dr:
            dr["modd"] = self.nc.dram_tensor("modd", [2, 6 * D], F32, kind="Internal").ap()
        outer = self.st
        with contextlib.ExitStack() as tmp:
            self.st = tmp
            cv = self.sb("cv", [128, 2, 8], F32)
            scv = self.sb("scv", [128, 2, 8], F32)
            for s_ in range(2):
                P.dma(cv[:, s_, :], dr["cvec"][s_].rearrange("(c p) -> p c", p=128), reads=["cv"], writes=["cv"],
                      allow_slow_non_contiguous=True)
            P.act(scv[:], cv[:], AF.Silu, ["cv"], ["scv"])
            modrow = self.sb("modrow", [2, 6 * D], F32)
            brow = self.sb("brow", [2, 6 * D], F32)
            P.dma(brow[:], dr["b_mod"].partition_broadcast(2), writes=["brow"])
            wm = [self.sb("wm%d" % i, [128, 8, 512], F32) for i in range(2)]
            pm = self.ps("pm", [128, 512], F32)
            P.op("pool", lambda e: e.memset(modrow[:], 0.0), (), ["modrow"])
            for j in (range(6, 12) if only_ffn else range(0, 6)):
                w = wm[j % 2]
                P.dma(w[:], dr["w_mod"][:, j * 512:(j + 1) * 512].rearrange("(c p) n -> p c n", p=128),
                      writes=["wm%d" % (j % 2)])
                for c in range(8):
                    P.mm(pm[0:2, :], scv[:, :, c], w[:, c, :], c == 0, c == 7, ["scv", "wm%d" % (j % 2)], ["pm"])
                P.tt("dve", modrow[:, j * 512:(j + 1) * 512], pm[0:2, :], brow[:, j * 512:(j + 1) * 512], ALU.add,
                     ["pm", "brow"], ["modrow"])
            P.dma(dr["modd"], modrow[:], reads=["modrow"], writes=["modd"])
            nT = self.sb("nT", [128, 4, 8], F32)
            for k_ in range(4):
                P.dma(nT[:, k_, :], dr["norms"][k_].rearrange("(c p) -> p c", p=128), reads=["nT"], writes=["nT"],
                      allow_slow_non_contiguous=True)
            mT = self.sb("mT", [128, 6, 8, 2], F32)
            pmt = self.ps("pmt", [128, 48, 2], F32)
            for v in range(6):
                for c in range(8):
                    P.tr(pmt[:, v * 8 + c, :], modrow[0:2, v * D + c * 128: v * D + (c + 1) * 128], self.identf[0:2, 0:2],
                         ["modrow", "identf"], ["pmt"])
            P.cp("dve", mT[:].rearrange("p v c s -> p (v c s)"), pmt[:].rearrange("p a s -> p (a s)"), ["pmt"], ["mT"])
            for s_ in range(2):
                P.stt("dve", self.modT[:, s_, 0, :], mT[:, 1, :, s_], 1.0, nT[:, 0, :], ALU.add, ALU.mult, ["mT", "nT"], ["modT"])
                P.cp("dve", self.modT[:, s_, 1, :], mT[:, 0, :, s_], ["mT"], ["modT"])
                P.stt("dve", self.modT[:, s_, 2, :], mT[:, 4, :, s_], 1.0, nT[:, 2, :], ALU.add, ALU.mult, ["mT", "nT"], ["modT"])
                P.cp("dve", self.modT[:, s_, 3, :], mT[:, 3, :, s_], ["mT"], ["modT"])
            nb = self.sb("nb", [128, D], F32)
            P.dma(nb[:], dr["norms"][3 if only_ffn else 1].partition_broadcast(128), writes=["nb"])
            v = 5 if only_ffn else 2
            for s_ in range(2):
                P.dma(self.gv[:, s_, :], dr["modd"][s_, v * D:(v + 1) * D].partition_broadcast(128), reads=["modd", "gv"], writes=["gv"])
            for s_ in range(2):
                P.tt("dve", self.gv[:, s_, :], self.gv[:, s_, :], nb[:], ALU.mult, ["gv", "nb"], ["gv"])
            P.barrier()
        self.st = outer

    def norm_tile_to_hT(self, xt, xkey, hT, hkey, col, sset, v, tagi):
        P = self.P
        junk, ss, xs, pT = self.n_junk, self.n_ss[tagi % 2], self.n_xs[tagi % 2], self.n_pT
        ssk, xsk = "n_ss%d" % (tagi % 2), "n_xs%d" % (tagi % 2)
        P.act(junk[:], xt, AF.Square, [xkey], ["n_junk", ssk], accum_out=ss[:])
        P.ts("dve", ss[:], ss[:], 1.0 / D, EPS, ALU.mult, ALU.add, [ssk], [ssk])
        P.act(ss[:], ss[:], AF.Sqrt, [ssk], [ssk])
        P.op("dve", lambda e: e.reciprocal(ss[:], ss[:]), [ssk], [ssk])
        P.ts("dve", xs[:], xt, ss[:, 0:1], None, ALU.mult, None, [xkey, ssk], [xsk])
        for c in range(8):
            P.tr(pT[:, c, :], xs[:, c * 128:(c + 1) * 128], self.identb[:], [xsk, "identb"], ["n_pT"])
        for c in range(8):
            eng = "dve" if c % 2 == 0 else "pool"
            if eng == "pool":
                eng = "dve"
            P.ts(eng, hT[:, c, col:col + 128], pT[:, c, :], self.modT[:, sset, v, c:c + 1], self.modT[:, sset, v + 1, c:c + 1],
                 ALU.mult, ALU.add, ["n_pT", "modT"], [hkey])

    def alloc_norm(self):
        self.n_junk = self.sb("n_junk", [128, D], BF16)
        self.n_ss = [self.sb("n_ss%d" % i, [128, 1], F32) for i in range(2)]
        self.n_xs = [self.sb("n_xs%d" % i, [128, D], BF16) for i in range(2)]
        self.n_pT = self.ps("n_pT", [128, 8, 128], BF16)

    def src_x(self, col, n):
        if col < CT:
            return self.dr["cin"][col:col + n, :]
        return self.dr["xin"][col - CT:col - CT + n, :]

    def ret_consts(self):
        P, dr = self.P, self.dr
        dl = self.sb("dl", [128, 8], F32)
        P.dma(dl[:], dr["decay"].partition_broadcast(128), writes=["dl"])
        lg = self.sb("lg", [128, 8], F32)
        P.act(lg[:], dl[:], AF.Exp, ["dl"], ["lg"], scale=-1.0)
        P.act(lg[:], lg[:], AF.Ln, ["lg"], ["lg"], bias=1.0)
        P.ts("dve", lg[:], lg[:], -1.0, None, ALU.mult, None, ["lg"], ["lg"])
        self.lg = lg
        lgs = self.sb("lgs", [128, 2, 2], F32)
        for d_ in range(2):
            for p in range(2):
                for hl in range(2):
                    P.cp("dve", lgs[hl * 64:(hl + 1) * 64, d_, p:p + 1], lg[hl * 64:(hl + 1) * 64, d_ * 4 + 2 * p + hl: d_ * 4 + 2 * p + hl + 1],
                         ["lg"], ["lgs"])
        self.lgs = lgs
        zx = self.sb("zx", [128, 2], F32)
        P.dma(zx[:], dr["zexp"], writes=["zx"])
        nCt = self.sb("nCt", [128, self.NCL + 2], F32)
        P.dma(nCt[:], dr["nC"], writes=["nCt"])
        zt = self.sb("zt", [128, 8], F32)
        for d_ in range(2):
            for h in range(4):
                P.act(zt[:, d_ * 4 + h: d_ * 4 + h + 1], zx[:, d_:d_ + 1], AF.Exp, ["zx", "lg"], ["zt"],
                      scale=lg[:, d_ * 4 + h: d_ * 4 + h + 1])
        ones = self.sb("ones64", [128, 64], F32)
        P.op("pool", lambda e: e.memset(ones[:], 1.0), (), ["ones64"])
        self.Z = self.sb("Z", [128, 2, 256], F32)
        for d_ in range(2):
            for h in range(4):
                P.ts("dve", self.Z[:, d_, h * 64:(h + 1) * 64], ones[:], zt[:, d_ * 4 + h: d_ * 4 + h + 1], None, ALU.mult, None,
                     ["ones64", "zt"], ["Z"])
        self.pw = self.sb("pw", [128, 2, 2, self.NCL + 2], F32)
        for d_ in range(2):
            for p in range(2):
                P.act(self.pw[:, d_, p, :], nCt[:], AF.Exp, ["nCt", "lgs"], ["pw"], scale=lgs[:, d_, p:p + 1])

    def emit_U(self, rk_t, rv_t, keys, pU, tag):
        P = self.P
        kz = self.u_kz
        for d_ in range(2):
            P.tt("dve", kz[:, d_, :], rk_t, self.Z[:, d_, :], ALU.mult, keys + ["Z"], ["u_kz"])
        for d_ in range(2):
            for p in range(2):
                P.mm(pU[:, d_ * 2 + p, :], kz[:, d_, p * 128:(p + 1) * 128], rv_t[:, p * 128:(p + 1) * 128], True, True,
                     ["u_kz"] + keys, [tag])

    def phase_A(self):
        P, dr = self.P, self.dr
        Tc = self.Tc
        self.alloc_norm()
        self.ret_consts()
        wfm = self.sb("wfm", [128, 8, 1792], BF16)
        wtm = self.sb("wtm", [128, 8, 1152], BF16)
        for c in range(8):
            P.dma(wfm[:, c, :], dr["w_fm"][c * 128:(c + 1) * 128, :], writes=["wfm"], reads=["wfm"], eng="pool")
            P.dma(wtm[:, c, :], dr["w_tm"][c * 128:(c + 1) * 128, :], writes=["wtm"], reads=["wtm"], eng="pool")
        gains = self.sb("gains", [128, 4], F32)
        P.dma(gains[:], dr["gains"], writes=["gains"])
        bonesf = self.sb("bonesf", [128, 128], F32)
        bones = self.sb("bones", [128, 128], BF16)
        P.dma(bonesf[:], dr["bones"], writes=["bonesf"])
        P.cp("dve", bones[:], bonesf[:], ["bonesf"], ["bones"])
        cosT = self.sb("cosT", [128, Tc], F32)
        sinT = self.sb("sinT", [128, Tc], F32)
        P.dma(cosT[:], dr["cosT"], writes=["cosT"])
        P.dma(sinT[:], dr["sinT"], writes=["sinT"])
        self.u_kz = self.sb("u_kz", [128, 2, 256], BF16)
        Lacc = self.sb("Lacc", [128, 2, 2, 64], F32)
        P.op("pool", lambda e: e.memset(Lacc[:], 0.0), (), ["Lacc"])

        xt = [self.sb("xt%d" % i, [128, D], F32) for i in range(2)]
        hT = [self.sb("hT%d" % i, [128, 8, 512], BF16) for i in range(2)]
        pa = self.ps("pa", [128, 512], F32)
        pb = self.ps("pb", [128, 512], F32)
        pss = self.ps("pss", [128, 512], F32)
        ptm = [self.ps("ptm%d" % i, [128, 512], F32) for i in range(2)]
        pU = self.ps("pU", [128, 4, 128], F32)
        sq = self.sb("sq", [128, 512], BF16)
        rr = self.sb("rr", [128, 512], F32)
        t1 = self.sb("t1", [128, 512], F32)
        t2 = self.sb("t2", [128, 512], F32)
        qo = [self.sb("qo%d" % i, [128, 4, 512], BF16) for i in range(2)]
        ko = [self.sb("ko%d" % i, [128, 512], BF16) for i in range(2)]
        rqo = [self.sb("rqo%d" % i, [128, 2, 512], BF16) for i in range(2)]
        rko = [self.sb("rko%d" % i, [128, 2, 512], BF16) for i in range(2)]
        va = [self.sb("va%d" % i, [128, 132], BF16) for i in range(2)]
        tko = [self.sb("tko%d" % i, [128, 256], BF16) for i in range(2)]
        tvo = [self.sb("tvo%d" % i, [128, 256], BF16) for i in range(2)]
        tgo = [self.sb("tgo%d" % i, [128, 256], BF16) for i in range(2)]
        tpo = [self.sb("tpo%d" % i, [128, 256], BF16) for i in range(2)]
        for i in range(2):
            P.op("pool", lambda e, i=i: e.memset(va[i][:], 1.0), (), ["va%d" % i])

        ti = 0
        if self.kstop == 2:
            return
        for bi, (col0, ntok) in enumerate(self.blocks):
            nt = ntok // 128
            hb = hT[bi % 2]
            hk = "hT%d" % (bi % 2)
            sset = 1 if col0 < CT else 0
            if self.kstop == 3 and bi > 0:
                return
            for t in range(nt):
                x_ = xt[ti % 2]
                xk = "xt%d" % (ti % 2)
                P.dma(x_[:], self.src_x(col0 + t * 128, 128), writes=[xk])
                self.norm_tile_to_hT(x_[:], xk, hb, hk, t * 128, sset, 0, ti)
                ti += 1
            if self.kstop == 4:
                return
            qb_, qk_ = qo[bi % 2], "qo%d" % (bi % 2)
            kb_, kk_ = ko[bi % 2], "ko%d" % (bi % 2)

            def fm(ps_, m):
                for c in range(8):
                    P.mm(ps_[:, 0:ntok], wfm[:, c, m * 128:(m + 1) * 128], hb[:, c, 0:ntok], c == 0, c == 7,
                         ["wfm", hk], [ps_.name if hasattr(ps_, "name") else "px"])

            for g in range(5):
                ma, mb = (g, g + 4) if g < 4 else (8, 9)
                gcol = 0 if g < 4 else 2
                for c in range(8):
                    P.mm(pa[:, 0:ntok], wfm[:, c, ma * 128:(ma + 1) * 128], hb[:, c, 0:ntok], c == 0, c == 7, ["wfm", hk], ["pa"])
                for c in range(8):
                    P.mm(pb[:, 0:ntok], wfm[:, c, mb * 128:(mb + 1) * 128], hb[:, c, 0:ntok], c == 0, c == 7, ["wfm", hk], ["pb"])
                P.act(sq[:, 0:ntok], pa[:, 0:ntok], AF.Square, ["pa"], ["sq"])
                P.mm(pss[:, 0:ntok], bones[:], sq[:, 0:ntok], True, True, ["bones", "sq"], ["pss"])
                P.act(rr[:, 0:ntok], pss[:, 0:ntok], AF.Sqrt, ["pss"], ["rr"], scale=1.0 / 64, bias=EPS)
                P.op("dve", lambda e, n=ntok: e.reciprocal(rr[:, 0:n], rr[:, 0:n]), ["rr"], ["rr"])
                P.stt("dve", t1[:, 0:ntok], pa[:, 0:ntok], gains[:, gcol:gcol + 1], cosT[:, col0:col0 + ntok], ALU.mult, ALU.mult,
                      ["pa", "gains", "cosT"], ["t1"])
                P.stt("dve", t2[:, 0:ntok], pb[:, 0:ntok], gains[:, gcol + 1:gcol + 2], sinT[:, col0:col0 + ntok], ALU.mult, ALU.mult,
                      ["pb", "gains", "sinT"], ["t2"])
                P.tt("pool", t1[:, 0:ntok], t1[:, 0:ntok], t2[:, 0:ntok], ALU.add, ["t1", "t2"], ["t1"])
                if g < 4:
                    P.tt("pool", qb_[:, g, 0:ntok], t1[:, 0:ntok], rr[:, 0:ntok], ALU.mult, ["t1", "rr"], [qk_])
                else:
                    P.tt("pool", kb_[:, 0:ntok], t1[:, 0:ntok], rr[:, 0:ntok], ALU.mult, ["t1", "rr"], [kk_])
            if self.kstop == 5:
                return
            P.dma(dr["QT"][:, :, col0:col0 + ntok], qb_[:, :, 0:ntok], reads=[qk_])
            P.dma(dr["KT"][:, col0:col0 + ntok], kb_[:, 0:ntok], reads=[kk_])
            rq_, rqk = rqo[bi % 2], "rqo%d" % (bi % 2)
            rk_, rkk = rko[bi % 2], "rko%d" % (bi % 2)
            for p in range(2):
                for c in range(8):
                    P.mm(pa[:, 0:ntok], wfm[:, c, (10 + p) * 128:(11 + p) * 128], hb[:, c, 0:ntok], c == 0, c == 7, ["wfm", hk], ["pa"])
                P.cp("act", rq_[:, p, 0:ntok], pa[:, 0:ntok], ["pa"], [rqk])
                for c in range(8):
                    P.mm(pb[:, 0:ntok], wfm[:, c, (12 + p) * 128:(13 + p) * 128], hb[:, c, 0:ntok], c == 0, c == 7, ["wfm", hk], ["pb"])
                P.ts("dve", rk_[:, p, 0:ntok], pb[:, 0:ntok], 0.125, None, ALU.mult, None, ["pb"], [rkk])
            P.dma(dr["rqT"][:, :, col0:col0 + ntok], rq_[:, :, 0:ntok], reads=[rqk])
            P.dma(dr["rkT"][:, :, col0:col0 + ntok], rk_[:, :, 0:ntok], reads=[rkk])
            if self.kstop == 6:
                return
            for t in range(nt):
                tcol = col0 + t * 128
                s = (bi * 4 + t) % 2
                p0 = ptm[0]
                for c in range(8):
                    P.mm(p0[:, 0:384], hb[:, c, t * 128:(t + 1) * 128], wtm[:, c, 0:384], c == 0, c == 7, [hk, "wtm"], ["ptm0"])
                if self.kstop == 9:
                    continue
                P.cp("dve", va[s][:, 0:64], p0[:, 0:64], ["ptm0"], ["va%d" % s])
                P.cp("dve", va[s][:, 66:130], p0[:, 64:128], ["ptm0"], ["va%d" % s])
                P.ts("dve", tko[s][:], p0[:, 128:384], 0.125, None, ALU.mult, None, ["ptm0"], ["tko%d" % s])
                if self.kstop == 8:
                    continue
                p1 = ptm[1]
                for c in range(8):
                    P.mm(p1[:, 0:512], hb[:, c, t * 128:(t + 1) * 128], wtm[:, c, 384:896], c == 0, c == 7, [hk, "wtm"], ["ptm1"])
                P.cp("dve", tvo[s][:], p1[:, 0:256], ["ptm1"], ["tvo%d" % s])
                P.cp("dve", tgo[s][:], p1[:, 256:512], ["ptm1"], ["tgo%d" % s])
                for c in range(8):
                    P.mm(p0[:, 0:256], hb[:, c, t * 128:(t + 1) * 128], wtm[:, c, 896:1152], c == 0, c == 7, [hk, "wtm"], ["ptm0"])
                P.cp("dve", tpo[s][:], p0[:, 0:256], ["ptm0"], ["tpo%d" % s])
                if self.kstop == 7:
                    continue
                P.dma(dr["VA"][tcol:tcol + 128, :], va[s][:], reads=["va%d" % s], eng="sp")
                P.dma(dr["rk"][tcol:tcol + 128, :], tko[s][:], reads=["tko%d" % s], eng="sp")
                P.dma(dr["rv"][tcol:tcol + 128, :], tvo[s][:], reads=["tvo%d" % s], eng="sp")
                P.dma(dr["rg"][tcol:tcol + 128, :], tgo[s][:], reads=["tgo%d" % s], eng="sp")
                P.dma(dr["pp"][tcol:tcol + 128, :], tpo[s][:], reads=["tpo%d" % s], eng="sp")
                if col0 >= CT and self.kstop != 10:
                    n = (tcol - CT) // 128
                    if self.kstop == 12:
                        continue
                    self.emit_U(tko[s][:], tvo[s][:], ["tko%d" % s, "tvo%d" % s], pU, "pU")
                    for d_ in range(2):
                        pwi = (self.NCL - 1 - n) if d_ == 0 else n
                        for p in range(2):
                            for hl in range(2):
                                r = slice(hl * 64, (hl + 1) * 64)
                                P.stt("dve", Lacc[r, d_, p, :], pU[r, d_ * 2 + p, hl * 64:(hl + 1) * 64],
                                      self.pw[r, d_, p, pwi:pwi + 1], Lacc[r, d_, p, :], ALU.mult, ALU.add,
                                      ["pU", "pw", "Lacc"], ["Lacc"])
        for d_ in range(2):
            P.dma(dr["Lout"][d_ * 128:(d_ + 1) * 128, :], Lacc[:, d_, :, :].rearrange("p a e -> p (a e)"), reads=["Lacc"])

    def phase_B1(self):
        P, dr = self.P, self.dr
        T, Tl, Tc, NCL, NKT = self.T, self.Tl, self.Tc, self.NCL, self.NKT
        self.ret_consts()
        lg, lgs = self.lg, self.lgs
        wo = self.sb("wo", [128, 8, D], BF16)
        for c in range(8):
            P.dma(wo[:, c, :], dr["w_out"][c * 128:(c + 1) * 128, :], writes=["wo"], reads=["wo"], eng="pool")
        KTa = self.sb("KTa", [128, CT + T], BF16)
        VAa = self.sb("VAa", [128, NKT, 132], BF16)
        P.dma(KTa[:, 0:CT], dr["KT"][:, 0:CT], writes=["KTa"])
        for r in range(4):
            P.dma(KTa[:, CT + r * Tl: CT + (r + 1) * Tl], dr["KTg"][r * 128:(r + 1) * 128, :], reads=["KTa"], writes=["KTa"])
        P.dma(VAa[:, 0:2, :], dr["VA"][0:CT, :].rearrange("(k p) c -> p k c", p=128), writes=["VAa"])
        for k0 in range(0, 4 * NCL, 8):
            k1 = min(k0 + 8, 4 * NCL)
            P.dma(VAa[:, 2 + k0: 2 + k1, :], dr["VAg"][k0 * 128:k1 * 128, :].rearrange("(k p) c -> p k c", p=128),
                  reads=["VAa"], writes=["VAa"])
        dm = self.sb("dm", [128, 4, 128], F32)
        P.dma(dm[:], dr["dmask"], writes=["dm"])
        DT = self.sb("DT", [128, 4, 128], F32)
        dtmp = self.sb("dtmp", [128, 128], F32)
        for h in range(4):
            P.act(DT[:, h, :], dm[:, 0, :], AF.Exp, ["dm", "lg"], ["DT"], scale=lg[:, h:h + 1])
            P.tt("dve", DT[:, h, :], DT[:, h, :], dm[:, 1, :], ALU.mult, ["DT", "dm"], ["DT"])
            P.act(dtmp[:], dm[:, 2, :], AF.Exp, ["dm", "lg"], ["dtmp"], scale=lg[:, 4 + h:5 + h])
            P.tt("dve", dtmp[:], dtmp[:], dm[:, 3, :], ALU.mult, ["dtmp", "dm"], ["dtmp"])
            P.tt("dve", DT[:, h, :], DT[:, h, :], dtmp[:], ALU.add, ["DT", "dtmp"], ["DT"])
        xe = self.sb("xe", [128, 2, 128], F32)
        P.dma(xe[:], dr["xiexp"], writes=["xe"])
        XiT = self.sb("XiT", [128, 2, 2, 128], BF16)
        for d_ in range(2):
            for p in range(2):
                P.act(XiT[:, d_, p, :], xe[:, d_, :], AF.Exp, ["xe", "lgs"], ["XiT"], scale=lgs[:, d_, p:p + 1])
        sel = self.sb("sel", [128, 4], F32)
        P.dma(sel[:], dr["sel"], writes=["sel"])
        def ldc(name, src, shape, eng="sp"):
            b = self.sb(name, shape, BF16)
            P.dma(b[:], src, writes=[name], eng=eng)
            return b
        Amain = ldc("Amain", dr["Amain"], [128, 12, 128])
        Anb = ldc("Anb", dr["Anb"], [128, 8, 128])
        Actx = ldc("Actx", dr["Actx"], [128, 8, 128])
        Ahalo = ldc("Ahalo", dr["Ahalo"], [64, 8, 128])
        pw_ = ldc("poolw", dr["pool_w"], [64, 4, 64], eng="pool")
        pscale = self.sb("pscale", [128, 2], F32)
        P.dma(pscale[:], dr["pool_s"], writes=["pscale"])
        Hg = self.sb("Hg", [64, 256], BF16)
        P.dma(Hg[:], dr["Hg"], writes=["Hg"])

        bS = [self.ps("bS%d" % i, [128, 512], F32) for i in range(3)]
        bO = self.ps("bO", [128, 512], F32)
        bR = self.ps("bR", [128, 512], F32)
        bY = self.ps("bY", [128, 512], F32)
        bU = self.ps("bU", [128, 4, 128], F32)
        self.n_pT = self.ps("n_pT", [128, 8, 128], BF16)
        pT = self.n_pT

        NCc = NCL + 2
        Sb = self.sb("Sb", [128, NCc, 2, 2, 64], BF16)
        outer = self.st
        tmpstack = contextlib.ExitStack()
        self.st = tmpstack
        self.u_kz = self.sb("u_kz", [128, 2, 256], BF16)
        rk_all = self.sb("rk_all", [128, NCc, 256], BF16)
        rv_all = self.sb("rv_all", [128, NCc, 256], BF16)
        for k0 in range(0, NCc, 8):
            k1 = min(k0 + 8, NCc)
            P.dma(rk_all[:, k0:k1, :], dr["rk"][k0 * 128:k1 * 128, :].rearrange("(k p) c -> p k c", p=128), reads=["rk_all"], writes=["rk_all"])
            P.dma(rv_all[:, k0:k1, :], dr["rv"][k0 * 128:k1 * 128, :].rearrange("(k p) c -> p k c", p=128), reads=["rv_all"], writes=["rv_all"])
        Uall = self.sb("Uall", [128, NCc, 2, 2, 64], F32)
        for n in range(NCc):
            self.emit_U(rk_all[:, n, :], rv_all[:, n, :], ["rk_all", "rv_all"], bU, "bU")
            for d_ in range(2):
                for p in range(2):
                    for hl in range(2):
                        r = slice(hl * 64, (hl + 1) * 64)
                        eng = "dve"
                        P.cp(eng, Uall[r, n, d_, p, :], bU[r, d_ * 2 + p, hl * 64:(hl + 1) * 64], ["bU"], [("Uall", n)])
        gC = self.pw[:, :, :, 1]
        sctx = self.sb("sctx", [128, 2, 2, 64], F32)
        for p in range(2):
            P.stt("dve", sctx[:, 0, p, :], Uall[:, 0, 0, p, :], self.pw[:, 0, p, 1:2], Uall[:, 1, 0, p, :], ALU.mult, ALU.add,
                  [("Uall", 0), ("Uall", 1), "pw"], ["sctx"])
            P.stt("dve", sctx[:, 1, p, :], Uall[:, 1, 1, p, :], self.pw[:, 1, p, 1:2], Uall[:, 0, 1, p, :], ALU.mult, ALU.add,
                  [("Uall", 0), ("Uall", 1), "pw"], ["sctx"])
        Lg = self.sb("Lg", [128, 4, 2, 128], F32)
        P.dma(Lg[:], dr["Lg"].rearrange("(r d p) c -> p r d c", r=4, d=2, p=128), writes=["Lg"])
        G = self.sb("G", [128, 2, 4, 2, 64], F32)
        for p in range(2):
            P.cp("dve", G[:, 0, 0, p, :], sctx[:, 0, p, :], ["sctx"], ["G"])
            for i in range(1, 4):
                P.stt("dve", G[:, 0, i, p, :], G[:, 0, i - 1, p, :], self.pw[:, 0, p, NCL:NCL + 1], Lg[:, i - 1, 0, p * 64:(p + 1) * 64],
                      ALU.mult, ALU.add, ["G", "pw", "Lg"], ["G"])
            P.cp("dve", G[:, 1, 3, p, :], sctx[:, 1, p, :], ["sctx"], ["G"])
            for i in (2, 1, 0):
                P.stt("dve", G[:, 1, i, p, :], G[:, 1, i + 1, p, :], self.pw[:, 1, p, NCL:NCL + 1], Lg[:, i + 1, 1, p * 64:(p + 1) * 64],
                      ALU.mult, ALU.add, ["G", "pw", "Lg"], ["G"])
        Gown = self.sb("Gown", [128, 2, 2, 64], F32)
        for d_ in range(2):
            P.ts("dve", Gown[:, d_, :, :], G[:, d_, 0, :, :], sel[:, 0:1], None, ALU.mult, None, ["G", "sel"], ["Gown"])
            for i in range(1, 4):
                P.stt("dve", Gown[:, d_, :, :], G[:, d_, i, :, :], sel[:, i:i + 1], Gown[:, d_, :, :], ALU.mult, ALU.add,
                      ["G", "sel", "Gown"], ["Gown"])
        run = self.sb("run", [128, 2, 2, 64], F32)
        P.op("pool", lambda e: e.memset(Sb[:, 0:2, :, :, :], 0.0), (), [("Sb", 0), ("Sb", 1)])
        P.cp("dve", Sb[:, 1, 0, :, :], Uall[:, 0, 0, :, :], [("Uall", 0), ("Sb", 1)], [("Sb", 1)])
        P.cp("dve", Sb[:, 0, 1, :, :], Uall[:, 1, 1, :, :], [("Uall", 1), ("Sb", 0)], [("Sb", 0)])
        P.cp("dve", run[:, 0, :, :], Gown[:, 0, :, :], ["Gown"], ["run"])
        for n in range(NCL):
            P.cp("pool", Sb[:, 2 + n, 0, :, :], run[:, 0, :, :], ["run"], [("Sb", 2 + n)])
            if n < NCL - 1:
                for p in range(2):
                    P.stt("dve", run[:, 0, p, :], run[:, 0, p, :], self.pw[:, 0, p, 1:2], Uall[:, 2 + n, 0, p, :], ALU.mult, ALU.add,
                          ["run", "pw", ("Uall", 2 + n)], ["run"])
        P.cp("dve", run[:, 1, :, :], Gown[:, 1, :, :], ["Gown"], ["run"])
        for n in range(NCL - 1, -1, -1):
            P.cp("pool", Sb[:, 2 + n, 1, :, :], run[:, 1, :, :], ["run", ("Sb", 2 + n)], [("Sb", 2 + n)])
            if n > 0:
                for p in range(2):
                    P.stt("dve", run[:, 1, p, :], run[:, 1, p, :], self.pw[:, 1, p, 1:2], Uall[:, 2 + n, 1, p, :], ALU.mult, ALU.add,
                          ["run", "pw", ("Uall", 2 + n)], ["run"])

        P.barrier()
        tmpstack.close()
        self.st = outer

        QTb = [self.sb("QTb%d" % i, [128, 4, 512], BF16) for i in range(2)]
        rqb = [self.sb("rqb%d" % i, [128, 2, 512], BF16) for i in range(2)]
        rkb = [self.sb("rkb%d" % i, [128, 2, 512], BF16) for i in range(2)]
        rgb = [self.sb("rgb%d" % i, [128, 4, 256], BF16) for i in range(2)]
        rvb = [self.sb("rvb%d" % i, [128, 4, 256], BF16) for i in range(2)]
        ppb = [self.sb("ppb%d" % i, [128, 6, 256], BF16) for i in range(2)]
        PT = [self.sb("PT%d" % i, [128, 512], BF16) for i in range(4)]
        zeros = self.sb("zeros", [128, 260], BF16)
        P.op("pool", lambda e: e.memset(zeros[:], 0.0), (), ["zeros"])
        mix = [self.sb("mix%d" % i, [128, D], BF16) for i in range(2)]
        mixT = [self.sb("mixT%d" % i, [128, 8, 128], BF16) for i in range(2)]
        rden = self.sb("rden", [128, 4], F32)
        sT = self.sb("sT", [128, 4, 128], BF16)
        qx = self.sb("qx", [128, 2, 2, 128], BF16)
        osq = self.sb("osq", [128, 256], F32)
        ob = self.sb("ob", [128, 256], F32)
        st1 = self.sb("st1", [128, 4], F32)
        st2 = self.sb("st2", [128, 4], F32)
        mean = self.sb("mean", [128, 4], F32)
        yn = self.sb("yn", [128, 256], F32)
        sg = self.sb("sg", [128, 256], F32)
        mxd = self.sb("mxd", [64, 4, 128], BF16)
        xt = [self.sb("xt%d" % i, [128, D], F32) for i in range(2)]
        mjunk = self.sb("mjunk", [128, D], F32)
        mss = self.sb("mss", [128, 1], F32)
        mtmp = self.sb("mtmp", [128, D], F32)
        xo = [self.sb("xo%d" % i, [128, D], F32) for i in range(2)]

        ptc = [0]
        gti = 0
        if self.kb == 1:
            return
        for bi, (col0, ntok) in enumerate(self.blocks):
            isctx = col0 < CT
            if isctx and not self.need_ctx:
                continue
            nt = ntok // 128
            sset = 1 if isctx else 0
            s2 = bi % 2
            Qb, Qk = QTb[s2], "QTb%d" % s2
            P.dma(Qb[:, :, 0:ntok], dr["QT"][:, :, col0:col0 + ntok], writes=[Qk])
            P.dma(rqb[s2][:, :, 0:ntok], dr["rqT"][:, :, col0:col0 + ntok], writes=["rqb%d" % s2])
            P.dma(rkb[s2][:, :, 0:ntok], dr["rkT"][:, :, col0:col0 + ntok], writes=["rkb%d" % s2])
            P.dma(rgb[s2][:, 0:nt, :], dr["rg"][col0:col0 + ntok, :].rearrange("(k p) c -> p k c", p=128), writes=["rgb%d" % s2])
            P.dma(rvb[s2][:, 0:nt, :], dr["rv"][col0:col0 + ntok, :].rearrange("(k p) c -> p k c", p=128), writes=["rvb%d" % s2])
            lo_t = col0 - 128 if (col0 > CT or (isctx and col0 > 0)) else col0
            hi_t = col0 + ntok + 128
            if isctx:
                hi_t = min(hi_t, CT)
            else:
                hi_t = min(hi_t, Tc)
            k0 = (lo_t - (col0 - 128)) // 128
            nk = (hi_t - lo_t) // 128
            P.dma(ppb[s2][:, k0:k0 + nk, :], dr["pp"][lo_t:hi_t, :].rearrange("(k p) c -> p k c", p=128), writes=["ppb%d" % s2])
            kts = list(range(2)) if isctx else list(range(NKT))
            for t in range(nt):
                tcol = col0 + t * 128
                mx_, mxk = mix[gti % 2], "mix%d" % (gti % 2)
                for kvh in range(2):
                    r = slice(kvh * 64, (kvh + 1) * 64)
                    P.mm(bO[:, 0:260], zeros[:, 0:128], zeros[:, 0:260], True, True, ["zeros"], ["bO"])
                    def issue_S(ki_):
                        kt_ = kts[ki_]
                        S_ = bS[ptc[0] % 3]
                        Sk = "bS%d" % (ptc[0] % 3)
                        Pt_, Pk_ = PT[ptc[0] % 4], "PT%d" % (ptc[0] % 4)
                        ptc[0] += 1
                        P.mm(S_[:, :].rearrange("p (g q) -> p g q", g=4), KTa[r, kt_ * 128:(kt_ + 1) * 128],
                             Qb[r, :, t * 128:(t + 1) * 128], True, True, ["KTa", Qk], [Sk])
                        P.op("act", lambda e, Pt_=Pt_, S_=S_: e.activation(Pt_[:], S_[:], AF.Exp, scale=0.125),
                             [Sk], [Pk_], nosame=True)
                        return Pt_, Pk_
                    pend = [issue_S(0)]
                    if len(kts) > 1:
                        pend.append(issue_S(1))
                    for ki, kt in enumerate(kts):
                        if ki + 2 < len(kts):
                            pend.append(issue_S(ki + 2))
                        Pt, Pk = pend.pop(0)
                        for g in range(4):
                            P.mm(bO[:, g * 65:(g + 1) * 65], Pt[:, g * 128:(g + 1) * 128], VAa[:, kt, kvh * 66:kvh * 66 + 65],
                                 False, ki == len(kts) - 1, [Pk, "VAa"], ["bO"], skip_group_check=True)
                    for g in range(4):
                        P.op("dve", lambda e, g=g: e.reciprocal(rden[:, g:g + 1], bO[:, g * 65 + 64: g * 65 + 65]), ["bO"], ["rden"])
                    for g in range(4):
                        h = kvh * 4 + g
                        P.ts("dve", mx_[:, h * 64:(h + 1) * 64], bO[:, g * 65: g * 65 + 64], rden[:, g:g + 1], None, ALU.mult, None,
                             ["bO", "rden"], [mxk])
                if self.kb == 2:
                    continue
                n = tcol // 128
                for d_ in range(2):
                    for p in range(2):
                        P.tt("pool", qx[:, d_, p, :], rqb[s2][:, p, t * 128:(t + 1) * 128], XiT[:, d_, p, :], ALU.mult,
                             ["rqb%d" % s2, "XiT"], ["qx"])
                bUf = bU[:, :, :].rearrange("p a b -> p (a b)")
                for h in range(4):
                    p, hl = h // 2, h % 2
                    r = slice(hl * 64, (hl + 1) * 64)
                    bank, bkey = (bR, "bR") if hl == 0 else (bUf, "bU")
                    P.mm(bank[:, p * 128:(p + 1) * 128], rkb[s2][r, p, t * 128:(t + 1) * 128], rqb[s2][r, p, t * 128:(t + 1) * 128],
                         True, True, ["rkb%d" % s2, "rqb%d" % s2], [bkey])
                for h in range(4):
                    p, hl = h // 2, h % 2
                    bank, bkey = (bR, "bR") if hl == 0 else (bUf, "bU")
                    P.tt("dve", sT[:, h, :], bank[:, p * 128:(p + 1) * 128], DT[:, h, :], ALU.mult, [bkey, "DT"], ["sT"])
                for h in range(4):
                    p, hl = h // 2, h % 2
                    r = slice(hl * 64, (hl + 1) * 64)
                    if hl == 0:
                        oreg, okey = bY[:, p * 64:(p + 1) * 64], "bY"
                    else:
                        oreg, okey = bO[:, 320 + p * 64: 320 + (p + 1) * 64], "bO"
                    P.mm(oreg, sT[:, h, :], rvb[s2][:, t, h * 64:(h + 1) * 64], True, False,
                         ["sT", "rvb%d" % s2], [okey], skip_group_check=True)
                    P.mm(oreg, qx[r, 0, p, :], Sb[r, n, 0, p, :], False, False,
                         ["qx", ("Sb", n)], [okey], skip_group_check=True)
                    P.mm(oreg, qx[r, 1, p, :], Sb[r, n, 1, p, :], False, True,
                         ["qx", ("Sb", n)], [okey], skip_group_check=True)
                ob4 = ob[:].rearrange("p (a b e) -> p a b e", a=2, b=2)
                P.cp("dve", ob4[:, :, 0, :], bY[:, 0:128].rearrange("p (a e) -> p a e", a=2), ["bY"], ["ob"])
                P.cp("dve", ob4[:, :, 1, :], bO[:, 320:448].rearrange("p (a e) -> p a e", a=2), ["bO"], ["ob"])
                P.op("dve", lambda e: e.reduce_sum(st1[:], ob[:].rearrange("p (h e) -> p h e", h=4), AX.X), ["ob"], ["st1"])
                P.act(osq[:], ob[:], AF.Square, ["ob"], ["osq"])
                P.op("dve", lambda e: e.reduce_sum(st2[:], osq[:].rearrange("p (h e) -> p h e", h=4), AX.X), ["osq"], ["st2"])
                P.ts("dve", mean[:], st1[:], 1.0 / 64, None, ALU.mult, None, ["st1"], ["mean"])
                P.tt("dve", st1[:], mean[:], mean[:], ALU.mult, ["mean"], ["st1"])
                P.stt("dve", st2[:], st2[:], 1.0 / 64, st1[:], ALU.mult, ALU.subtract, ["st2", "st1"], ["st2"])
                P.act(st2[:], st2[:], AF.Sqrt, ["st2"], ["st2"], bias=EPS)
                P.op("dve", lambda e: e.reciprocal(st2[:], st2[:]), ["st2"], ["st2"])
                for h in range(4):
                    P.ts("dve", yn[:, h * 64:(h + 1) * 64], ob[:, h * 64:(h + 1) * 64], mean[:, h:h + 1], st2[:, h:h + 1],
                         ALU.subtract, ALU.mult, ["ob", "mean", "st2"], ["yn"])
                P.act(sg[:], rgb[s2][:, t, :], AF.Silu, ["rgb%d" % s2], ["sg"])
                P.tt("pool", mx_[:, 512:768], yn[:], sg[:], ALU.mult, ["yn", "sg"], [mxk])
                if self.kb == 3:
                    continue
                mT_, mTk = mixT[gti % 2], "mixT%d" % (gti % 2)
                for c in range(6):
                    P.tr(pT[:, c, :], mx_[:, c * 128:(c + 1) * 128], self.identb[:], [mxk, "identb"], ["n_pT"])
                P.cp("act", mT_[:, 0:6, :], pT[:, 0:6, :], ["n_pT"], [mTk])
                first = (not isctx and tcol == CT) or (isctx and tcol == 0)
                last = (not isctx and tcol == Tc - 128) or (isctx and tcol == CT - 128)
                for g in range(4):
                    gs = slice(g * 64, (g + 1) * 64)
                    if isctx:
                        Am = Actx[:, (0 if first else 4) + g, :]
                    else:
                        Am = Amain[:, (0 if first else (8 if last else 4)) + g, :]
                    ops_ = [(ppb[s2][:, t + 1, gs], Am)]
                    if not first:
                        ops_.append((ppb[s2][:, t, gs], Anb[:, g, :]))
                    elif not isctx:
                        ops_.append((Hg[:, gs], Ahalo[:, g, :]))
                    if not last:
                        ops_.append((ppb[s2][:, t + 2, gs], Anb[:, 4 + g, :]))
                    elif not isctx:
                        ops_.append((Hg[:, gs], Ahalo[:, 4 + g, :]))
                    mbank, mkey = (bR, "bR") if g < 2 else (bUf, "bU")
                    mcol = 256 + (g % 2) * 128
                    for oi, (l_, r_) in enumerate(ops_):
                        P.mm(mbank[0:64, mcol:mcol + 128], l_, r_, oi == 0, oi == len(ops_) - 1,
                             ["ppb%d" % s2, "Amain", "Anb", "Actx", "Ahalo", "Hg"], [mkey], skip_group_check=True)
                P.cp("act", mxd[:, 0:2, :].rearrange("p g t -> p (g t)"), bR[0:64, 256:512], ["bR"], ["mxd"])
                P.cp("act", mxd[:, 2:4, :].rearrange("p g t -> p (g t)"), bUf[0:64, 256:512], ["bU"], ["mxd"])
                for g in range(4):
                    P.mm(bY[(g % 2) * 64:(g % 2 + 1) * 64, 256 + (g // 2) * 128: 256 + (g // 2 + 1) * 128], pw_[:, g, :], mxd[:, g, :],
                         True, True, ["poolw", "mxd"], ["bYp"], skip_group_check=True)
                for c in range(2):
                    P.ts("dve", mT_[:, 6 + c, :], bY[:, 256 + c * 128: 256 + (c + 1) * 128], pscale[:, c:c + 1], None, ALU.mult, None,
                         ["bYp", "pscale"], [mTk])
                if self.kb == 4:
                    continue
                for hf in range(2):
                    for c in range(8):
                        P.mm(bS[hf][:, :], mT_[:, c, :], wo[:, c, hf * 512:(hf + 1) * 512], c == 0, c == 7, [mTk, "wo"], ["bS%d" % hf])
                x_, xk = xt[gti % 2], "xt%d" % (gti % 2)
                P.dma(x_[:], self.src_x(tcol, 128), writes=[xk])
                P.act(mjunk[:, 0:512], bS[0][:, :], AF.Square, ["bS0"], ["mjunk", "mss"], accum_out=mss[:])
                mss2 = st1[:, 0:1]
                P.act(mjunk[:, 512:1024], bS[1][:, :], AF.Square, ["bS1"], ["mjunk", "st1"], accum_out=mss2)
                P.tt("dve", mss[:], mss[:], mss2, ALU.add, ["mss", "st1"], ["mss"])
                P.ts("dve", mss[:], mss[:], 1.0 / D, EPS, ALU.mult, ALU.add, ["mss"], ["mss"])
                P.act(mss[:], mss[:], AF.Sqrt, ["mss"], ["mss"])
                P.op("dve", lambda e: e.reciprocal(mss[:], mss[:]), ["mss"], ["mss"])
                for hf in range(2):
                    P.stt("dve", mtmp[:, hf * 512:(hf + 1) * 512], bS[hf][:, :], mss[:, 0:1], self.gv[:, sset, hf * 512:(hf + 1) * 512],
                          ALU.mult, ALU.mult, ["bS%d" % hf, "mss", "gv"], ["mtmp"])
                xo_, xok = xo[gti % 2], "xo%d" % (gti % 2)
                P.tt("pool", xo_[:], mtmp[:], x_[:], ALU.add, ["mtmp", xk], [xok])
                dst = dr["cout"][tcol:tcol + 128, :] if isctx else dr["xout"][tcol - CT: tcol - CT + 128, :]
                P.dma(dst, xo_[:], reads=[xok], eng="pool")
                gti += 1

    def phase_B2(self):
        P, dr = self.P, self.dr
        E = self.n_exp
        self.alloc_norm()
        blocks = [b for b in self.blocks if not (b[0] < CT and not self.need_ctx)]
        xt = [self.sb("xt%d" % i, [128, D], F32) for i in range(2)]
        hT = [self.sb("hT0", [128, 8, 512], BF16)] * 2
        w1 = self.sb("w1", [128, 8, DFF], BF16)
        w3 = self.sb("w3", [128, 8, DFF], BF16)
        w2 = self.sb("w2", [128, NF, D], BF16)
        gT = self.sb("gT", [128, NF, 512], BF16)
        sa = self.sb("sa", [128, 512], F32)
        pa = [self.ps("pa%d" % i, [128, 512], F32) for i in range(2)]
        pb = [self.ps("pb%d" % i, [128, 512], F32) for i in range(2)]
        py = [self.ps("py%d" % i, [128, 512], F32) for i in range(2)]
        pr = self.ps("pr", [128, 8], F32)
        ntl = self.Tc // 128
        gates = self.sb("gates", [128, ntl, 8], F32)
        if E > 1:
            rtf = self.sb("rtf", [128, 8, 8], F32)
            rtb = self.sb("rtb", [128, 8, 8], BF16)
            P.dma(rtf[:], dr["router"].rearrange("(c p) e -> p c e", p=128), writes=["rtf"])
            P.cp("dve", rtb[:], rtf[:], ["rtf"], ["rtb"])
            lgt = self.sb("lgt", [128, 8], F32)
            m8 = self.sb("m8", [128, 8], F32)
            msk = self.sb("msk", [128, 8], F32)
            ex = self.sb("ex", [128, 8], F32)
            den = self.sb("den", [128, 1], F32)
        ti = 0
        for bi, (col0, ntok) in enumerate(blocks):
            nt = ntok // 128
            hb, hk = hT[0], "hT0"
            sset = 1 if col0 < CT else 0
            for t in range(nt):
                x_, xk = xt[ti % 2], "xt%d" % (ti % 2)
                src = dr["cout"][col0 + t * 128: col0 + (t + 1) * 128, :] if col0 < CT else \
                    dr["xout"][col0 - CT + t * 128: col0 - CT + (t + 1) * 128, :]
                P.dma(x_[:], src, writes=[xk])
                self.norm_tile_to_hT(x_[:], xk, hb, hk, t * 128, sset, 2, ti)
                ti += 1
                if E > 1:
                    tl = (col0 + t * 128) // 128
                    for c in range(8):
                        P.mm(pr[:, :], hb[:, c, t * 128:(t + 1) * 128], rtb[:, c, :], c == 0, c == 7, [hk, "rtb"], ["pr"])
                    P.cp("dve", lgt[:], pr[:, :], ["pr"], ["lgt"])
                    P.op("dve", lambda e: e.reduce_max(m8[:, 0:1], lgt[:], AX.X), ["lgt"], ["m8"])
                    P.ts("dve", msk[:], lgt[:], m8[:, 0:1], None, ALU.is_equal, None, ["lgt", "m8"], ["msk"])
                    P.stt("dve", ex[:], msk[:], -1e30, lgt[:], ALU.mult, ALU.add, ["msk", "lgt"], ["ex"])
                    P.op("dve", lambda e: e.reduce_max(m8[:, 1:2], ex[:], AX.X), ["ex", "m8"], ["m8"])
                    P.ts("dve", msk[:], lgt[:], m8[:, 1:2], None, ALU.is_ge, None, ["lgt", "m8"], ["msk"])
                    P.ts("dve", ex[:], lgt[:], m8[:, 0:1], None, ALU.subtract, None, ["lgt", "m8"], ["ex"])
                    P.act(ex[:], ex[:], AF.Exp, ["ex"], ["ex"])
                    P.tt("dve", ex[:], ex[:], msk[:], ALU.mult, ["ex", "msk"], ["ex"])
                    P.op("dve", lambda e: e.reduce_sum(den[:], ex[:], AX.X), ["ex"], ["den"])
                    P.op("dve", lambda e: e.reciprocal(den[:], den[:]), ["den"], ["den"])
                    P.ts("dve", gates[:, tl, :], ex[:], den[:, 0:1], None, ALU.mult, None, ["ex", "den"], ["gates"])
            P.dma(dr["hTd"][bi, :, :, 0:ntok], hb[:, :, 0:ntok], reads=[hk], writes=[("hTd", bi)], eng="pool")
        yo = [self.sb("yo%d" % i, [128, D], F32) for i in range(2)]
        ya = [self.sb("ya0", [128, D], F32)] * 2
        yi = 0
        fi = 0
        for e_ in range(E):
            for c in range(8):
                P.dma(w1[:, c, :], dr["w1"][e_, c * 128:(c + 1) * 128, :], reads=["w1"], writes=["w1"], eng="pool")
                P.dma(w3[:, c, :], dr["w3"][e_, c * 128:(c + 1) * 128, :], reads=["w3"], writes=["w3"], eng="pool")
            for f in range(NF):
                P.dma(w2[:, f, :], dr["w2"][e_, f * 128:(f + 1) * 128, :], reads=["w2"], writes=["w2"], eng="pool")
            for bi, (col0, ntok) in enumerate(blocks):
                nt = ntok // 128
                hb, hk = hT[0], "hT0"
                P.dma(hb[:, :, 0:ntok], dr["hTd"][bi, :, :, 0:ntok], writes=[hk], reads=[("hTd", bi)])
                for f in range(NF):
                    a_, ak = pa[fi % 2], "pa%d" % (fi % 2)
                    b_, bk = pb[fi % 2], "pb%d" % (fi % 2)
                    fi += 1
                    for c in range(8):
                        P.mm(a_[:, 0:ntok], w1[:, c, f * 128:(f + 1) * 128], hb[:, c, 0:ntok], c == 0, c == 7, ["w1", hk], [ak])
                    for c in range(8):
                        P.mm(b_[:, 0:ntok], w3[:, c, f * 128:(f + 1) * 128], hb[:, c, 0:ntok], c == 0, c == 7, ["w3", hk], [bk])
                    P.act(sa[:, 0:ntok], a_[:, 0:ntok], AF.Silu, [ak], ["sa"])
                    P.tt("dve", gT[:, f, 0:ntok], sa[:, 0:ntok], b_[:, 0:ntok], ALU.mult, ["sa", bk], ["gT"])
                for t in range(nt):
                    tcol = col0 + t * 128
                    tl = tcol // 128
                    for hf in range(2):
                        for f in range(NF):
                            P.mm(py[hf][:, :], gT[:, f, t * 128:(t + 1) * 128], w2[:, f, hf * 512:(hf + 1) * 512], f == 0, f == NF - 1,
                                 ["gT", "w2"], ["py%d" % hf])
                    yo_, yok = yo[yi % 2], "yo%d" % (yi % 2)
                    ya_, yak = ya[0], "ya0"
                    yi += 1
                    acc = dr["accd"][tcol:tcol + 128, :]
                    if E == 1:
                        for hf in range(2):
                            P.cp("act" if hf == 0 else "dve", yo_[:, hf * 512:(hf + 1) * 512], py[hf][:, :], ["py%d" % hf], [yok])
                    else:
                        if e_ > 0:
                            P.dma(ya_[:], acc, writes=[yak], reads=[("acc", tl)])
                        for hf in range(2):
                            if e_ == 0:
                                P.ts("dve", yo_[:, hf * 512:(hf + 1) * 512], py[hf][:, :], gates[:, tl, e_:e_ + 1], None, ALU.mult, None,
                                     ["py%d" % hf, "gates"], [yok])
                            else:
                                P.stt("dve", yo_[:, hf * 512:(hf + 1) * 512], py[hf][:, :], gates[:, tl, e_:e_ + 1],
                                      ya_[:, hf * 512:(hf + 1) * 512], ALU.mult, ALU.add, ["py%d" % hf, "gates", yak], [yok])
                    P.dma(acc, yo_[:], reads=[yok], writes=[("acc", tl)], eng="pool")
        mss = self.sb("fss", [128, 1], F32)
        ti = 0
        for bi, (col0, ntok) in enumerate(blocks):
            sset = 1 if col0 < CT else 0
            for t in range(ntok // 128):
                tcol = col0 + t * 128
                tl = tcol // 128
                x_, xk = xt[ti % 2], "xt%d" % (ti % 2)
                ya_, yak = ya[0], "ya0"
                yo_, yok = yo[ti % 2], "yo%d" % (ti % 2)
                ti += 1
                dst = dr["cout"][tcol:tcol + 128, :] if col0 < CT else dr["xout"][tcol - CT: tcol - CT + 128, :]
                P.dma(x_[:], dst, writes=[xk], reads=[("xres", tl)])
                P.dma(ya_[:], dr["accd"][tcol:tcol + 128, :], writes=[yak], reads=[("acc", tl)])
                P.act(self.n_junk[:], ya_[:], AF.Square, [yak], ["n_junk", "fss"], accum_out=mss[:])
                P.ts("dve", mss[:], mss[:], 1.0 / D, EPS, ALU.mult, ALU.add, ["fss"], ["fss"])
                P.act(mss[:], mss[:], AF.Sqrt, ["fss"], ["fss"])
                P.op("dve", lambda e: e.reciprocal(mss[:], mss[:]), ["fss"], ["fss"])
                P.stt("dve", yo_[:], ya_[:], mss[:, 0:1], self.gv[:, sset, :], ALU.mult, ALU.mult, [yak, "fss", "gv"], [yok])
                P.tt("pool", yo_[:], yo_[:], x_[:], ALU.add, [yok, xk], [yok])
                P.dma(dst, yo_[:], reads=[yok], writes=[("xres", tl)], eng="pool")


def _pool_weight(L, w, s, t):
    lo = np.clip(t - w // 2, 0, L)
    hi = np.clip(t + w // 2, 0, L)
    val = ((s >= lo) & (s < hi)).astype(np.float64) / (hi - lo)
    val = val - (s == t)
    return val


def host_consts(T, j):
    Tl = T // 4
    Tc = CT + Tl
    NCL = Tl // 128
    cst = {}
    cst["ident"] = np.eye(128, dtype=np.float32)
    bo = np.zeros((128, 128), np.float32)
    bo[0:64, 0:64] = 1
    bo[64:, 64:] = 1
    cst["bones"] = bo
    tpos = np.arange(j * Tl, (j + 1) * Tl)
    row = (tpos // 64).astype(np.float32)
    colp = (tpos % 64).astype(np.float32)
    inv = (np.float32(10000.0) ** (-np.arange(16, dtype=np.float32) / np.float32(16))).astype(np.float32)
    cosT = np.ones((64, Tc), np.float32)
    sinT = np.zeros((64, Tc), np.float32)
    for a, pos in enumerate((row, colp)):
        ang = pos[None, :] * inv[:, None]
        for b in range(2):
            rows = slice(a * 32 + b * 16, a * 32 + b * 16 + 16)
            cosT[rows, CT:] = np.cos(ang)
            sinT[rows, CT:] = np.sin(ang) * (-1.0 if b == 0 else 1.0)
    cst["cosT"] = np.concatenate([cosT, cosT], 0)
    cst["sinT"] = np.concatenate([sinT, sinT], 0)
    m = np.arange(128, dtype=np.float32)
    cst["zexp"] = np.stack([C - 1 - m, m], 1).astype(np.float32)
    cst["nC"] = np.tile((np.arange(NCL + 2, dtype=np.float32) * C)[None, :], (128, 1)).astype(np.float32)
    mm, cc = np.meshgrid(m, m, indexing="ij")
    dmask = np.stack([np.maximum(cc - mm, 0), (cc >= mm).astype(np.float32),
                      np.maximum(mm - cc, 0), (mm >= cc).astype(np.float32)], 1).astype(np.float32)
    cst["dmask"] = dmask
    cst["xiexp"] = np.tile(np.stack([m + 1, C - m], 0)[None], (128, 1, 1)).astype(np.float32)
    sel = np.zeros((128, 4), np.float32)
    sel[:, j] = 1
    cst["sel"] = sel
    wins = (2, 4, 8, 16)
    s_loc = np.arange(128)[:, None]
    t_loc = np.arange(128)[None, :]
    Amain = np.zeros((128, 12, 128), np.float32)
    Anb = np.zeros((128, 8, 128), np.float32)
    Actx = np.zeros((128, 8, 128), np.float32)
    Ahalo = np.zeros((64, 8, 128), np.float32)
    for g, w in enumerate(wins):
        base_f = j * Tl
        Amain[:, 0 + g, :] = _pool_weight(T, w, base_f + s_loc, base_f + t_loc)
        mid = T // 2 // 128 * 128 if T >= 512 else 128
        midb = 128 * (T // 256)
        Amain[:, 4 + g, :] = _pool_weight(T + 4096, w, 2048 + s_loc, 2048 + t_loc)
        base_l = (j + 1) * Tl - 128
        Amain[:, 8 + g, :] = _pool_weight(T, w, base_l + s_loc, base_l + t_loc)
        Anb[:, g, :] = _pool_weight(T + 4096, w, 2048 - 128 + s_loc, 2048 + t_loc)
        Anb[:, 4 + g, :] = _pool_weight(T + 4096, w, 2048 + 128 + s_loc, 2048 + t_loc)
        Actx[:, g, :] = _pool_weight(CT, w, s_loc, t_loc)
        Actx[:, 4 + g, :] = _pool_weight(CT, w, 128 + s_loc, 128 + t_loc)
        for i in range(4):
            for r in range(16):
                spos = i * Tl + r if r < 8 else (i + 1) * Tl - 16 + r
                if i == j:
                    continue
                Ahalo[i * 16 + r, g, :] = _pool_weight(T, w, np.array([[spos]]), base_f + t_loc)[0]
                Ahalo[i * 16 + r, 4 + g, :] = _pool_weight(T, w, np.array([[spos]]), base_l + t_loc)[0]
    cst["Amain"], cst["Anb"], cst["Actx"], cst["Ahalo"] = [a.astype(NPBF) for a in (Amain, Anb, Actx, Ahalo)]
    return cst


def _rope_perm():
    perm = np.zeros(64, np.int64)
    for a in range(2):
        for b in range(2):
            for f in range(16):
                perm[a * 32 + b * 16 + f] = a * 32 + (1 - b) * 16 + f
    return perm


def layer_weights(inp, i):
    perm = _rope_perm()
    w_in = inp["w_in"][i]
    cols = {"aq": 0, "ak": 512, "av": 640, "rq": 768, "rk": 1024, "rv": 1280, "rg": 1536, "pp": 1792}
    fm = []
    for g in range(4):
        fm += [cols["aq"] + g * 64 + d for d in range(64)] + [cols["aq"] + (4 + g) * 64 + d for d in range(64)]
    for g in range(4):
        fm += [cols["aq"] + g * 64 + perm[d] for d in range(64)] + [cols["aq"] + (4 + g) * 64 + perm[d] for d in range(64)]
    fm += [cols["ak"] + d for d in range(128)]
    fm += [cols["ak"] + kv * 64 + perm[d] for kv in range(2) for d in range(64)]
    fm += [cols["rq"] + d for d in range(256)]
    fm += [cols["rk"] + d for d in range(256)]
    tm = [cols["av"] + d for d in range(128)] + [cols["rk"] + d for d in range(256)] + [cols["rv"] + d for d in range(256)] + \
         [cols["rg"] + d for d in range(256)] + [cols["pp"] + d for d in range(256)]
    gq, gk = inp["q_norm"][i], inp["k_norm"][i]
    gains = np.stack([np.tile(gq, 2), np.tile(gq[perm], 2), np.tile(gk, 2), np.tile(gk[perm], 2)], 1).astype(np.float32)
    ps = inp["pool_scale"][i]
    out = {
        "w_fm": np.ascontiguousarray(w_in[:, fm]),
        "w_tm": np.ascontiguousarray(w_in[:, tm]),
        "gains": gains,
        "w_mod": inp["w_mod"][i], "b_mod": inp["b_mod"][i],
        "norms": np.stack([inp["norm_pre_mix"][i], inp["norm_post_mix"][i], inp["norm_pre_ffn"][i], inp["norm_post_ffn"][i]], 0),
        "decay": np.ascontiguousarray(inp["ret_decay_logit"][i].reshape(8)),
        "w_out": inp["w_out"][i],
        "pool_w": np.ascontiguousarray(np.transpose(inp["pool_w"][i], (1, 0, 2))),
        "pool_s": np.ascontiguousarray(ps.reshape(2, 128).T),
    }
    if i % 2 == 0:
        out["w1"], out["w3"], out["w2"] = inp["ffn_w1"][i // 2][None], inp["ffn_w3"][i // 2][None], inp["ffn_w2"][i // 2][None]
    else:
        out["w1"], out["w3"], out["w2"] = inp["moe_w1"][i // 2], inp["moe_w3"][i // 2], inp["moe_w2"][i // 2]
        out["router"] = inp["moe_router"][i // 2]
    return out


_NC_CACHE = {}


def get_nc(T, layer, phases, n_exp):
    key = (T, layer, phases, n_exp)
    if key not in _NC_CACHE:
        b = Builder(T, layer, phases, n_exp)
        nc = b.build()
        _NC_CACHE[key] = (nc, sorted(k for k in b.dr))
    return _NC_CACHE[key][0]


A_IN = ["xin", "cin", "cvec", "w_mod", "b_mod", "norms", "decay", "ident", "zexp", "nC", "w_fm", "w_tm", "gains", "bones",
        "cosT", "sinT"]
A_OUT = ["QT", "KT", "VA", "rqT", "rkT", "rk", "rv", "rg", "pp", "Lout"]
B_IN = ["xin", "cin", "cvec", "w_mod", "b_mod", "norms", "decay", "ident", "zexp", "nC", "QT", "KT", "VA", "rqT", "rkT", "rk",
        "rv", "rg", "pp", "KTg", "VAg", "Lg", "Hg", "w_out", "dmask", "xiexp", "sel", "Amain", "Anb", "Actx", "Ahalo", "pool_w",
        "pool_s", "w1", "w3", "w2"]


def run_model(inp, n_layers=2):
    x = np.asarray(inp["x"], np.float32)
    B, T, _ = x.shape
    Tl = T // 4
    ncore = 8
    xs = [np.ascontiguousarray(x[c // 4, (c % 4) * Tl:((c % 4) + 1) * Tl]) for c in range(ncore)]
    cs = [np.ascontiguousarray(np.asarray(inp["ctx"], np.float32)[c // 4]) for c in range(ncore)]
    csts = [host_consts(T, c % 4) for c in range(ncore)]
    cvecs = [np.stack([inp["c"][c // 4], inp["c_ctx"]], 0).astype(np.float32) for c in range(ncore)]
    for i in range(n_layers):
        lw = layer_weights(inp, i)
        n_exp = 1 if i % 2 == 0 else NE
        ncA = get_nc(T, i, "A", n_exp)
        maps = []
        for c in range(ncore):
            m = {"xin": xs[c], "cin": cs[c], "cvec": cvecs[c]}
            m.update(lw)
            m.update(csts[c])
            maps.append({k: np.ascontiguousarray(m[k]) for k in A_IN})
        resA = run_bass_kernel_spmd(ncA, maps, core_ids=list(range(ncore))).results
        ncB = get_nc(T, i, "B", n_exp)
        maps = []
        for c in range(ncore):
            b = c // 4
            grp = [resA[b * 4 + r] for r in range(4)]
            m = {"xin": xs[c], "cin": cs[c], "cvec": cvecs[c]}
            m.update(lw)
            m.update(csts[c])
            for k in A_OUT:
                m[k] = resA[c][k]
            m["KTg"] = np.concatenate([g["KT"][:, CT:] for g in grp], 0)
            m["VAg"] = np.concatenate([g["VA"][CT:] for g in grp], 0)
            m["Lg"] = np.concatenate([g["Lout"] for g in grp], 0)
            m["Hg"] = np.concatenate([np.concatenate([g["pp"][CT:CT + 8], g["pp"][-8:]], 0) for g in grp], 0)
            names = list(B_IN) + (["router"] if n_exp > 1 else [])
            maps.append({k: np.ascontiguousarray(m[k]) for k in names})
        resB = run_bass_kernel_spmd(ncB, maps, core_ids=list(range(ncore))).results
        xs = [resB[c]["xout"] for c in range(ncore)]
        if i == 0:
            cs = [resB[c]["cout"] for c in range(ncore)]
    out = np.zeros((B, T, D), np.float32)
    for c in range(ncore):
        out[c // 4, (c % 4) * Tl:((c % 4) + 1) * Tl] = xs[c]
    return out


def kernel(**inputs):
    inp = {k: np.asarray(v) for k, v in inputs.items()}
    return run_model(inp, 2)
```
